# Optimizing a Trainium2 kernel written in Bass

```python
import jax, jax.numpy as jnp
from jax import lax
import numpy as np

D_MODEL = 2048
BATCH = 4
SEQ = 4096
DEPTH = 2

GRID_W = 64
CTX_LEN = 256
W_RG = 2048
RG_BLOCKS = 16
RG_BLOCK = W_RG // RG_BLOCKS
RG_C = 8.0
CONV_W = 4
M_HEADS = 8
M_HEAD_DIM = 256
W_M = M_HEADS * M_HEAD_DIM
M_CHUNK = 128
C_RGG = W_RG
C_M = 2 * W_RG
M_PART = 4 * W_M + 4 * M_HEADS
C_G = C_M + M_PART
N_IN = C_G + 2 * D_MODEL
D_FF = 7168
N_EXPERTS = 8
TOP_K = 2
MOE_BLOCK = 256
N_DENSE = (DEPTH + 1) // 2
N_MOE = DEPTH // 2
ALPHA = (2.0 * DEPTH) ** 0.25
BETA = (8.0 * DEPTH) ** -0.25
LN_EPS = 1e-6

kernel_name = "hybrid_rglru_mlstm_moe_diffusion_block"


def layer_norm(t):
    tf = t.astype(jnp.float32)
    mu = jnp.mean(tf, axis=-1, keepdims=True)
    var = jnp.mean(jnp.square(tf - mu), axis=-1, keepdims=True)
    return ((tf - mu) * lax.rsqrt(var + LN_EPS)).astype(t.dtype)


def ln_affine(t, g, b):
    return layer_norm(t) * g + b


def modulate(t, shift, scale):
    return layer_norm(t) * (1.0 + scale) + shift


def centred_dwconv(t, w, b):
    L = t.shape[1]
    tp = jnp.pad(t, ((0, 0), (CONV_W // 2, CONV_W - 1 - CONV_W // 2), (0, 0)))
    y = b
    for j in range(CONV_W):
        y = y + w[j] * tp[:, j:j + L]
    return y


def to_colmajor(t):
    B, L, C = t.shape
    rows = L // GRID_W
    return t.reshape(B, rows, GRID_W, C).transpose(0, 2, 1, 3).reshape(B, L, C)


def from_colmajor(t):
    B, L, C = t.shape
    rows = L // GRID_W
    return t.reshape(B, GRID_W, rows, C).transpose(0, 2, 1, 3).reshape(B, L, C)


def _lin_combine(e1, e2):
    a1, b1 = e1
    a2, b2 = e2
    return a1 * a2, a2 * b1 + b2


def linear_scan(a, b, h0, reverse):
    if reverse:
        b = b.at[:, -1].add(a[:, -1] * h0)
    else:
        b = b.at[:, 0].add(a[:, 0] * h0)
    _, h = lax.associative_scan(_lin_combine, (a, b), axis=1, reverse=reverse)
    return h


def rglru_coeffs(xc, wa, ba, wx, bx, lam):
    B, L, _ = xc.shape
    xh = xc.reshape(B, L, RG_BLOCKS, RG_BLOCK)
    r = jax.nn.sigmoid(jnp.einsum('blhi,hij->blhj', xh, wa).reshape(B, L, W_RG) + ba)
    i = jax.nn.sigmoid(jnp.einsum('blhi,hij->blhj', xh, wx).reshape(B, L, W_RG) + bx)
    log_a = -RG_C * r * jax.nn.softplus(-lam)
    a = jnp.exp(log_a)
    b = jnp.sqrt(-jnp.expm1(2.0 * log_a)) * (i * xc)
    return a, b


def rglru_bidir(xc, gate_pre, h0, wa, ba, wx, bx, lam):
    xc = xc.astype(jnp.float32)
    hs = []
    for d, rev in ((0, False), (1, True)):
        a, b = rglru_coeffs(xc, wa[d], ba[d], wx[d], bx[d], lam[d])
        hs.append(linear_scan(a, b, h0[d], rev))
    y = (hs[0] + hs[1]) * jax.nn.gelu(gate_pre.astype(jnp.float32))
    return y, (hs[0][:, -1], hs[1][:, 0])


def mlstm_chunkwise(q, k, v, ig, lf, state):
    B, H, L, DH = q.shape
    nc = L // M_CHUNK

    def chunks(t):
        return jnp.moveaxis(t.reshape((B, H, nc, M_CHUNK) + t.shape[3:]), 2, 0)

    mask = jnp.tril(jnp.ones((M_CHUNK, M_CHUNK), dtype=bool))

    def step(carry, inp):
        C, n, m = carry
        qc, kc, vc, ic, fc = inp
        bcum = jnp.cumsum(fc, axis=-1)
        log_d = jnp.where(mask, bcum[..., :, None] - bcum[..., None, :] + ic[..., None, :], -jnp.inf)
        log_inter = bcum + m[..., None]
        m_t = jnp.maximum(jnp.max(log_d, axis=-1), log_inter)
        s = jnp.einsum('bhtd,bhsd->bhts', qc, kc) * jnp.exp(log_d - m_t[..., None])
        w_inter = jnp.exp(log_inter - m_t)
        num = jnp.einsum('bhts,bhsd->bhtd', s, vc) + w_inter[..., None] * jnp.einsum('bhed,bhtd->bhte', C, qc)
        den = jnp.sum(s, axis=-1) + w_inter * jnp.einsum('bhd,bhtd->bht', n, qc)
        h = num / jnp.maximum(jnp.abs(den), jnp.exp(-m_t))[..., None]
        b_last = bcum[..., -1]
        log_w = b_last[..., None] - bcum + ic
        m_new = jnp.maximum(b_last + m, jnp.max(log_w, axis=-1))
        w = jnp.exp(log_w - m_new[..., None])
        decay = jnp.exp(b_last + m - m_new)
        C = decay[..., None, None] * C + jnp.einsum('bhs,bhse,bhsd->bhed', w, vc, kc)
        n = decay[..., None] * n + jnp.einsum('bhs,bhsd->bhd', w, kc)
        return (C, n, m_new), h

    state, h = lax.scan(step, state, (chunks(q), chunks(k), chunks(v), chunks(ig), chunks(lf)))
    h = jnp.moveaxis(h, 0, 2).reshape(B, H, L, DH)
    return h, state


def mlstm_zero_state(B):
    z = (jnp.zeros((B, M_HEADS, M_HEAD_DIM, M_HEAD_DIM), jnp.float32),
         jnp.zeros((B, M_HEADS, M_HEAD_DIM), jnp.float32),
         jnp.zeros((B, M_HEADS), jnp.float32))
    return (z, z)


def mlstm_branch(mp, state0, conv_w, conv_b, gate_b, gn_g):
    B, L, _ = mp.shape
    qk = jax.nn.silu(centred_dwconv(mp[..., :2 * W_M], conv_w, conv_b))
    v = mp[..., 2 * W_M:3 * W_M]
    o = mp[..., 3 * W_M:4 * W_M]
    g = mp[..., 4 * W_M:].reshape(B, L, 2, 2, M_HEADS).astype(jnp.float32) + gate_b

    def heads(t):
        return t.reshape(B, L, M_HEADS, M_HEAD_DIM).transpose(0, 2, 1, 3).astype(jnp.float32)

    q = heads(qk[..., :W_M]) * (M_HEAD_DIM ** -0.5)
    k = heads(qk[..., W_M:])
    v = heads(v)
    ig = g[:, :, :, 0].transpose(0, 2, 3, 1)
    lf = jax.nn.log_sigmoid(g[:, :, :, 1]).transpose(0, 2, 3, 1)
    flip = lambda t: jnp.flip(t, axis=2)
    h_f, st_f = mlstm_chunkwise(q, k, v, ig[:, 0], lf[:, 0], state0[0])
    h_b, st_b = mlstm_chunkwise(flip(q), flip(k), flip(v), flip(ig[:, 1]), flip(lf[:, 1]), state0[1])
    h = h_f + flip(h_b)
    mu = jnp.mean(h, axis=-1, keepdims=True)
    var = jnp.mean(jnp.square(h - mu), axis=-1, keepdims=True)
    h = ((h - mu) * lax.rsqrt(var + LN_EPS)).transpose(0, 2, 1, 3).reshape(B, L, W_M) * gn_g
    y = h * jax.nn.sigmoid(o.astype(jnp.float32))
    return y, (st_f, st_b)


def mixer(u_c, u_l, w_in, conv_rg_w, conv_rg_b, conv_m_w, conv_m_b, rg_wa, rg_ba, rg_wx, rg_bx, rg_lam,
          m_gate_b, m_gn_g, p_rg, p_m, w_out, ctx_out):
    B = u_l.shape[0]
    pc = u_c @ w_in
    pl = u_l @ w_in
    z = jnp.zeros((B, W_RG), jnp.float32)
    rgp = (rg_wa, rg_ba, rg_wx, rg_bx, rg_lam)
    yr_c, st_rg = rglru_bidir(centred_dwconv(pc[..., :W_RG], conv_rg_w, conv_rg_b), pc[..., C_RGG:C_M], (z, z), *rgp)
    yr_l, _ = rglru_bidir(centred_dwconv(pl[..., :W_RG], conv_rg_w, conv_rg_b), pl[..., C_RGG:C_M], st_rg, *rgp)
    ym_c, st_m = mlstm_branch(pc[..., C_M:C_G], mlstm_zero_state(B), conv_m_w, conv_m_b, m_gate_b, m_gn_g)
    ym_l, _ = mlstm_branch(to_colmajor(pl[..., C_M:C_G]), st_m, conv_m_w, conv_m_b, m_gate_b, m_gn_g)
    ym_l = from_colmajor(ym_l)

    def merge(p, yr, ym):
        g = jax.nn.sigmoid(p[..., C_G:])
        mix = g[..., :D_MODEL] * (yr.astype(p.dtype) @ p_rg) + g[..., D_MODEL:] * (ym.astype(p.dtype) @ p_m)
        return mix @ w_out

    out_l = merge(pl, yr_l, ym_l)
    out_c = merge(pc, yr_c, ym_c) if ctx_out else None
    return out_c, out_l


def swiglu(t, w1, w3, w2):
    return (jax.nn.silu(t @ w1) * (t @ w3)) @ w2


def moe_swiglu(t, router_w, router_b, w1, w3, w2):
    n = t.shape[0]
    logits = (t @ router_w).astype(jnp.float32) + router_b
    top_val, top_idx = lax.top_k(logits, TOP_K)
    gates = jax.nn.softmax(top_val, axis=-1)
    expert = top_idx.reshape(-1)
    token = jnp.repeat(jnp.arange(n), TOP_K)
    weight = gates.reshape(-1)
    n_assign = n * TOP_K
    order = jnp.argsort(expert)
    e_s, tok_s, w_s = expert[order], token[order], weight[order]
    counts = jnp.bincount(expert, length=N_EXPERTS)
    padded = (counts + MOE_BLOCK - 1) // MOE_BLOCK * MOE_BLOCK
    pend = jnp.cumsum(padded)
    pstart = pend - padded
    sstart = jnp.cumsum(counts) - counts
    pos = pstart[e_s] + jnp.arange(n_assign) - sstart[e_s]
    n_blocks = -(-n_assign // MOE_BLOCK) + N_EXPERTS
    xb = jnp.zeros((n_blocks * MOE_BLOCK, t.shape[1]), t.dtype).at[pos].set(t[tok_s])
    block_e = jnp.minimum(jnp.searchsorted(pend, jnp.arange(n_blocks) * MOE_BLOCK, side='right'), N_EXPERTS - 1)

    def run(args):
        xg, e = args
        return swiglu(xg, w1[e], w3[e], w2[e])

    yb = lax.map(run, (xb.reshape(n_blocks, MOE_BLOCK, -1), block_e)).reshape(n_blocks * MOE_BLOCK, -1)
    return jnp.zeros_like(t).at[tok_s].add((yb[pos] * w_s[:, None]).astype(t.dtype))


def setup_inputs(seed: int = 0) -> dict:
    key = jax.random.key(seed)
    ks = jax.random.split(key, 32)
    f32 = jnp.float32

    def nrm(k, shape, scale):
        return jax.random.normal(k, shape, f32) * scale

    u = jax.random.uniform(ks[15], (DEPTH, 2, W_RG), f32, 0.9, 0.999)
    a0 = u ** (1.0 / RG_C)
    rg_lam = jnp.log(a0) - jnp.log1p(-a0)
    ib = nrm(ks[16], (DEPTH, 2, M_HEADS), 0.1)
    fb = jnp.linspace(3.0, 6.0, M_HEADS, dtype=f32) + nrm(ks[17], (DEPTH, 2, M_HEADS), 0.1)
    return {
        "x": nrm(ks[0], (BATCH, SEQ, D_MODEL), 1.0),
        "c": nrm(ks[1], (BATCH, D_MODEL), 1.0),
        "ctx": nrm(ks[2], (BATCH, CTX_LEN, D_MODEL), 1.0),
        "c_ctx": nrm(ks[3], (D_MODEL,), 1.0),
        "w_mod": nrm(ks[4], (DEPTH, D_MODEL, 6 * D_MODEL), 0.5 * D_MODEL ** -0.5),
        "b_mod": nrm(ks[5], (DEPTH, 6 * D_MODEL), 0.01),
        "w_in": nrm(ks[6], (DEPTH, D_MODEL, N_IN), D_MODEL ** -0.5),
        "conv_rg_w": nrm(ks[7], (DEPTH, CONV_W, W_RG), CONV_W ** -0.5),
        "conv_rg_b": nrm(ks[8], (DEPTH, W_RG), 0.01),
        "conv_m_w": nrm(ks[9], (DEPTH, CONV_W, 2 * W_M), CONV_W ** -0.5),
        "conv_m_b": nrm(ks[10], (DEPTH, 2 * W_M), 0.01),
        "rg_wa": nrm(ks[11], (DEPTH, 2, RG_BLOCKS, RG_BLOCK, RG_BLOCK), RG_BLOCK ** -0.5),
        "rg_ba": nrm(ks[12], (DEPTH, 2, W_RG), 0.01),
        "rg_wx": nrm(ks[13], (DEPTH, 2, RG_BLOCKS, RG_BLOCK, RG_BLOCK), RG_BLOCK ** -0.5),
        "rg_bx": nrm(ks[14], (DEPTH, 2, W_RG), 0.01),
        "rg_lam": rg_lam,
        "m_gate_b": jnp.stack([ib, fb], axis=2),
        "m_gn_g": 1.0 + nrm(ks[18], (DEPTH, W_M), 0.01),
        "p_rg": nrm(ks[19], (DEPTH, W_RG, D_MODEL), W_RG ** -0.5),
        "p_m": nrm(ks[20], (DEPTH, W_M, D_MODEL), W_M ** -0.5),
        "w_out": nrm(ks[21], (DEPTH, D_MODEL, D_MODEL), BETA * D_MODEL ** -0.5),
        "ln_g": 1.0 + nrm(ks[22], (DEPTH, 2, D_MODEL), 0.01),
        "ln_b": nrm(ks[23], (DEPTH, 2, D_MODEL), 0.01),
        "ff_w1": nrm(ks[24], (N_DENSE, D_MODEL, D_FF), D_MODEL ** -0.5),
        "ff_w3": nrm(ks[25], (N_DENSE, D_MODEL, D_FF), D_MODEL ** -0.5),
        "ff_w2": nrm(ks[26], (N_DENSE, D_FF, D_MODEL), BETA * D_FF ** -0.5),
        "router_w": nrm(ks[27], (N_MOE, D_MODEL, N_EXPERTS), D_MODEL ** -0.5),
        "router_b": nrm(ks[28], (N_MOE, N_EXPERTS), 0.01),
        "ex_w1": nrm(ks[29], (N_MOE, N_EXPERTS, D_MODEL, D_FF), D_MODEL ** -0.5),
        "ex_w3": nrm(ks[30], (N_MOE, N_EXPERTS, D_MODEL, D_FF), D_MODEL ** -0.5),
        "ex_w2": nrm(ks[31], (N_MOE, N_EXPERTS, D_FF, D_MODEL), BETA * D_FF ** -0.5),
    }


def reference(x, c, ctx, c_ctx, w_mod, b_mod, w_in, conv_rg_w, conv_rg_b, conv_m_w, conv_m_b, rg_wa, rg_ba,
              rg_wx, rg_bx, rg_lam, m_gate_b, m_gn_g, p_rg, p_m, w_out, ln_g, ln_b, ff_w1, ff_w3, ff_w2,
              router_w, router_b, ex_w1, ex_w3, ex_w2):
    B, L, D = x.shape
    h_l, h_c = x, ctx
    s_lat = jax.nn.silu(c)
    s_ctx = jax.nn.silu(c_ctx)
    for l in range(DEPTH):
        last = l == DEPTH - 1
        mod_l = (s_lat @ w_mod[l] + b_mod[l])[:, None, :]
        mod_c = s_ctx @ w_mod[l] + b_mod[l]
        sh1_l, sc1_l, g1_l, sh2_l, sc2_l, g2_l = jnp.split(mod_l, 6, axis=-1)
        sh1_c, sc1_c, g1_c, sh2_c, sc2_c, g2_c = jnp.split(mod_c, 6, axis=-1)
        o_c, o_l = mixer(modulate(h_c, sh1_c, sc1_c), modulate(h_l, sh1_l, sc1_l), w_in[l],
                         conv_rg_w[l], conv_rg_b[l], conv_m_w[l], conv_m_b[l], rg_wa[l], rg_ba[l], rg_wx[l],
                         rg_bx[l], rg_lam[l], m_gate_b[l], m_gn_g[l], p_rg[l], p_m[l], w_out[l], not last)
        h_l = ln_affine(ALPHA * h_l + g1_l * o_l, ln_g[l, 0], ln_b[l, 0])
        v_l = modulate(h_l, sh2_l, sc2_l).reshape(B * L, D)
        if last:
            t = v_l
        else:
            h_c = ln_affine(ALPHA * h_c + g1_c * o_c, ln_g[l, 0], ln_b[l, 0])
            v_c = modulate(h_c, sh2_c, sc2_c).reshape(B * CTX_LEN, D)
            t = jnp.concatenate([v_c, v_l], axis=0)
        if l % 2 == 0:
            f = swiglu(t, ff_w1[l // 2], ff_w3[l // 2], ff_w2[l // 2])
        else:
            f = moe_swiglu(t, router_w[l // 2], router_b[l // 2], ex_w1[l // 2], ex_w3[l // 2], ex_w2[l // 2])
        n_c = 0 if last else B * CTX_LEN
        f_l = f[n_c:].reshape(B, L, D)
        h_l = ln_affine(ALPHA * h_l + g2_l * f_l, ln_g[l, 1], ln_b[l, 1])
        if not last:
            f_c = f[:n_c].reshape(B, CTX_LEN, D)
            h_c = ln_affine(ALPHA * h_c + g2_c * f_c, ln_g[l, 1], ln_b[l, 1])
    return h_l
```

```python
import numpy as np
import ml_dtypes
from contextlib import ExitStack
import concourse.bass as bass
import concourse.mybir as mybir
from concourse.bass_utils import run_bass_kernel_spmd

F32 = mybir.dt.float32
BF16 = mybir.dt.bfloat16
AF = mybir.ActivationFunctionType
ALU = mybir.AluOpType
AX = mybir.AxisListType
NPBF = ml_dtypes.bfloat16

D = 2048
KD = 16
DFF = 7168
KF = 56
NCORE = 8
ALPHA = 4.0 ** 0.25
LN_EPS = 1e-6
GRID = 64
CTX = 256
SEQ = 4096
NB = 4
W_RG = 2048
W_M = 2048
C_RGG = W_RG
C_M = 2 * W_RG
C_G = C_M + 4 * W_M + 32
N_IN = C_G + 2 * D


class Res:
    __slots__ = ("last_w", "readers")

    def __init__(self):
        self.last_w = None
        self.readers = []


class Op:
    __slots__ = ("eng", "fn", "deps", "dma", "idx", "signal", "count", "slot", "slot_count")

    def __init__(self, eng, fn, dma):
        self.eng = eng
        self.fn = fn
        self.deps = []
        self.dma = dma
        self.signal = False
        self.count = 0
        self.slot = None
        self.slot_count = 0


class Prog:
    ENGS = ("pe", "act", "dve", "pool", "sp")
    NSLOT = 8

    def __init__(self, nc):
        self.nc = nc
        self.ops = []
        self.es = ExitStack()
        self._n = 0

    def sbuf(self, shape, dtype):
        self._n += 1
        return self.es.enter_context(self.nc.sbuf_tensor(f"sb{self._n}_{id(self) % 9973}", list(shape), dtype))

    def psum(self, shape, dtype):
        self._n += 1
        return self.es.enter_context(self.nc.psum_tensor(f"ps{self._n}_{id(self) % 9973}", list(shape), dtype))

    def add(self, eng, fn, reads=(), writes=(), dma=False):
        op = Op(eng, fn, dma)
        op.idx = len(self.ops)
        deps = {}
        for r in reads:
            if r.last_w is not None:
                deps[r.last_w.idx] = r.last_w
        for w in writes:
            if w.last_w is not None:
                deps[w.last_w.idx] = w.last_w
            for rd in w.readers:
                deps[rd.idx] = rd
        last = {}
        for d in deps.values():
            if d.dma:
                op.deps.append(d)
                continue
            if d.eng == "pe" and eng == "pe" and not dma:
                continue
            if d.eng not in last or last[d.eng].idx < d.idx:
                last[d.eng] = d
        op.deps.extend(last.values())
        for r in reads:
            r.readers.append(op)
        for w in writes:
            w.last_w = op
            w.readers = []
        self.ops.append(op)
        return op

    def dma(self, q, out, in_, reads=(), writes=()):
        return self.add(q, lambda e: e.dma_start(out=out, in_=in_), reads, writes, dma=True)

    def emit(self):
        nc = self.nc
        ops = self.ops
        for op in ops:
            for d in op.deps:
                d.signal = True
        counts = {e: 0 for e in self.ENGS}
        dcount = {e: 0 for e in self.ENGS}
        slot_counts = {e: [0] * self.NSLOT for e in self.ENGS}
        for op in ops:
            if op.dma:
                k = dcount[op.eng]
                dcount[op.eng] += 1
                op.slot = k % self.NSLOT
                slot_counts[op.eng][op.slot] += 1
                op.slot_count = slot_counts[op.eng][op.slot]
            elif op.signal:
                counts[op.eng] += 1
                op.count = counts[op.eng]
        tag = id(self) % 9973
        sems = {e: self.es.enter_context(nc.semaphore(f"s_{e}_{tag}")) for e in self.ENGS}
        dsems = {}
        for e in self.ENGS:
            if dcount[e]:
                dsems[e] = [self.es.enter_context(nc.semaphore(f"d_{e}{i}_{tag}"))
                            for i in range(min(self.NSLOT, dcount[e]))]
        by_eng = {e: [op for op in ops if op.eng == e] for e in self.ENGS}
        block = self.es.enter_context(nc.Block())

        def run(e, h):
            seen = {}
            for op in by_eng[e]:
                waits = {}
                for d in op.deps:
                    if d.dma:
                        key = ("d", d.eng, d.slot)
                        val = 16 * d.slot_count
                        sem = dsems[d.eng][d.slot]
                    else:
                        key = ("c", d.eng)
                        val = d.count
                        sem = sems[d.eng]
                    if seen.get(key, 0) >= val:
                        continue
                    if key not in waits or waits[key][1] < val:
                        waits[key] = (sem, val)
                if op.dma and op.slot_count > 1:
                    key = ("d", e, op.slot)
                    val = 16 * (op.slot_count - 1)
                    if seen.get(key, 0) < val and (key not in waits or waits[key][1] < val):
                        waits[key] = (dsems[e][op.slot], val)
                for key, (sem, val) in waits.items():
                    h.wait_ge(sem, val)
                    seen[key] = val
                ins = op.fn(h)
                if op.dma:
                    ins.then_inc(dsems[e][op.slot], 16)
                elif op.signal:
                    ins.then_inc(sems[e], 1)
            if e in dsems:
                for s in range(len(dsems[e])):
                    val = 16 * slot_counts[e][s]
                    if seen.get(("d", e, s), 0) < val:
                        h.wait_ge(dsems[e][s], val)

        @block.tensor
        def _(h):
            run("pe", h)

        @block.scalar
        def _(h):
            run("act", h)

        @block.vector
        def _(h):
            run("dve", h)

        @block.gpsimd
        def _(h):
            run("pool", h)

        @block.sync
        def _(h):
            run("sp", h)

        self.es.close()


class Rot:
    def __init__(self, P, n, shape, dtype, psum=False):
        self.bufs = [(P.psum(shape, dtype) if psum else P.sbuf(shape, dtype)) for _ in range(n)]
        self.res = [Res() for _ in range(n)]
        self.i = 0
        self.n = n

    def next(self):
        j = self.i % self.n
        self.i += 1
        return self.bufs[j], self.res[j]


def new_nc():
    return bass.Bass("TRN2", target_bir_lowering=False)


def din(nc, name, shape, dt):
    return nc.dram_tensor(name, list(shape), dt, kind="ExternalInput").ap()


def dout(nc, name, shape, dt):
    return nc.dram_tensor(name, list(shape), dt, kind="ExternalOutput").ap()


class Ctx:
    def __init__(self, P, wslots=6, wcols=4096, ntmp=6, nmm=6):
        self.P = P
        self.mm = Rot(P, nmm, [128, 512], F32, psum=True)
        self.st = Rot(P, 2, [128, 512], F32, psum=True)
        self.tmp = Rot(P, ntmp, [128, 512], F32)
        self.wp = Rot(P, wslots, [128, wcols], BF16) if wslots else None
        self.ones = P.sbuf([128, 128], F32)
        self.r_ones = Res()
        P.add("dve", lambda e: e.memset(self.ones[:], 1.0), writes=[self.r_ones])
        self.stat = Rot(P, 4, [128, 512], F32)

    def wtile(self, src, nk, ncols):
        buf, res = self.wp.next()
        view = buf[:, 0:nk * ncols].rearrange("p (k c) -> p k c", k=nk)
        self.P.dma("pool", view, src.rearrange("(k p) c -> p k c", p=128), writes=[res])
        return view, res


def fm_mm(cx, wview, wres, c0, xs, xres, G, nk=None, koff=0, ps=None, first=True, last=True):
    P = cx.P
    if ps is None:
        ps = cx.mm.next()
    pbuf, pres = ps
    nk = nk if nk is not None else KD
    for k in range(nk):
        P.add("pe", lambda e, k=k: e.matmul(pbuf[:, 0:G], wview[:, k, c0:c0 + 128], xs(koff + k),
                                            start=(first and k == 0), stop=(last and k == nk - 1)),
              reads=[wres, xres(koff + k)], writes=[pres])
    return ps


def layer_norm_fm(cx, src, src_res, G, outs):
    P = cx.P
    s1, r1 = cx.st.next()
    s2, r2 = cx.st.next()
    for c in range(KD):
        sq, rsq = cx.tmp.next()
        P.add("act", lambda e, c=c, sq=sq: e.activation(sq[:, 0:G], src(c), AF.Square), reads=[src_res(c)], writes=[rsq])
        P.add("pe", lambda e, c=c: e.matmul(s1[:, 0:G], cx.ones[:], src(c), start=(c == 0), stop=(c == KD - 1)),
              reads=[cx.r_ones, src_res(c)], writes=[r1])
        P.add("pe", lambda e, c=c, sq=sq: e.matmul(s2[:, 0:G], cx.ones[:], sq[:, 0:G], start=(c == 0), stop=(c == KD - 1)),
              reads=[cx.r_ones, rsq], writes=[r2])
    mean, rm = cx.stat.next()
    msq, rq = cx.stat.next()
    var, rv = cx.stat.next()
    rstd, rr = cx.stat.next()
    P.add("act", lambda e: e.mul(mean[:, 0:G], s1[:, 0:G], 1.0 / D), reads=[r1], writes=[rm])
    P.add("dve", lambda e: e.tensor_tensor(msq[:, 0:G], mean[:, 0:G], mean[:, 0:G], ALU.mult), reads=[rm], writes=[rq])
    P.add("dve", lambda e: e.scalar_tensor_tensor(var[:, 0:G], s2[:, 0:G], 1.0 / D, msq[:, 0:G], ALU.mult, ALU.subtract),
          reads=[r2, rq], writes=[rv])
    P.add("act", lambda e: e.activation(var[:, 0:G], var[:, 0:G], AF.Sqrt, bias=LN_EPS, scale=1.0), reads=[rv], writes=[rv])
    P.add("dve", lambda e: e.reciprocal(rstd[:, 0:G], var[:, 0:G]), reads=[rv], writes=[rr])
    for c in range(KD):
        t1, rt1 = cx.tmp.next()
        P.add("dve", lambda e, c=c, t1=t1: e.tensor_tensor(t1[:, 0:G], src(c), mean[:, 0:G], ALU.subtract),
              reads=[src_res(c), rm], writes=[rt1])
        P.add("dve", lambda e, t1=t1: e.tensor_tensor(t1[:, 0:G], t1[:, 0:G], rstd[:, 0:G], ALU.mult),
              reads=[rt1, rr], writes=[rt1])
        for (dst, dres, sc, bi, extra) in outs:
            P.add("act", lambda e, c=c, t1=t1, dst=dst, sc=sc, bi=bi: e.activation(dst(c), t1[:, 0:G], AF.Identity, bias=bi(c), scale=sc(c)),
                  reads=[rt1] + list(extra), writes=[dres(c)])


def ffn_fm(cx, w1, w3, w2, vview, vres, G, act_view, act_res, consume):
    P = cx.P
    for f2 in range(KF // 2):
        t1, rw1 = cx.wtile(w1[:, f2 * 256:(f2 + 1) * 256], KD, 256)
        t3, rw3 = cx.wtile(w3[:, f2 * 256:(f2 + 1) * 256], KD, 256)
        for cc in range(2):
            f = 2 * f2 + cc
            p1, rp1 = fm_mm(cx, t1, rw1, cc * 128, vview, vres, G)
            p3, rp3 = fm_mm(cx, t3, rw3, cc * 128, vview, vres, G)
            s, rs = cx.tmp.next()
            P.add("act", lambda e, p1=p1, s=s: e.activation(s[:, 0:G], p1[:, 0:G], AF.Silu), reads=[rp1], writes=[rs])
            P.add("dve", lambda e, f=f, p3=p3, s=s: e.tensor_tensor(act_view(f), s[:, 0:G], p3[:, 0:G], ALU.mult),
                  reads=[rs, rp3], writes=[act_res(f)])
    for c in range(KD):
        ta, ra = cx.wtile(w2[0:(KF // 2) * 128, c * 128:(c + 1) * 128], KF // 2, 128)
        tb, rb = cx.wtile(w2[(KF // 2) * 128:KF * 128, c * 128:(c + 1) * 128], KF // 2, 128)
        ps = cx.mm.next()
        fm_mm(cx, ta, ra, 0, act_view, act_res, G, nk=KF // 2, koff=0, ps=ps, first=True, last=False)
        fm_mm(cx, tb, rb, 0, act_view, act_res, G, nk=KF // 2, koff=KF // 2, ps=ps, first=False, last=True)
        consume(c, ps[0], ps[1])


V_G1, V_SC2, V_SH2, V_G2, V_NSC1, V_NSH1 = range(6)
V_LN = 12


def build_B(groups, T, ffn, router):
    nc = new_nc()
    yrT = din(nc, "yrT", [D, T], BF16)
    ymT = din(nc, "ymT", [D, T], BF16)
    uT = din(nc, "uT", [D, T], BF16)
    hT = din(nc, "hT", [D, T], F32)
    w_g = din(nc, "w_g", [D, 2 * D], F32)
    p_rg = din(nc, "p_rg", [W_RG, D], F32)
    p_m = din(nc, "p_m", [W_M, D], F32)
    w_out = din(nc, "w_out", [D, D], F32)
    vec = din(nc, "vec", [128, 16, KD], F32)
    if ffn:
        w1 = din(nc, "w1", [D, DFF], F32)
        w3 = din(nc, "w3", [D, DFF], F32)
        w2 = din(nc, "w2", [DFF, D], F32)
        hT_out = dout(nc, "hT_out", [D, T], F32)
        uT_out = dout(nc, "uT_out", [D, T], BF16)
    if router:
        rw = din(nc, "rw", [D, 8], F32)
        rb = din(nc, "rb", [128, 8], F32)
        h1T_out = dout(nc, "h1T_out", [D, T], F32)
        vT_out = dout(nc, "vT_out", [D, T], BF16)
        gw_out = dout(nc, "gw", [T, 8], F32)

    P = Prog(nc)
    cx = Ctx(P, wslots=5, ntmp=4) if router else Ctx(P)
    vs = P.sbuf([128, 16, KD], F32)
    r_vs = Res()
    P.dma("sp", vs[:], vec, writes=[r_vs])
    for kind in range(2):
        for slot in (V_SC2, V_NSC1):
            j = kind * 6 + slot
            P.add("dve", lambda e, j=j: e.tensor_scalar_add(vs[:, j, :], vs[:, j, :], 1.0), reads=[r_vs], writes=[r_vs])

    def vcol(kind, slot):
        j = (kind * 6 + slot) if slot < 6 else slot
        return lambda c: vs[:, j, c:c + 1]

    scr = P.sbuf([128, 64 * 512], BF16)
    rj = [Res() for _ in range(64)]
    hbuf = P.sbuf([128, KD, 512], F32)
    rh = [Res() for _ in range(KD)]
    vbuf = P.sbuf([128, KD, 512], BF16)
    rv = [Res() for _ in range(KD)]
    if router:
        v32 = P.sbuf([128, KD, 512], F32)
        rv32 = [Res() for _ in range(KD)]
        rws = P.sbuf([128, KD, 8], F32)
        r_rws = Res()
        P.dma("sp", rws[:], rw.rearrange("(k p) e -> p k e", p=128), writes=[r_rws])
        rbs = P.sbuf([128, 8], F32)
        r_rbs = Res()
        P.dma("sp", rbs[:], rb, writes=[r_rbs])
        small = Rot(P, 12, [128, 8], F32)

    def do_group(tok0, G, kind):
        def q(qi, k, G=G):
            return scr[:, (qi * 16 + k) * 512:(qi * 16 + k) * 512 + G]

        def qall(qi, G=G):
            return scr[:, qi * 16 * 512:(qi + 1) * 16 * 512].rearrange("p (k t) -> p k t", k=16)[:, :, 0:G]

        for qi, src in enumerate((yrT, ymT, uT)):
            P.dma("sp", qall(qi), src[:, tok0:tok0 + G].rearrange("(k p) t -> p k t", p=128),
                  writes=rj[qi * 16:(qi + 1) * 16])
        P.dma("sp", hbuf[:, :, 0:G], hT[:, tok0:tok0 + G].rearrange("(k p) t -> p k t", p=128), writes=rh)
        for c in range(KD):
            P.add("act", lambda e, c=c, G=G: e.mul(hbuf[:, c, 0:G], hbuf[:, c, 0:G], ALPHA), reads=[rh[c]], writes=[rh[c]])
        for c2 in range(KD // 2):
            cs = slice(c2 * 256, (c2 + 1) * 256)
            tg1, rg1 = cx.wtile(w_g[:, cs], KD, 256)
            tg2, rg2 = cx.wtile(w_g[:, D + c2 * 256:D + (c2 + 1) * 256], KD, 256)
            tp1, rp1 = cx.wtile(p_rg[:, cs], KD, 256)
            tp2, rp2 = cx.wtile(p_m[:, cs], KD, 256)
            for cc in range(2):
                c = 2 * c2 + cc
                pg1, rpg1 = fm_mm(cx, tg1, rg1, cc * 128, lambda k: q(2, k), lambda k: rj[32 + k], G)
                pg2, rpg2 = fm_mm(cx, tg2, rg2, cc * 128, lambda k: q(2, k), lambda k: rj[32 + k], G)
                pa1, rpa1 = fm_mm(cx, tp1, rp1, cc * 128, lambda k: q(0, k), lambda k: rj[k], G)
                pa2, rpa2 = fm_mm(cx, tp2, rp2, cc * 128, lambda k: q(1, k), lambda k: rj[16 + k], G)
                s1, rs1 = cx.tmp.next()
                s2, rs2 = cx.tmp.next()
                P.add("act", lambda e, pg1=pg1, s1=s1, G=G: e.activation(s1[:, 0:G], pg1[:, 0:G], AF.Sigmoid), reads=[rpg1], writes=[rs1])
                P.add("act", lambda e, pg2=pg2, s2=s2, G=G: e.activation(s2[:, 0:G], pg2[:, 0:G], AF.Sigmoid), reads=[rpg2], writes=[rs2])
                P.add("dve", lambda e, pa1=pa1, s1=s1, G=G: e.tensor_tensor(s1[:, 0:G], s1[:, 0:G], pa1[:, 0:G], ALU.mult), reads=[rs1, rpa1], writes=[rs1])
                P.add("dve", lambda e, pa2=pa2, s2=s2, G=G: e.tensor_tensor(s2[:, 0:G], s2[:, 0:G], pa2[:, 0:G], ALU.mult), reads=[rs2, rpa2], writes=[rs2])
                P.add("dve", lambda e, c=c, s1=s1, s2=s2, G=G: e.tensor_tensor(q(3, c, G), s1[:, 0:G], s2[:, 0:G], ALU.add),
                      reads=[rs1, rs2], writes=[rj[48 + c]])
        g1 = vcol(kind, V_G1)
        for c2 in range(KD // 2):
            to, ro = cx.wtile(w_out[:, c2 * 256:(c2 + 1) * 256], KD, 256)
            for cc in range(2):
                c = 2 * c2 + cc
                po, rpo = fm_mm(cx, to, ro, cc * 128, lambda k: q(3, k), lambda k: rj[48 + k], G)
                P.add("dve", lambda e, c=c, po=po, G=G, g1=g1: e.scalar_tensor_tensor(hbuf[:, c, 0:G], po[:, 0:G], g1(c), hbuf[:, c, 0:G], ALU.mult, ALU.add),
                      reads=[rpo, rh[c], r_vs], writes=[rh[c]])
        hsrc = lambda c, G=G: hbuf[:, c, 0:G]
        layer_norm_fm(cx, hsrc, lambda c: rh[c], G,
                      [(hsrc, lambda c: rh[c], vcol(0, V_LN + 0), vcol(0, V_LN + 1), [r_vs])])
        outs = [(lambda c, G=G: vbuf[:, c, 0:G], lambda c: rv[c], vcol(kind, V_SC2), vcol(kind, V_SH2), [r_vs])]
        if router:
            outs.append((lambda c, G=G: v32[:, c, 0:G], lambda c: rv32[c], vcol(kind, V_SC2), vcol(kind, V_SH2), [r_vs]))
        layer_norm_fm(cx, hsrc, lambda c: rh[c], G, outs)
        if ffn:
            g2 = vcol(kind, V_G2)
            for c in range(KD):
                P.add("act", lambda e, c=c, G=G: e.mul(hbuf[:, c, 0:G], hbuf[:, c, 0:G], ALPHA), reads=[rh[c]], writes=[rh[c]])

            def consume(c, pbuf, pres, G=G, g2=g2):
                P.add("dve", lambda e: e.scalar_tensor_tensor(hbuf[:, c, 0:G], pbuf[:, 0:G], g2(c), hbuf[:, c, 0:G], ALU.mult, ALU.add),
                      reads=[pres, rh[c], r_vs], writes=[rh[c]])

            ffn_fm(cx, w1, w3, w2, lambda k, G=G: vbuf[:, k, 0:G], lambda k: rv[k], G,
                   lambda f, G=G: scr[:, f * 512:f * 512 + G], lambda f: rj[f], consume)
            layer_norm_fm(cx, hsrc, lambda c: rh[c], G,
                          [(hsrc, lambda c: rh[c], vcol(0, V_LN + 2), vcol(0, V_LN + 3), [r_vs])])
            P.dma("sp", hT_out[:, tok0:tok0 + G].rearrange("(k p) t -> p k t", p=128), hbuf[:, :, 0:G], reads=rh)
            layer_norm_fm(cx, hsrc, lambda c: rh[c], G,
                          [(lambda c, G=G: vbuf[:, c, 0:G], lambda c: rv[c], vcol(kind, V_NSC1), vcol(kind, V_NSH1), [r_vs])])
            P.dma("sp", uT_out[:, tok0:tok0 + G].rearrange("(k p) t -> p k t", p=128), vbuf[:, :, 0:G], reads=rv)
        if router:
            P.dma("sp", h1T_out[:, tok0:tok0 + G].rearrange("(k p) t -> p k t", p=128), hbuf[:, :, 0:G], reads=rh)
            P.dma("sp", vT_out[:, tok0:tok0 + G].rearrange("(k p) t -> p k t", p=128), vbuf[:, :, 0:G], reads=rv)
            for t0 in range(0, G, 128):
                pl, rpl = cx.mm.next()
                for k in range(KD):
                    P.add("pe", lambda e, k=k, t0=t0, pl=pl: e.matmul(pl[:, 0:8], v32[:, k, t0:t0 + 128], rws[:, k, :], start=(k == 0), stop=(k == KD - 1)),
                          reads=[rv32[k], r_rws], writes=[rpl])
                lg, rlg = small.next()
                m1, rm1 = small.next()
                e1, re1 = small.next()
                l2, rl2 = small.next()
                m2, rm2 = small.next()
                e2, re2 = small.next()
                dd, rdd = small.next()
                gwt, rgw = small.next()
                P.add("dve", lambda e, pl=pl, lg=lg: e.tensor_tensor(lg[:], pl[:, 0:8], rbs[:], ALU.add), reads=[rpl, r_rbs], writes=[rlg])
                P.add("dve", lambda e, lg=lg, m1=m1: e.reduce_max(m1[:, 0:1], lg[:], AX.X), reads=[rlg], writes=[rm1])
                P.add("dve", lambda e, lg=lg, m1=m1, e1=e1: e.tensor_scalar(e1[:], lg[:], m1[:, 0:1], None, ALU.is_equal), reads=[rlg, rm1], writes=[re1])
                P.add("dve", lambda e, lg=lg, e1=e1, l2=l2: e.scalar_tensor_tensor(l2[:], e1[:], -1e30, lg[:], ALU.mult, ALU.add), reads=[rlg, re1], writes=[rl2])
                P.add("dve", lambda e, l2=l2, m2=m2: e.reduce_max(m2[:, 0:1], l2[:], AX.X), reads=[rl2], writes=[rm2])
                P.add("dve", lambda e, l2=l2, m2=m2, e2=e2: e.tensor_scalar(e2[:], l2[:], m2[:, 0:1], None, ALU.is_equal), reads=[rl2, rm2], writes=[re2])
                P.add("dve", lambda e, m1=m1, m2=m2, dd=dd: e.tensor_tensor(dd[:, 0:1], m2[:, 0:1], m1[:, 0:1], ALU.subtract), reads=[rm1, rm2], writes=[rdd])
                P.add("act", lambda e, dd=dd: e.activation(dd[:, 1:2], dd[:, 0:1], AF.Exp), reads=[rdd], writes=[rdd])
                P.add("dve", lambda e, dd=dd: e.tensor_scalar_add(dd[:, 2:3], dd[:, 1:2], 1.0), reads=[rdd], writes=[rdd])
                P.add("dve", lambda e, dd=dd: e.reciprocal(dd[:, 2:3], dd[:, 2:3]), reads=[rdd], writes=[rdd])
                P.add("dve", lambda e, dd=dd: e.tensor_tensor(dd[:, 3:4], dd[:, 1:2], dd[:, 2:3], ALU.mult), reads=[rdd], writes=[rdd])
                P.add("dve", lambda e, e1=e1, dd=dd, gwt=gwt: e.tensor_scalar(gwt[:], e1[:], dd[:, 2:3], None, ALU.mult), reads=[re1, rdd], writes=[rgw])
                P.add("dve", lambda e, e2=e2, dd=dd, gwt=gwt: e.scalar_tensor_tensor(gwt[:], e2[:], dd[:, 3:4], gwt[:], ALU.mult, ALU.add), reads=[re2, rdd, rgw], writes=[rgw])
                P.dma("sp", gw_out[tok0 + t0:tok0 + t0 + 128, :], gwt[:], reads=[rgw])
    for (tok0, G, kind) in groups:
        do_group(tok0, G, kind)
    P.emit()
    return nc


def build_C(T):
    nc = new_nc()
    vT = din(nc, "vT", [D, T], BF16)
    wrow = din(nc, "wrow", [128, T], F32)
    w1 = din(nc, "w1", [D, DFF], F32)
    w3 = din(nc, "w3", [D, DFF], F32)
    w2 = din(nc, "w2", [DFF, D], F32)
    yT = dout(nc, "yT", [D, T], F32)
    P = Prog(nc)
    cx = Ctx(P, wslots=8)
    vb = Rot(P, 2, [128, KD, 512], BF16)
    wr = Rot(P, 2, [128, 512], F32)
    scr = P.sbuf([128, KF * 512], BF16)
    rj = [Res() for _ in range(KF)]

    def do_group(tok0, G):
        vbuf, rvb = vb.next()
        wrt, rwr = wr.next()
        P.dma("sp", vbuf[:, :, 0:G], vT[:, tok0:tok0 + G].rearrange("(k p) t -> p k t", p=128), writes=[rvb])
        P.dma("sp", wrt[:, 0:G], wrow[:, tok0:tok0 + G], writes=[rwr])

        def consume(c, pbuf, pres):
            o, ro = cx.tmp.next()
            P.add("dve", lambda e: e.tensor_tensor(o[:, 0:G], pbuf[:, 0:G], wrt[:, 0:G], ALU.mult), reads=[pres, rwr], writes=[ro])
            P.dma("sp", yT[c * 128:(c + 1) * 128, tok0:tok0 + G], o[:, 0:G], reads=[ro])

        ffn_fm(cx, w1, w3, w2, lambda k: vbuf[:, k, 0:G], lambda k: rvb, G,
               lambda f: scr[:, f * 512:f * 512 + G], lambda f: rj[f], consume)

    for tok0 in range(0, T, 512):
        do_group(tok0, min(512, T - tok0))
    P.emit()
    return nc


def build_D(T):
    nc = new_nc()
    yaT = din(nc, "yaT", [D, T], F32)
    ybT = din(nc, "ybT", [D, T], F32)
    h1T = din(nc, "h1T", [D, T], F32)
    vec = din(nc, "vec", [128, 16, KD], F32)
    oT = dout(nc, "oT", [D, T], F32)
    P = Prog(nc)
    cx = Ctx(P, wslots=0)
    vs = P.sbuf([128, 16, KD], F32)
    r_vs = Res()
    P.dma("sp", vs[:], vec, writes=[r_vs])
    ya = P.sbuf([128, KD, 512], F32)
    yb = P.sbuf([128, KD, 512], F32)
    hb = P.sbuf([128, KD, 512], F32)
    rya = [Res() for _ in range(KD)]
    ryb = [Res() for _ in range(KD)]
    rh = [Res() for _ in range(KD)]

    def do_group(tok0, G):
        for buf, res, src in ((ya, rya, yaT), (yb, ryb, ybT), (hb, rh, h1T)):
            P.dma("sp", buf[:, :, 0:G], src[:, tok0:tok0 + G].rearrange("(k p) t -> p k t", p=128), writes=res)
        for c in range(KD):
            P.add("act", lambda e, c=c: e.mul(hb[:, c, 0:G], hb[:, c, 0:G], ALPHA), reads=[rh[c]], writes=[rh[c]])
            P.add("dve", lambda e, c=c: e.tensor_tensor(ya[:, c, 0:G], ya[:, c, 0:G], yb[:, c, 0:G], ALU.add), reads=[rya[c], ryb[c]], writes=[rya[c]])
            P.add("dve", lambda e, c=c: e.scalar_tensor_tensor(hb[:, c, 0:G], ya[:, c, 0:G], vs[:, V_G2, c:c + 1], hb[:, c, 0:G], ALU.mult, ALU.add),
                  reads=[rya[c], rh[c], r_vs], writes=[rh[c]])
        hsrc = lambda c: hb[:, c, 0:G]
        layer_norm_fm(cx, hsrc, lambda c: rh[c], G,
                      [(hsrc, lambda c: rh[c], lambda c: vs[:, V_LN + 2, c:c + 1], lambda c: vs[:, V_LN + 3, c:c + 1], [r_vs])])
        P.dma("sp", oT[:, tok0:tok0 + G].rearrange("(k p) t -> p k t", p=128), hb[:, :, 0:G], reads=rh)

    for tok0 in range(0, T, 512):
        do_group(tok0, min(512, T - tok0))
    P.emit()
    return nc


def build_Kln(groups, T):
    nc = new_nc()
    hT = din(nc, "hT", [D, T], F32)
    vec = din(nc, "vec", [128, 16, KD], F32)
    uT_out = dout(nc, "uT_out", [D, T], BF16)
    P = Prog(nc)
    cx = Ctx(P, wslots=0)
    vs = P.sbuf([128, 16, KD], F32)
    r_vs = Res()
    P.dma("sp", vs[:], vec, writes=[r_vs])
    for kind in range(2):
        j = kind * 6 + V_NSC1
        P.add("dve", lambda e, j=j: e.tensor_scalar_add(vs[:, j, :], vs[:, j, :], 1.0), reads=[r_vs], writes=[r_vs])
    hb = Rot(P, 2, [128, KD, 512], F32)
    ub = Rot(P, 2, [128, KD, 512], BF16)

    def do_group(tok0, G, kind):
        hbuf, rhb = hb.next()
        ubuf, rub = ub.next()
        P.dma("sp", hbuf[:, :, 0:G], hT[:, tok0:tok0 + G].rearrange("(k p) t -> p k t", p=128), writes=[rhb])
        layer_norm_fm(cx, lambda c: hbuf[:, c, 0:G], lambda c: rhb, G,
                      [(lambda c: ubuf[:, c, 0:G], lambda c: rub, lambda c: vs[:, kind * 6 + V_NSC1, c:c + 1],
                        lambda c: vs[:, kind * 6 + V_NSH1, c:c + 1], [r_vs])])
        P.dma("sp", uT_out[:, tok0:tok0 + G].rearrange("(k p) t -> p k t", p=128), ubuf[:, :, 0:G], reads=[rub])

    for (tok0, G, kind) in groups:
        do_group(tok0, G, kind)
    P.emit()
    return nc


MOD_COLS = 2 * 6 * D // NCORE


def build_Kmod():
    nc = new_nc()
    cT = din(nc, "cT", [D, 5], F32)
    wm = din(nc, "wm", [D, MOD_COLS], F32)
    bm = din(nc, "bm", [5, MOD_COLS], F32)
    out = dout(nc, "mod", [5, MOD_COLS], F32)
    P = Prog(nc)
    ps = Rot(P, 2, [128, 512], F32, psum=True)
    wt = Rot(P, 2, [128, KD, 512], F32)
    cs = P.sbuf([128, KD, 5], F32)
    r_cs = Res()
    P.dma("sp", cs[:], cT.rearrange("(k p) r -> p k r", p=128), writes=[r_cs])
    P.add("act", lambda e: e.activation(cs[:], cs[:], AF.Silu), reads=[r_cs], writes=[r_cs])
    bs = P.sbuf([5, MOD_COLS], F32)
    r_bs = Res()
    P.dma("sp", bs[:], bm, writes=[r_bs])
    ob = P.sbuf([5, MOD_COLS], F32)
    r_ob = Res()
    for n in range(MOD_COLS // 512):
        w, rw_ = wt.next()
        P.dma("sp" if n % 2 == 0 else "act", w[:], wm[:, n * 512:(n + 1) * 512].rearrange("(k p) c -> p k c", p=128), writes=[rw_])
        p, rp = ps.next()
        for k in range(KD):
            P.add("pe", lambda e, k=k, p=p, w=w: e.matmul(p[0:5, :], cs[:, k, :], w[:, k, :], start=(k == 0), stop=(k == KD - 1)),
                  reads=[r_cs, rw_], writes=[rp])
        P.add("dve", lambda e, n=n, p=p: e.tensor_tensor(ob[:, n * 512:(n + 1) * 512], p[0:5, :], bs[:, n * 512:(n + 1) * 512], ALU.add),
              reads=[rp, r_bs], writes=[r_ob])
    P.dma("sp", out, ob[:], reads=[r_ob])
    P.emit()
    return nc


TPB = CTX + SEQ
GELU_C = 0.044715
GELU_S = 1.5957691216057308


def batch_groups():
    return [(0, CTX)] + [(CTX + 512 * i, 512) for i in range(SEQ // 512)]


def build_Arg(nbatch, nc=None):
    NT = nbatch * TPB
    nc = nc or new_nc()
    uT = din(nc, "uT", [D, NT], BF16)
    wx = din(nc, "wx", [D, 256], F32)
    wg = din(nc, "wg", [D, 256], F32)
    cw = din(nc, "rcw", [128, 2, 4], F32)
    cb = din(nc, "rcb", [128, 2], F32)
    rgw = din(nc, "rgw", [128, 8, 128], F32)
    rgb = din(nc, "rgb", [128, 8], F32)
    lam = din(nc, "lam", [128, 4], F32)
    yrT = dout(nc, "yrT", [256, NT], BF16)
    P = Prog(nc)
    cx = Ctx(P, wslots=2, wcols=KD * 256, ntmp=8)
    twx, rwx = cx.wtile(wx, KD, 256)
    twg, rwg = cx.wtile(wg, KD, 256)
    small = {}
    r_small = Res()
    for name, src, shape in (("cw", cw, [128, 2, 4]), ("cb", cb, [128, 2]), ("rgw", rgw, [128, 8, 128]), ("rgb", rgb, [128, 8]), ("lam", lam, [128, 4])):
        small[name] = P.sbuf(shape, F32)
        P.dma("sp", small[name][:], src, writes=[r_small])
    clam = P.sbuf([128, 4], F32)
    clam2 = P.sbuf([128, 4], F32)
    r_cl = Res()
    P.add("act", lambda e: e.activation(clam[:], small["lam"][:], AF.Exp, scale=-1.0), reads=[r_small], writes=[r_cl])
    P.add("act", lambda e: e.activation(clam[:], clam[:], AF.Ln, bias=1.0), reads=[r_cl], writes=[r_cl])
    P.add("dve", lambda e: e.tensor_scalar_mul(clam2[:], clam[:], -16.0), reads=[r_cl], writes=[r_cl])
    P.add("dve", lambda e: e.tensor_scalar_mul(clam[:], clam[:], -8.0), reads=[r_cl], writes=[r_cl])

    ub = Rot(P, 2, [128, KD, 512], BF16)
    XR = [P.sbuf([128, TPB + 6], F32) for _ in range(2)]
    rxr = [Res(), Res()]
    XC = [P.sbuf([128, TPB], F32) for _ in range(2)]
    rxc = [Res(), Res()]
    GB = [P.sbuf([128, TPB], BF16) for _ in range(2)]
    rgbuf = [Res(), Res()]
    H = [P.sbuf([128, TPB + 1], F32) for _ in range(2)]
    rH = [Res(), Res()]
    for ch in range(2):
        P.add("dve", lambda e, ch=ch: e.memset(XR[ch][:], 0.0), writes=[rxr[ch]])
    A, B = XR[0], XR[1]
    segs = ((0, 0, CTX), (CTX + 3, CTX, SEQ))

    def posraw(tok):
        return tok + 2 if tok < CTX else tok + 5

    def do_batch(b):
        base = b * TPB
        if b > 0:
            for ch in range(2):
                for col in (0, CTX + 2, TPB + 5):
                    w = 2 if col != TPB + 5 else 1
                    if col == CTX + 2:
                        w = 3
                    P.add("dve", lambda e, ch=ch, col=col, w=w: e.memset(XR[ch][:, col:col + w], 0.0), writes=[rxr[ch]])
        for (tok0, G) in batch_groups():
            ubuf, rub = ub.next()
            P.dma("sp", ubuf[:, :, 0:G], uT[:, base + tok0:base + tok0 + G].rearrange("(k p) t -> p k t", p=128), writes=[rub])
            for ch in range(2):
                px, rpx = fm_mm(cx, twx, rwx, ch * 128, lambda k, ubuf=ubuf, G=G: ubuf[:, k, 0:G], lambda k, rub=rub: rub, G)
                P.add("act", lambda e, ch=ch, px=px, tok0=tok0, G=G: e.copy(XR[ch][:, posraw(tok0):posraw(tok0) + G], px[:, 0:G]),
                      reads=[rpx], writes=[rxr[ch]])
                pg, rpg = fm_mm(cx, twg, rwg, ch * 128, lambda k, ubuf=ubuf, G=G: ubuf[:, k, 0:G], lambda k, rub=rub: rub, G)
                t, rt = cx.tmp.next()
                P.add("act", lambda e, pg=pg, t=t, G=G: e.activation(t[:, 0:G], pg[:, 0:G], AF.Square), reads=[rpg], writes=[rt])
                P.add("dve", lambda e, t=t, G=G: e.tensor_scalar(t[:, 0:G], t[:, 0:G], GELU_C, 1.0, ALU.mult, ALU.add), reads=[rt], writes=[rt])
                P.add("dve", lambda e, pg=pg, t=t, G=G: e.tensor_tensor(t[:, 0:G], t[:, 0:G], pg[:, 0:G], ALU.mult), reads=[rt, rpg], writes=[rt])
                P.add("act", lambda e, t=t, G=G: e.activation(t[:, 0:G], t[:, 0:G], AF.Sigmoid, scale=GELU_S), reads=[rt], writes=[rt])
                P.add("dve", lambda e, ch=ch, pg=pg, t=t, tok0=tok0, G=G: e.tensor_tensor(GB[ch][:, tok0:tok0 + G], t[:, 0:G], pg[:, 0:G], ALU.mult),
                      reads=[rt, rpg], writes=[rgbuf[ch]])
        for ch in range(2):
            for (praw, ptok, L) in segs:
                for h0 in range(0, L, 2048):
                    hl = min(2048, L - h0)
                    o = XC[ch][:, ptok + h0:ptok + h0 + hl]
                    P.add("dve", lambda e, ch=ch, o=o, praw=praw, h0=h0, hl=hl: e.tensor_scalar(
                        o, XR[ch][:, praw + h0:praw + h0 + hl], small["cw"][:, ch, 0:1], small["cb"][:, ch:ch + 1], ALU.mult, ALU.add),
                        reads=[rxr[ch], r_small], writes=[rxc[ch]])
                    for j in range(1, 4):
                        P.add("dve", lambda e, ch=ch, o=o, praw=praw, h0=h0, hl=hl, j=j: e.scalar_tensor_tensor(
                            o, XR[ch][:, praw + h0 + j:praw + h0 + j + hl], small["cw"][:, ch, j:j + 1], o, ALU.mult, ALU.add),
                            reads=[rxr[ch], r_small, rxc[ch]], writes=[rxc[ch]])
        for ch in range(2):
            for d in range(2):
                ia = (d * 2 + ch) * 2
                for (tok0, G) in batch_groups():
                    sb_, sl = (0, CTX) if tok0 < CTX else (CTX, SEQ)
                    if d == 0:
                        oa = A[:, 1 + tok0:1 + tok0 + G]
                        obb = B[:, 1 + tok0:1 + tok0 + G]
                    else:
                        phi = sb_ + sl - 1 - (tok0 - sb_)
                        oa = A[:, 1 + phi:1 + phi - G:-1]
                        obb = B[:, 1 + phi:1 + phi - G:-1]
                    xcs = XC[ch][:, tok0:tok0 + G]
                    pr, rpr = cx.mm.next()
                    P.add("pe", lambda e, pr=pr, ia=ia, xcs=xcs, G=G: e.matmul(pr[:, 0:G], small["rgw"][:, ia, :], xcs, start=True, stop=True),
                          reads=[r_small, rxc[ch]], writes=[rpr])
                    pi, rpi = cx.mm.next()
                    P.add("pe", lambda e, pi=pi, ia=ia, xcs=xcs, G=G: e.matmul(pi[:, 0:G], small["rgw"][:, ia + 1, :], xcs, start=True, stop=True),
                          reads=[r_small, rxc[ch]], writes=[rpi])
                    r_, rr = cx.tmp.next()
                    s_, rs = cx.tmp.next()
                    i_, ri = cx.tmp.next()
                    P.add("act", lambda e, pr=pr, r_=r_, ia=ia, G=G: e.activation(r_[:, 0:G], pr[:, 0:G], AF.Sigmoid, bias=small["rgb"][:, ia:ia + 1]),
                          reads=[rpr, r_small], writes=[rr])
                    P.add("act", lambda e, r_=r_, oa=oa, d=d, ch=ch, G=G: e.activation(oa, r_[:, 0:G], AF.Exp, scale=clam[:, d * 2 + ch:d * 2 + ch + 1]),
                          reads=[rr, r_cl], writes=[rxr[0]])
                    P.add("act", lambda e, r_=r_, s_=s_, d=d, ch=ch, G=G: e.activation(s_[:, 0:G], r_[:, 0:G], AF.Exp, scale=clam2[:, d * 2 + ch:d * 2 + ch + 1]),
                          reads=[rr, r_cl], writes=[rs])
                    P.add("act", lambda e, s_=s_, G=G: e.activation(s_[:, 0:G], s_[:, 0:G], AF.Sqrt, bias=1.0, scale=-1.0), reads=[rs], writes=[rs])
                    P.add("act", lambda e, pi=pi, i_=i_, ia=ia, G=G: e.activation(i_[:, 0:G], pi[:, 0:G], AF.Sigmoid, bias=small["rgb"][:, ia + 1:ia + 2]),
                          reads=[rpi, r_small], writes=[ri])
                    P.add("dve", lambda e, s_=s_, i_=i_, G=G: e.tensor_tensor(s_[:, 0:G], s_[:, 0:G], i_[:, 0:G], ALU.mult), reads=[rs, ri], writes=[rs])
                    P.add("dve", lambda e, s_=s_, obb=obb, xcs=xcs, G=G: e.tensor_tensor(obb, s_[:, 0:G], xcs, ALU.mult), reads=[rs, rxc[ch]], writes=[rxr[1]])
                half = TPB // 2
                P.add("dve", lambda e, d=d: e.tensor_tensor_scan(H[d][:, 1:1 + half], A[:, 1:1 + half], B[:, 1:1 + half], 0.0, ALU.mult, ALU.add),
                      reads=[rxr[0], rxr[1]], writes=[rH[d]])
                P.add("dve", lambda e, d=d: e.tensor_tensor_scan(H[d][:, 1 + half:1 + TPB], A[:, 1 + half:1 + TPB], B[:, 1 + half:1 + TPB],
                                                                 H[d][:, half:half + 1], ALU.mult, ALU.add),
                      reads=[rxr[0], rxr[1], rH[d]], writes=[rH[d]])
            for (praw, ptok, L) in segs:
                P.add("dve", lambda e, ptok=ptok, L=L: e.tensor_tensor(H[0][:, 1 + ptok:1 + ptok + L], H[1][:, ptok + L:ptok:-1],
                                                                       H[0][:, 1 + ptok:1 + ptok + L], ALU.add),
                      reads=[rH[0], rH[1]], writes=[rH[0]])
            P.add("dve", lambda e, ch=ch: e.tensor_tensor(GB[ch][:], H[0][:, 1:1 + TPB], GB[ch][:], ALU.mult), reads=[rH[0], rgbuf[ch]], writes=[rgbuf[ch]])
            P.dma("sp", yrT[ch * 128:(ch + 1) * 128, base:base + TPB], GB[ch][:], reads=[rgbuf[ch]])

    for b in range(nbatch):
        do_batch(b)
    P.emit()
    return nc


AM2_POOL = "dve"
MG = 256
NCH = TPB // 128
DH = 256


def build_Am(nbatch, nc=None):
    NT = nbatch * TPB
    nc = nc or new_nc()
    uT = din(nc, "uTm", [D, NT], BF16)
    wq = din(nc, "wq", [D, 256], F32)
    wk = din(nc, "wk", [D, 256], F32)
    wvo = din(nc, "wvo", [D, 512], F32)
    wgt = din(nc, "wgt", [D, 4], F32)
    gb = din(nc, "gb", [128, 4], F32)
    cwd = din(nc, "cw", [128, 4, 4], F32)
    cbd = din(nc, "cb", [128, 4], F32)
    gn = din(nc, "gn", [128, 256], F32)
    ident = din(nc, "ident", [128, 128], BF16)
    tri = din(nc, "tri", [128, 2, 128], F32)
    negm = din(nc, "negm", [128, 2, 128], F32)
    ym = dout(nc, "ym", [NT, 256], BF16)
    P = Prog(nc)
    ps = Rot(P, 6, [128, 512], F32, psum=True)
    pt = Rot(P, 2, [128, 512], BF16, psum=True)
    tmp = Rot(P, 6, [128, MG + 3], F32)
    wqs = P.sbuf([128, KD, 256], BF16)
    wks = P.sbuf([128, KD, 256], BF16)
    wvos = P.sbuf([128, KD, 512], BF16)
    wgts = P.sbuf([128, KD, 4], BF16)
    r_w = Res()
    for dst, src in ((wqs, wq), (wks, wk), (wvos, wvo), (wgts, wgt)):
        P.dma("pool", dst[:], src.rearrange("(k p) c -> p k c", p=128), writes=[r_w])
    cst = {}
    r_c = Res()
    for name, src, shape, dt in (("gb", gb, [128, 4], F32), ("cw", cwd, [128, 4, 4], F32), ("cb", cbd, [128, 4], F32), ("gn", gn, [128, 256], F32),
                                 ("ident", ident, [128, 128], BF16), ("tri", tri, [128, 2, 128], F32), ("negm", negm, [128, 2, 128], F32)):
        cst[name] = P.sbuf(shape, dt)
        P.dma("sp", cst[name][:], src, writes=[r_c])
    ones = P.sbuf([128, 128], F32)
    P.add("dve", lambda e: e.memset(ones[:], 1.0), writes=[r_c])

    ub = Rot(P, 2, [128, KD, MG + 3], BF16)
    QK = [P.sbuf([128, TPB], BF16) for _ in range(4)]
    rqk = [[Res() for _ in range(NCH // 2)] for _ in range(4)]
    VA = P.sbuf([128, NCH, DH + 1], BF16)
    rva = [Res() for _ in range(NCH)]
    SO = P.sbuf([128, NCH, DH], BF16)
    rso = [Res() for _ in range(NCH)]
    GT = P.sbuf([128, NCH, 4], F32)
    rgt = Res()
    HF = P.sbuf([128, NCH, DH], F32)
    rhf = [Res() for _ in range(NCH)]
    C32 = [P.sbuf([128, DH + 1], F32) for _ in range(2)]
    CB = [P.sbuf([128, DH + 1], BF16) for _ in range(2)]
    rc32 = [Res(), Res()]
    rcb = [Res(), Res()]
    t128 = Rot(P, 8, [128, 128], F32)
    b128 = Rot(P, 8, [128, 128], BF16)
    kwr = Rot(P, 2, [128, DH], BF16)
    vsm = Rot(P, 16, [128, 8], F32)
    hsr = Rot(P, 3, [128, DH], F32)
    obr = Rot(P, 3, [128, DH], BF16)
    P.add("dve", lambda e: e.memset(VA[:, :, DH:DH + 1], 1.0), writes=rva)
    QSCALE = DH ** -0.5

    def do_group(base, g, t0, first, last):
        ubuf, rub = ub.next()
        hl = 0 if first else 2
        hr = 0 if last else 1
        if first:
            P.add("dve", lambda e: e.memset(ubuf[:, :, 0:2], 0.0), writes=[rub])
        if last:
            P.add("dve", lambda e: e.memset(ubuf[:, :, MG + 2:MG + 3], 0.0), writes=[rub])
        P.dma("sp", ubuf[:, :, 2 - hl:2 + MG + hr],
              uT[:, base + t0 - hl:base + t0 + MG + hr].rearrange("(k p) t -> p k t", p=128), writes=[rub])
        for ci in range(4):
            wsb = wqs if ci < 2 else wks
            c0 = (ci % 2) * 128
            pb, rp = ps.next()
            for k in range(KD):
                P.add("pe", lambda e, k=k, pb=pb, wsb=wsb, c0=c0: e.matmul(pb[:, 0:MG + 3], wsb[:, k, c0:c0 + 128], ubuf[:, k, :], start=(k == 0), stop=(k == KD - 1)),
                      reads=[r_w, rub], writes=[rp])
            cv, rcv = tmp.next()
            sg, rsg = tmp.next()
            P.add("dve", lambda e, pb=pb, cv=cv, ci=ci: e.tensor_scalar(cv[:, 0:MG], pb[:, 0:MG], cst["cw"][:, ci, 0:1], cst["cb"][:, ci:ci + 1], ALU.mult, ALU.add),
                  reads=[rp, r_c], writes=[rcv])
            for j in range(1, 4):
                P.add("dve", lambda e, pb=pb, cv=cv, ci=ci, j=j: e.scalar_tensor_tensor(cv[:, 0:MG], pb[:, j:j + MG], cst["cw"][:, ci, j:j + 1], cv[:, 0:MG], ALU.mult, ALU.add),
                      reads=[rp, r_c, rcv], writes=[rcv])
            P.add("act", lambda e, cv=cv, sg=sg: e.activation(sg[:, 0:MG], cv[:, 0:MG], AF.Sigmoid), reads=[rcv], writes=[rsg])
            sc = QSCALE if ci < 2 else 1.0
            P.add("dve", lambda e, cv=cv, sg=sg, ci=ci, sc=sc: e.scalar_tensor_tensor(QK[ci][:, t0:t0 + MG], cv[:, 0:MG], sc, sg[:, 0:MG], ALU.mult, ALU.mult),
                  reads=[rcv, rsg], writes=[rqk[ci][g]])
        for i in range(2):
            c = 2 * g + i
            pv, rpv = ps.next()
            pg, rpg = ps.next()
            for k in range(KD):
                P.add("pe", lambda e, k=k, pv=pv, i=i: e.matmul(pv[:, 0:512], ubuf[:, k, 2 + i * 128:2 + (i + 1) * 128], wvos[:, k, :], start=(k == 0), stop=(k == KD - 1)),
                      reads=[r_w, rub], writes=[rpv])
            for k in range(KD):
                P.add("pe", lambda e, k=k, pg=pg, i=i: e.matmul(pg[:, 0:4], ubuf[:, k, 2 + i * 128:2 + (i + 1) * 128], wgts[:, k, :], start=(k == 0), stop=(k == KD - 1)),
                      reads=[r_w, rub], writes=[rpg])
            P.add("act", lambda e, pv=pv, c=c: e.copy(VA[:, c, 0:DH], pv[:, 0:DH]), reads=[rpv], writes=[rva[c]])
            P.add("act", lambda e, pv=pv, c=c: e.activation(SO[:, c, :], pv[:, DH:2 * DH], AF.Sigmoid), reads=[rpv], writes=[rso[c]])
            P.add("dve", lambda e, pg=pg, c=c: e.tensor_tensor(GT[:, c, :], pg[:, 0:4], cst["gb"][:], ALU.add), reads=[rpg, r_c], writes=[rgt])

    def do_chunk(base, c, d, finalize):
        cs = slice(c * 128, (c + 1) * 128)
        g = c // 2
        ig = GT[:, c, 2 * d:2 * d + 1]
        lf = GT[:, c, 2 * d + 1:2 * d + 2]
        lastcol = 127 if d == 0 else 0
        LU, rLU = t128.next()
        P.add("dve", lambda e: e.tensor_scalar(LU[:], cst["tri"][:, d, :], lf, None, ALU.mult), reads=[r_c, rgt], writes=[rLU])
        prow, rprow = ps.next()
        P.add("pe", lambda e: e.matmul(prow[:, 0:128], ones[:], LU[:], start=True, stop=True), reads=[r_c, rLU], writes=[rprow])
        pcol, rpcol = ps.next()
        P.add("pe", lambda e: e.matmul(pcol[:, 0:1], cst["tri"][:, d, :], lf, start=True, stop=True), reads=[r_c, rgt], writes=[rpcol])
        sv, rsv = vsm.next()
        P.add("dve", lambda e: e.tensor_tensor(sv[:, 0:1], ig, pcol[:, 0:1], ALU.subtract), reads=[rgt, rpcol], writes=[rsv])
        P.add("dve", lambda e: e.tensor_copy(sv[:, 1:2], prow[:, lastcol:lastcol + 1]), reads=[rprow], writes=[rsv])
        DT, rDT = t128.next()
        P.add("dve", lambda e: e.tensor_tensor(DT[:], prow[:, 0:128], cst["negm"][:, d, :], ALU.add), reads=[rprow, r_c], writes=[rDT])
        P.add("act", lambda e: e.activation(DT[:], DT[:], AF.Exp, bias=sv[:, 0:1]), reads=[rDT, rsv], writes=[rDT])
        EB, rEB = t128.next()
        P.add("act", lambda e: e.activation(EB[:], prow[:, 0:128], AF.Exp), reads=[rprow], writes=[rEB])
        QS = []
        for i in range(2):
            qs, rqs = b128.next()
            P.add("dve", lambda e, i=i, qs=qs: e.tensor_tensor(qs[:], QK[i][:, cs], EB[:], ALU.mult), reads=[rqk[i][g], rEB], writes=[rqs])
            QS.append((qs, rqs))
        pst, rpst = ps.next()
        for i in range(2):
            P.add("pe", lambda e, i=i: e.matmul(pst[:, 0:128], QK[2 + i][:, cs], QK[i][:, cs], start=(i == 0), stop=(i == 1)),
                  reads=[rqk[2 + i][g], rqk[i][g]], writes=[rpst])
        ST, rST = b128.next()
        P.add("dve", lambda e: e.tensor_tensor(ST[:], pst[:, 0:128], DT[:], ALU.mult), reads=[rpst, rDT], writes=[rST])
        pnum, rpnum = ps.next()
        P.add("pe", lambda e: e.matmul(pnum[:, 0:DH + 1], ST[:], VA[:, c, :], start=True, stop=False), reads=[rST, rva[c]], writes=[rpnum])
        for i in range(2):
            P.add("pe", lambda e, i=i: e.matmul(pnum[:, 0:DH + 1], QS[i][0][:], CB[i][:], start=False, stop=(i == 1)),
                  reads=[QS[i][1], rcb[i]], writes=[rpnum])
        P.add("act", lambda e: e.activation(sv[:, 4:5], pnum[:, DH:DH + 1], AF.Abs), reads=[rpnum], writes=[rsv])
        P.add("dve", lambda e: e.tensor_scalar_max(sv[:, 4:5], sv[:, 4:5], 1.0), reads=[rsv], writes=[rsv])
        P.add("dve", lambda e: e.reciprocal(sv[:, 5:6], sv[:, 4:5]), reads=[rsv], writes=[rsv])
        if not finalize:
            P.add("act", lambda e: e.activation(HF[:, c, :], pnum[:, 0:DH], AF.Copy, scale=sv[:, 5:6]), reads=[rpnum, rsv], writes=[rhf[c]])
        else:
            hs, rhs = hsr.next()
            P.add("dve", lambda e: e.scalar_tensor_tensor(hs[:], pnum[:, 0:DH], sv[:, 5:6], HF[:, c, :], ALU.mult, ALU.add), reads=[rpnum, rsv, rhf[c]], writes=[rhs])
            st6, rst6 = vsm.next()
            P.add("dve", lambda e: e.bn_stats(st6[:, 0:6], hs[:]), reads=[rhs], writes=[rst6])
            mv, rmv = vsm.next()
            P.add("dve", lambda e: e.bn_aggr(mv[:, 0:2], st6[:, 0:6]), reads=[rst6], writes=[rmv])
            P.add("act", lambda e: e.activation(mv[:, 2:3], mv[:, 1:2], AF.Sqrt, bias=LN_EPS, scale=1.0), reads=[rmv], writes=[rmv])
            P.add("dve", lambda e: e.reciprocal(mv[:, 2:3], mv[:, 2:3]), reads=[rmv], writes=[rmv])
            P.add("dve", lambda e: e.tensor_scalar(hs[:], hs[:], mv[:, 0:1], mv[:, 2:3], ALU.subtract, ALU.mult), reads=[rhs, rmv], writes=[rhs])
            P.add("dve", lambda e: e.tensor_tensor(hs[:], hs[:], cst["gn"][:], ALU.mult), reads=[rhs, r_c], writes=[rhs])
            ob, rob = obr.next()
            P.add("dve", lambda e: e.tensor_tensor(ob[:], hs[:], SO[:, c, :], ALU.mult), reads=[rhs, rso[c]], writes=[rob])
            P.dma("sp", ym[base + c * 128:base + (c + 1) * 128, :], ob[:], reads=[rob])
        P.add("act", lambda e: e.activation(sv[:, 2:3], sv[:, 0:1], AF.Exp, bias=sv[:, 1:2]), reads=[rsv], writes=[rsv])
        P.add("act", lambda e: e.activation(sv[:, 3:4], sv[:, 1:2], AF.Exp), reads=[rsv], writes=[rsv])
        kw, rkw = kwr.next()
        for i in range(2):
            ptb, rpt = pt.next()
            P.add("pe", lambda e, i=i, ptb=ptb: e.transpose(ptb[:, 0:128], QK[2 + i][:, cs], cst["ident"][:]), reads=[rqk[2 + i][g], r_c], writes=[rpt])
            P.add("dve", lambda e, i=i, ptb=ptb: e.tensor_scalar(kw[:, i * 128:(i + 1) * 128], ptb[:, 0:128], sv[:, 2:3], None, ALU.mult), reads=[rpt, rsv], writes=[rkw])
        pcus = [ps.next(), ps.next()]
        for i in range(2):
            P.add("pe", lambda e, i=i: e.matmul(pcus[i][0][:, 0:DH + 1], kw[:, i * 128:(i + 1) * 128], VA[:, c, :], start=True, stop=True),
                  reads=[rkw, rva[c]], writes=[pcus[i][1]])
        for i in range(2):
            P.add("dve", lambda e, i=i: e.scalar_tensor_tensor(C32[i][:], C32[i][:], sv[:, 3:4], pcus[i][0][:, 0:DH + 1], ALU.mult, ALU.add),
                  reads=[rc32[i], rsv, pcus[i][1]], writes=[rc32[i]])
            P.add("act", lambda e, i=i: e.copy(CB[i][:], C32[i][:]), reads=[rc32[i]], writes=[rcb[i]])

    def zero_state():
        for i in range(2):
            P.add("dve", lambda e, i=i: e.memset(C32[i][:], 0.0), writes=[rc32[i]])
            P.add("dve", lambda e, i=i: e.memset(CB[i][:], 0.0), writes=[rcb[i]])

    def do_batch(b):
        base = b * TPB
        ngrp = TPB // MG
        for g in range(ngrp):
            first = g in (0, 1)
            last = g in (0, ngrp - 1)
            do_group(base, g, g * MG, first, last)
        for col in (1, 3):
            P.add("act", lambda e, col=col: e.activation(GT[:, :, col], GT[:, :, col], AF.Exp, scale=-1.0), reads=[rgt], writes=[rgt])
            P.add("act", lambda e, col=col: e.activation(GT[:, :, col], GT[:, :, col], AF.Ln, bias=1.0), reads=[rgt], writes=[rgt])
            P.add("dve", lambda e, col=col: e.tensor_scalar_mul(GT[:, :, col], GT[:, :, col], -1.0), reads=[rgt], writes=[rgt])
        zero_state()
        for c in range(NCH):
            do_chunk(base, c, 0, False)
        zero_state()
        for c in [1, 0] + list(range(NCH - 1, 1, -1)):
            do_chunk(base, c, 1, True)

    for b in range(nbatch):
        do_batch(b)
    P.emit()
    return nc


def build_Am2(nbatch, nc=None):
    NT = nbatch * TPB
    nc = nc or new_nc()
    uT = din(nc, "uTm", [D, NT], BF16)
    wq = din(nc, "wq", [D, 256], F32)
    wk = din(nc, "wk", [D, 256], F32)
    wvo = din(nc, "wvo", [D, 512], F32)
    wgt = din(nc, "wgt", [D, 4], F32)
    gb = din(nc, "gb", [128, 4], F32)
    cwd = din(nc, "cw", [128, 4, 4], F32)
    cbd = din(nc, "cb", [128, 4], F32)
    gn = din(nc, "gn", [128, 256], F32)
    ident = din(nc, "ident", [128, 128], BF16)
    tri = din(nc, "tri", [128, 2, 128], F32)
    negm = din(nc, "negm", [128, 2, 128], F32)
    ym = dout(nc, "ym", [NT, 256], BF16)
    P = Prog(nc)
    banks = [P.psum([128, 512], F32) for _ in range(7)]
    rbk = [Res() for _ in range(7)]
    tbank = P.psum([128, 1024], BF16)
    rtb = Res()
    ps = Rot(P, 0, None, None)
    ps.bufs, ps.res, ps.n = banks[0:6], rbk[0:6], 6
    tmp = Rot(P, 6, [128, MG + 3], F32)
    wqs = P.sbuf([128, KD, 256], BF16)
    wks = P.sbuf([128, KD, 256], BF16)
    wvos = P.sbuf([128, KD, 512], BF16)
    wgts = P.sbuf([128, KD, 4], BF16)
    r_w = Res()
    for dst, src in ((wqs, wq), (wks, wk), (wvos, wvo), (wgts, wgt)):
        P.dma("pool", dst[:], src.rearrange("(k p) c -> p k c", p=128), writes=[r_w])
    cst = {}
    r_c = Res()
    for name, src, shape, dt in (("gb", gb, [128, 4], F32), ("cw", cwd, [128, 4, 4], F32), ("cb", cbd, [128, 4], F32), ("gn", gn, [128, 256], F32),
                                 ("ident", ident, [128, 128], BF16), ("tri", tri, [128, 2, 128], F32), ("negm", negm, [128, 2, 128], F32)):
        cst[name] = P.sbuf(shape, dt)
        P.dma("sp", cst[name][:], src, writes=[r_c])
    ones = P.sbuf([128, 128], F32)
    P.add("dve", lambda e: e.memset(ones[:], 1.0), writes=[r_c])

    ub = Rot(P, 2, [128, KD, MG + 3], BF16)
    QK = [P.sbuf([128, TPB], BF16) for _ in range(4)]
    rqk = [[Res() for _ in range(NCH // 2)] for _ in range(4)]
    VA = P.sbuf([128, NCH, DH + 1], BF16)
    rva = [Res() for _ in range(NCH)]
    SO = P.sbuf([128, NCH, DH], BF16)
    rso = [Res() for _ in range(NCH)]
    GT = P.sbuf([128, NCH, 4], F32)
    rgt = Res()
    HF = P.sbuf([128, NCH, DH], F32)
    rhf = [Res() for _ in range(NCH)]
    C32 = [P.sbuf([128, DH + 1], F32) for _ in range(2)]
    rc32 = [Res(), Res()]
    NS = 8
    LUa = P.sbuf([128, NS, 128], F32)
    DTa = P.sbuf([128, NS, 128], F32)
    EBa = P.sbuf([128, NS, 128], F32)
    QSa = P.sbuf([128, NS, 2, 128], BF16)
    STa = P.sbuf([128, NS, 128], BF16)
    KWa = P.sbuf([128, NS, DH], BF16)
    SVa = P.sbuf([128, NS, 8], F32)
    rLU = [Res() for _ in range(NS)]
    rDT = [Res() for _ in range(NS)]
    rEB = [Res() for _ in range(NS)]
    rQS = [Res() for _ in range(NS)]
    rST = [Res() for _ in range(NS)]
    rKW = [Res() for _ in range(NS)]
    rSV = [Res() for _ in range(NS)]
    CUs = Rot(P, 4, [128, 2, DH + 1], F32)
    CBr = [P.sbuf([128, 2, DH + 16], BF16) for _ in range(4)]
    rCBr = [Res() for _ in range(4)]
    vsm = Rot(P, 16, [128, 8], F32)
    hsr = Rot(P, 3, [128, DH], F32)
    obr = Rot(P, 3, [128, DH], BF16)
    P.add("dve", lambda e: e.memset(VA[:, :, DH:DH + 1], 1.0), writes=rva)
    QSCALE = DH ** -0.5

    def do_group(base, g, t0, first, last):
        ubuf, rub = ub.next()
        hl = 0 if first else 2
        hr = 0 if last else 1
        if first:
            P.add("dve", lambda e: e.memset(ubuf[:, :, 0:2], 0.0), writes=[rub])
        if last:
            P.add("dve", lambda e: e.memset(ubuf[:, :, MG + 2:MG + 3], 0.0), writes=[rub])
        P.dma("sp", ubuf[:, :, 2 - hl:2 + MG + hr],
              uT[:, base + t0 - hl:base + t0 + MG + hr].rearrange("(k p) t -> p k t", p=128), writes=[rub])
        for ci in range(4):
            wsb = wqs if ci < 2 else wks
            c0 = (ci % 2) * 128
            pb, rp = ps.next()
            for k in range(KD):
                P.add("pe", lambda e, k=k, pb=pb, wsb=wsb, c0=c0: e.matmul(pb[:, 0:MG + 3], wsb[:, k, c0:c0 + 128], ubuf[:, k, :], start=(k == 0), stop=(k == KD - 1)),
                      reads=[r_w, rub], writes=[rp])
            cv, rcv = tmp.next()
            sg, rsg = tmp.next()
            P.add("dve", lambda e, pb=pb, cv=cv, ci=ci: e.tensor_scalar(cv[:, 0:MG], pb[:, 0:MG], cst["cw"][:, ci, 0:1], cst["cb"][:, ci:ci + 1], ALU.mult, ALU.add),
                  reads=[rp, r_c], writes=[rcv])
            for j in range(1, 4):
                P.add("dve", lambda e, pb=pb, cv=cv, ci=ci, j=j: e.scalar_tensor_tensor(cv[:, 0:MG], pb[:, j:j + MG], cst["cw"][:, ci, j:j + 1], cv[:, 0:MG], ALU.mult, ALU.add),
                      reads=[rp, r_c, rcv], writes=[rcv])
            P.add("act", lambda e, cv=cv, sg=sg: e.activation(sg[:, 0:MG], cv[:, 0:MG], AF.Sigmoid), reads=[rcv], writes=[rsg])
            sc = QSCALE if ci < 2 else 1.0
            P.add("dve", lambda e, cv=cv, sg=sg, ci=ci, sc=sc: e.scalar_tensor_tensor(QK[ci][:, t0:t0 + MG], cv[:, 0:MG], sc, sg[:, 0:MG], ALU.mult, ALU.mult),
                  reads=[rcv, rsg], writes=[rqk[ci][g]])
        for i in range(2):
            c = 2 * g + i
            pv, rpv = ps.next()
            pg, rpg = ps.next()
            for k in range(KD):
                P.add("pe", lambda e, k=k, pv=pv, i=i: e.matmul(pv[:, 0:512], ubuf[:, k, 2 + i * 128:2 + (i + 1) * 128], wvos[:, k, :], start=(k == 0), stop=(k == KD - 1)),
                      reads=[r_w, rub], writes=[rpv])
            for k in range(KD):
                P.add("pe", lambda e, k=k, pg=pg, i=i: e.matmul(pg[:, 0:4], ubuf[:, k, 2 + i * 128:2 + (i + 1) * 128], wgts[:, k, :], start=(k == 0), stop=(k == KD - 1)),
                      reads=[r_w, rub], writes=[rpg])
            P.add("act", lambda e, pv=pv, c=c: e.copy(VA[:, c, 0:DH], pv[:, 0:DH]), reads=[rpv], writes=[rva[c]])
            P.add("act", lambda e, pv=pv, c=c: e.activation(SO[:, c, :], pv[:, DH:2 * DH], AF.Sigmoid), reads=[rpv], writes=[rso[c]])
            P.add("dve", lambda e, pg=pg, c=c: e.tensor_tensor(GT[:, c, :], pg[:, 0:4], cst["gb"][:], ALU.add), reads=[rpg, r_c], writes=[rgt])

    def do_block(base, chunks, d, finalize, par, n0):
        lastcol = 127 if d == 0 else 0
        sl = [par * 4 + i for i in range(len(chunks))]
        for i, c in enumerate(chunks):
            P.add(AM2_POOL, lambda e, i=i, c=c: e.tensor_scalar(LUa[:, sl[i], :], cst["tri"][:, d, :], GT[:, c, 2 * d + 1:2 * d + 2], None, ALU.mult),
                  reads=[r_c, rgt], writes=[rLU[sl[i]]])
        for i, c in enumerate(chunks):
            P.add("pe", lambda e, i=i: e.matmul(banks[0][:, i * 128:(i + 1) * 128], ones[:], LUa[:, sl[i], :], start=True, stop=True),
                  reads=[r_c, rLU[sl[i]]], writes=[rbk[0]])
            P.add("pe", lambda e, i=i, c=c: e.matmul(banks[2][:, 16 * i:16 * i + 1], cst["tri"][:, d, :], GT[:, c, 2 * d + 1:2 * d + 2], start=True, stop=True),
                  reads=[r_c, rgt], writes=[rbk[2]])
        for i, c in enumerate(chunks):
            sv = SVa[:, sl[i], :]
            P.add("dve", lambda e, i=i, c=c, sv=sv: e.tensor_tensor(sv[:, 0:1], GT[:, c, 2 * d:2 * d + 1], banks[2][:, 16 * i:16 * i + 1], ALU.subtract),
                  reads=[rgt, rbk[2]], writes=[rSV[sl[i]]])
            P.add("dve", lambda e, i=i, sv=sv: e.tensor_copy(sv[:, 1:2], banks[0][:, i * 128 + lastcol:i * 128 + lastcol + 1]), reads=[rbk[0]], writes=[rSV[sl[i]]])
            P.add("dve", lambda e, i=i: e.tensor_tensor(DTa[:, sl[i], :], banks[0][:, i * 128:(i + 1) * 128], cst["negm"][:, d, :], ALU.add),
                  reads=[rbk[0], r_c], writes=[rDT[sl[i]]])
        for i, c in enumerate(chunks):
            sv = SVa[:, sl[i], :]
            P.add("act", lambda e, i=i, sv=sv: e.activation(DTa[:, sl[i], :], DTa[:, sl[i], :], AF.Exp, bias=sv[:, 0:1]), reads=[rDT[sl[i]], rSV[sl[i]]], writes=[rDT[sl[i]]])
            P.add("act", lambda e, i=i: e.activation(EBa[:, sl[i], :], banks[0][:, i * 128:(i + 1) * 128], AF.Exp), reads=[rbk[0]], writes=[rEB[sl[i]]])
            P.add("act", lambda e, sv=sv: e.activation(sv[:, 2:3], sv[:, 0:1], AF.Exp, bias=sv[:, 1:2]), reads=[rSV[sl[i]]], writes=[rSV[sl[i]]])
            P.add("act", lambda e, sv=sv: e.activation(sv[:, 3:4], sv[:, 1:2], AF.Exp), reads=[rSV[sl[i]]], writes=[rSV[sl[i]]])
        for i, c in enumerate(chunks):
            cs = slice(c * 128, (c + 1) * 128)
            for h in range(2):
                P.add(AM2_POOL, lambda e, i=i, h=h, cs=cs: e.tensor_tensor(QSa[:, sl[i], h, :], QK[h][:, cs], EBa[:, sl[i], :], ALU.mult),
                      reads=[rqk[h][c // 2], rEB[sl[i]]], writes=[rQS[sl[i]]])
        for i, c in enumerate(chunks):
            cs = slice(c * 128, (c + 1) * 128)
            for h in range(2):
                P.add("pe", lambda e, i=i, h=h, cs=cs: e.matmul(banks[1][:, i * 128:(i + 1) * 128], QK[2 + h][:, cs], QK[h][:, cs], start=(h == 0), stop=(h == 1)),
                      reads=[rqk[2 + h][c // 2], rqk[h][c // 2]], writes=[rbk[1]])
            for h in range(2):
                P.add("pe", lambda e, i=i, h=h, cs=cs: e.transpose(tbank[:, (2 * i + h) * 128:(2 * i + h + 1) * 128], QK[2 + h][:, cs], cst["ident"][:]),
                      reads=[rqk[2 + h][c // 2], r_c], writes=[rtb])
        for i, c in enumerate(chunks):
            sv = SVa[:, sl[i], :]
            P.add("dve", lambda e, i=i: e.tensor_tensor(STa[:, sl[i], :], banks[1][:, i * 128:(i + 1) * 128], DTa[:, sl[i], :], ALU.mult),
                  reads=[rbk[1], rDT[sl[i]]], writes=[rST[sl[i]]])
            for h in range(2):
                P.add("dve", lambda e, i=i, h=h, sv=sv: e.tensor_scalar(KWa[:, sl[i], h * 128:(h + 1) * 128], tbank[:, (2 * i + h) * 128:(2 * i + h + 1) * 128], sv[:, 2:3], None, ALU.mult),
                      reads=[rtb, rSV[sl[i]]], writes=[rKW[sl[i]]])
        for i, c in enumerate(chunks):
            n = n0 + i
            sv = SVa[:, sl[i], :]
            cu, rcu = CUs.next()
            for h in range(2):
                P.add("pe", lambda e, i=i, h=h, c=c: e.matmul(banks[5 + h][:, 0:DH + 1], KWa[:, sl[i], h * 128:(h + 1) * 128], VA[:, c, :], start=True, stop=True),
                      reads=[rKW[sl[i]], rva[c]], writes=[rbk[5 + h]])
            pn = banks[3 + (n % 2)]
            rpn = rbk[3 + (n % 2)]
            P.add("pe", lambda e, i=i, c=c, pn=pn: e.matmul(pn[:, 0:DH + 1], STa[:, sl[i], :], VA[:, c, :], start=True, stop=False), reads=[rST[sl[i]], rva[c]], writes=[rpn])
            for h in range(2):
                P.add("pe", lambda e, i=i, h=h, pn=pn, n=n: e.matmul(pn[:, 0:DH + 1], QSa[:, sl[i], h, :], CBr[n % 4][:, h, 0:DH + 1], start=False, stop=(h == 1)),
                      reads=[rQS[sl[i]], rCBr[n % 4]], writes=[rpn])
            for h in range(2):
                P.add("act", lambda e, h=h, cu=cu: e.copy(cu[:, h, :], banks[5 + h][:, 0:DH + 1]), reads=[rbk[5 + h]], writes=[rcu])
            for h in range(2):
                P.add("dve", lambda e, h=h, cu=cu, sv=sv: e.scalar_tensor_tensor(C32[h][:], C32[h][:], sv[:, 3:4], cu[:, h, :], ALU.mult, ALU.add),
                      reads=[rc32[h], rSV[sl[i]], rcu], writes=[rc32[h]])
                P.add("act", lambda e, h=h, n=n: e.copy(CBr[(n + 1) % 4][:, h, 0:DH + 1], C32[h][:]), reads=[rc32[h]], writes=[rCBr[(n + 1) % 4]])
            dn, rdn = vsm.next()
            P.add("act", lambda e, pn=pn, dn=dn: e.activation(dn[:, 0:1], pn[:, DH:DH + 1], AF.Abs), reads=[rpn], writes=[rdn])
            P.add("dve", lambda e, dn=dn: e.tensor_scalar_max(dn[:, 0:1], dn[:, 0:1], 1.0), reads=[rdn], writes=[rdn])
            P.add("dve", lambda e, dn=dn: e.reciprocal(dn[:, 1:2], dn[:, 0:1]), reads=[rdn], writes=[rdn])
            if not finalize:
                P.add("act", lambda e, pn=pn, dn=dn, c=c: e.activation(HF[:, c, :], pn[:, 0:DH], AF.Copy, scale=dn[:, 1:2]), reads=[rpn, rdn], writes=[rhf[c]])
            else:
                hs, rhs = hsr.next()
                P.add("dve", lambda e, pn=pn, dn=dn, c=c, hs=hs: e.scalar_tensor_tensor(hs[:], pn[:, 0:DH], dn[:, 1:2], HF[:, c, :], ALU.mult, ALU.add),
                      reads=[rpn, rdn, rhf[c]], writes=[rhs])
                P.add("dve", lambda e, dn=dn, hs=hs: e.bn_stats(dn[:, 2:8], hs[:]), reads=[rhs], writes=[rdn])
                mv, rmv = vsm.next()
                P.add("dve", lambda e, dn=dn, mv=mv: e.bn_aggr(mv[:, 0:2], dn[:, 2:8]), reads=[rdn], writes=[rmv])
                P.add("act", lambda e, mv=mv: e.activation(mv[:, 2:3], mv[:, 1:2], AF.Sqrt, bias=LN_EPS, scale=1.0), reads=[rmv], writes=[rmv])
                P.add("dve", lambda e, mv=mv: e.reciprocal(mv[:, 2:3], mv[:, 2:3]), reads=[rmv], writes=[rmv])
                P.add("dve", lambda e, mv=mv, hs=hs: e.tensor_scalar(hs[:], hs[:], mv[:, 0:1], mv[:, 2:3], ALU.subtract, ALU.mult), reads=[rhs, rmv], writes=[rhs])
                P.add(AM2_POOL, lambda e, hs=hs: e.tensor_tensor(hs[:], hs[:], cst["gn"][:], ALU.mult), reads=[rhs, r_c], writes=[rhs])
                ob, rob = obr.next()
                P.add(AM2_POOL, lambda e, hs=hs, ob=ob, c=c: e.tensor_tensor(ob[:], hs[:], SO[:, c, :], ALU.mult), reads=[rhs, rso[c]], writes=[rob])
                P.dma("sp", ym[base + c * 128:base + (c + 1) * 128, :], ob[:], reads=[rob])

    def zero_state():
        for h in range(2):
            P.add("dve", lambda e, h=h: e.memset(C32[h][:], 0.0), writes=[rc32[h]])
        P.add("dve", lambda e: e.memset(CBr[0][:], 0.0), writes=[rCBr[0]])

    blk_counter = [0]

    def do_dir(base, order, d, finalize):
        zero_state()
        for s0 in range(0, len(order), 4):
            do_block(base, order[s0:s0 + 4], d, finalize, blk_counter[0] % 2, s0)
            blk_counter[0] += 1

    def do_batch(b):
        base = b * TPB
        ngrp = TPB // MG
        for g in range(ngrp):
            first = g in (0, 1)
            last = g in (0, ngrp - 1)
            do_group(base, g, g * MG, first, last)
        for col in (1, 3):
            P.add("act", lambda e, col=col: e.activation(GT[:, :, col], GT[:, :, col], AF.Exp, scale=-1.0), reads=[rgt], writes=[rgt])
            P.add("act", lambda e, col=col: e.activation(GT[:, :, col], GT[:, :, col], AF.Ln, bias=1.0), reads=[rgt], writes=[rgt])
            P.add("dve", lambda e, col=col: e.tensor_scalar_mul(GT[:, :, col], GT[:, :, col], -1.0), reads=[rgt], writes=[rgt])
        do_dir(base, list(range(NCH)), 0, False)
        do_dir(base, [1, 0] + list(range(NCH - 1, 1, -1)), 1, True)

    for b in range(nbatch):
        do_batch(b)
    P.emit()
    return nc


def build_A(nbatch):
    nc = new_nc()
    build_Arg(nbatch, nc)
    nc.all_engine_barrier()
    build_Am(nbatch, nc)
    return nc


_PROGS = {}


def _prog(key, builder):
    if key not in _PROGS:
        _PROGS[key] = builder()
    return _PROGS[key]


def _run(nc, in_maps, tag=""):
    r = run_bass_kernel_spmd(nc, in_maps, core_ids=list(range(NCORE)))
    if getattr(r, "exec_time_ns", None):
        print(f"[kernel] launch {tag} exec_time_ns={r.exec_time_ns}", flush=True)
    return r.results


def _vec_pack(vecs):
    return np.ascontiguousarray(np.asarray(vecs, np.float32).reshape(16, KD, 128).transpose(2, 0, 1))


def _cm_perm():
    t = np.arange(SEQ)
    return (t % GRID) * GRID + (t // GRID)


def _core_tokens(b, half, with_ctx):
    lat = CTX + np.arange(half * (SEQ // 2), (half + 1) * (SEQ // 2))
    if with_ctx:
        return np.concatenate([np.arange(half * (CTX // 2), (half + 1) * (CTX // 2)), lat])
    return lat


def kernel(x, c, ctx, c_ctx, w_mod, b_mod, w_in, conv_rg_w, conv_rg_b, conv_m_w, conv_m_b, rg_wa, rg_ba,
           rg_wx, rg_bx, rg_lam, m_gate_b, m_gn_g, p_rg, p_m, w_out, ln_g, ln_b, ff_w1, ff_w3, ff_w2,
           router_w, router_b, ex_w1, ex_w3, ex_w2):
    f32 = lambda a: np.asarray(a, np.float32)
    x, c, ctx, c_ctx = f32(x), f32(c), f32(ctx), f32(c_ctx)
    w_mod, b_mod, w_in = f32(w_mod), f32(b_mod), f32(w_in)
    ncores = NCORE
    cores = [(j // 2, j % 2) for j in range(ncores)]

    cT = np.ascontiguousarray(np.concatenate([c, c_ctx[None]], 0).T)
    maps = []
    for j in range(ncores):
        l, q = divmod(j, 4)
        cs = slice(q * MOD_COLS, (q + 1) * MOD_COLS)
        maps.append({"cT": cT, "wm": np.ascontiguousarray(w_mod[l][:, cs]),
                     "bm": np.ascontiguousarray(np.broadcast_to(b_mod[l][cs], (5, MOD_COLS)))})
    res = _run(_prog("Kmod", build_Kmod), maps, "Kmod")
    mod = [np.concatenate([res[l * 4 + q]["mod"] for q in range(4)], 1).reshape(5, 6, D) for l in range(2)]
    SH1, SC1, G1, SH2, SC2, G2 = range(6)

    def vecs_for(l, b, nxt):
        v = np.zeros((16, D), np.float32)
        for kind, row in ((0, b), (1, 4)):
            o = kind * 6
            v[o + V_G1] = mod[l][row, G1]
            v[o + V_SC2] = mod[l][row, SC2]
            v[o + V_SH2] = mod[l][row, SH2]
            v[o + V_G2] = mod[l][row, G2]
            if nxt is not None:
                v[o + V_NSC1] = mod[nxt][row, SC1]
                v[o + V_NSH1] = mod[nxt][row, SH1]
        v[V_LN + 0], v[V_LN + 1] = ln_g[l, 0], ln_b[l, 0]
        v[V_LN + 2], v[V_LN + 3] = ln_g[l, 1], ln_b[l, 1]
        return _vec_pack(v)

    hfull = [np.concatenate([ctx[b], x[b]], 0) for b in range(NB)]

    groups0 = [(0, 128, 1)] + [(128 + 512 * i, 512, 0) for i in range(4)]
    T0 = 128 + 2048
    hT_core = []
    maps = []
    for (b, half) in cores:
        hT = np.ascontiguousarray(hfull[b][_core_tokens(b, half, True)].T)
        hT_core.append(hT)
        maps.append({"hT": hT, "vec": vecs_for(0, b, 0)})
    res = _run(_prog("Kln", lambda: build_Kln(groups0, T0)), maps, "Kln")
    u_core = [r["uT_out"] for r in res]

    perm = _cm_perm()
    tri_s = np.arange(128)
    U = (tri_s[:, None] <= tri_s[None, :]).astype(np.float32)
    Lo = (tri_s[:, None] >= tri_s[None, :]).astype(np.float32)
    tri = np.ascontiguousarray(np.stack([U, Lo], 1))
    negm = np.ascontiguousarray(((tri - 1.0) * 30000.0).astype(np.float32))
    ident = np.eye(128).astype(NPBF)

    def assemble_u(u_core):
        uT = np.empty((D, NB * TPB), NPBF)
        for j, (b, half) in enumerate(cores):
            uT[:, b * TPB + _core_tokens(b, half, True)] = u_core[j]
        uTm = uT.copy()
        for b in range(NB):
            lat = uT[:, b * TPB + CTX:(b + 1) * TPB]
            uTm[:, b * TPB + CTX:(b + 1) * TPB] = lat[:, perm]
        return uT, uTm

    def mixer(l, uT, uTm):
        wl = w_in[l]
        maps = []
        for j in range(ncores):
            cs = slice(j * 256, (j + 1) * 256)
            rgw = np.zeros((128, 8, 128), np.float32)
            rgb = np.zeros((128, 8), np.float32)
            lm = np.zeros((128, 4), np.float32)
            for d in range(2):
                for ch in range(2):
                    ia = (d * 2 + ch) * 2
                    blk = 2 * j + ch
                    rgw[:, ia, :] = rg_wa[l, d, blk]
                    rgw[:, ia + 1, :] = rg_wx[l, d, blk]
                    rgb[:, ia] = rg_ba[l, d, blk * 128:(blk + 1) * 128]
                    rgb[:, ia + 1] = rg_bx[l, d, blk * 128:(blk + 1) * 128]
                    lm[:, d * 2 + ch] = rg_lam[l, d, blk * 128:(blk + 1) * 128]
            maps.append({"uT": uT, "wx": np.ascontiguousarray(wl[:, cs]), "wg": np.ascontiguousarray(wl[:, C_RGG + j * 256:C_RGG + (j + 1) * 256]),
                         "rcw": np.ascontiguousarray(f32(conv_rg_w[l])[:, cs].reshape(4, 2, 128).transpose(2, 1, 0)),
                         "rcb": np.ascontiguousarray(f32(conv_rg_b[l])[cs].reshape(2, 128).T),
                         "rgw": rgw, "rgb": rgb, "lam": lm})
        for j in range(ncores):
            qs = slice(C_M + j * 256, C_M + (j + 1) * 256)
            ks = slice(C_M + W_M + j * 256, C_M + W_M + (j + 1) * 256)
            vs = slice(C_M + 2 * W_M + j * 256, C_M + 2 * W_M + (j + 1) * 256)
            os_ = slice(C_M + 3 * W_M + j * 256, C_M + 3 * W_M + (j + 1) * 256)
            gcols = [C_M + 4 * W_M + d * 16 + g * 8 + j for d in range(2) for g in range(2)]
            gbv = np.array([m_gate_b[l][d, g, j] for d in range(2) for g in range(2)], np.float32)
            cwm = f32(conv_m_w[l])
            cbm = f32(conv_m_b[l])
            cwj = np.concatenate([cwm[:, j * 256:(j + 1) * 256], cwm[:, W_M + j * 256:W_M + (j + 1) * 256]], 1)
            cbj = np.concatenate([cbm[j * 256:(j + 1) * 256], cbm[W_M + j * 256:W_M + (j + 1) * 256]])
            maps[j].update({"uTm": uTm, "wq": np.ascontiguousarray(wl[:, qs]), "wk": np.ascontiguousarray(wl[:, ks]),
                         "wvo": np.ascontiguousarray(np.concatenate([wl[:, vs], wl[:, os_]], 1)),
                         "wgt": np.ascontiguousarray(wl[:, gcols]),
                         "gb": np.ascontiguousarray(np.broadcast_to(gbv, (128, 4))),
                         "cw": np.ascontiguousarray(cwj.reshape(4, 4, 128).transpose(2, 1, 0)),
                         "cb": np.ascontiguousarray(cbj.reshape(4, 128).T),
                         "gn": np.ascontiguousarray(np.broadcast_to(f32(m_gn_g[l])[j * 256:(j + 1) * 256], (128, 256))),
                         "ident": ident, "tri": tri, "negm": negm})
        res = _run(_prog("A", lambda: build_A(NB)), maps, "A")
        yrT = np.concatenate([r["yrT"] for r in res], 0)
        ymT = np.empty((D, NB * TPB), NPBF)
        for j in range(ncores):
            ymj = res[j]["ym"]
            for b in range(NB):
                blk = ymj[b * TPB:(b + 1) * TPB]
                ymT[j * 256:(j + 1) * 256, b * TPB:b * TPB + CTX] = blk[:CTX].T
                lat = np.empty((SEQ, 256), NPBF)
                lat[perm] = blk[CTX:]
                ymT[j * 256:(j + 1) * 256, b * TPB + CTX:(b + 1) * TPB] = lat.T
        return yrT, ymT

    uT, uTm = assemble_u(u_core)
    yrT, ymT = mixer(0, uT, uTm)
    w_g0 = np.ascontiguousarray(w_in[0][:, C_G:])
    maps = []
    for j, (b, half) in enumerate(cores):
        tk = b * TPB + _core_tokens(b, half, True)
        maps.append({"yrT": np.ascontiguousarray(yrT[:, tk]), "ymT": np.ascontiguousarray(ymT[:, tk]), "uT": u_core[j], "hT": hT_core[j],
                     "w_g": w_g0, "p_rg": f32(p_rg[0]), "p_m": f32(p_m[0]), "w_out": f32(w_out[0]), "vec": vecs_for(0, b, 1),
                     "w1": f32(ff_w1[0]), "w3": f32(ff_w3[0]), "w2": f32(ff_w2[0])})
    res = _run(_prog("B0", lambda: build_B(groups0, T0, True, False)), maps, "B0")
    h1_core = [r["hT_out"] for r in res]
    u1_core = [r["uT_out"] for r in res]

    uT, uTm = assemble_u(u1_core)
    yrT, ymT = mixer(1, uT, uTm)
    groups1 = [(512 * i, 512, 0) for i in range(4)]
    T1 = 2048
    w_g1 = np.ascontiguousarray(w_in[1][:, C_G:])
    rb = np.ascontiguousarray(np.broadcast_to(f32(router_b[0]), (128, 8)))
    maps = []
    for j, (b, half) in enumerate(cores):
        tk = b * TPB + _core_tokens(b, half, False)
        maps.append({"yrT": np.ascontiguousarray(yrT[:, tk]), "ymT": np.ascontiguousarray(ymT[:, tk]),
                     "uT": np.ascontiguousarray(u1_core[j][:, 128:]), "hT": np.ascontiguousarray(h1_core[j][:, 128:]),
                     "w_g": w_g1, "p_rg": f32(p_rg[1]), "p_m": f32(p_m[1]), "w_out": f32(w_out[1]), "vec": vecs_for(1, b, None),
                     "rw": f32(router_w[0]), "rb": rb})
    res = _run(_prog("B1", lambda: build_B(groups1, T1, False, True)), maps, "B1")
    hmid_core = [r["h1T_out"] for r in res]
    vT_all = np.concatenate([r["vT_out"] for r in res], 1)
    gw_all = np.concatenate([r["gw"] for r in res], 0)

    NTOK = gw_all.shape[0]
    top2 = np.argsort(-gw_all, axis=1, kind="stable")[:, :2]
    top2.sort(axis=1)
    lists = [np.nonzero((top2 == e).any(1))[0] for e in range(8)]
    CAP = min(5632, max(512, -(-max(len(l) for l in lists) // 128) * 128))
    pos = np.zeros((NTOK, 8), np.int64)
    for e in range(8):
        pos[lists[e], e] = np.arange(len(lists[e]))
    nround = max(1, max((len(l) + CAP - 1) // CAP for l in lists))
    Y = [np.zeros((D, nround * CAP), np.float32) for _ in range(8)]
    for rd in range(nround):
        maps = []
        for e in range(8):
            idx = lists[e][rd * CAP:(rd + 1) * CAP]
            vT = np.zeros((D, CAP), NPBF)
            vT[:, :len(idx)] = vT_all[:, idx]
            wr = np.zeros((CAP,), np.float32)
            wr[:len(idx)] = gw_all[idx, e]
            maps.append({"vT": vT, "wrow": np.ascontiguousarray(np.broadcast_to(wr, (128, CAP))),
                         "w1": f32(ex_w1[0][e]), "w3": f32(ex_w3[0][e]), "w2": f32(ex_w2[0][e])})
        res = _run(_prog(("C", CAP), lambda: build_C(CAP)), maps, f"C{CAP}")
        for e in range(8):
            Y[e][:, rd * CAP:(rd + 1) * CAP] = res[e]["yT"]

    maps = []
    for j, (b, half) in enumerate(cores):
        tl = np.arange(j * T1, (j + 1) * T1)
        ya = np.empty((D, T1), np.float32)
        yb = np.empty((D, T1), np.float32)
        for e in range(8):
            ma = top2[tl, 0] == e
            mb = top2[tl, 1] == e
            ya[:, ma] = Y[e][:, pos[tl[ma], e]]
            yb[:, mb] = Y[e][:, pos[tl[mb], e]]
        maps.append({"yaT": ya, "ybT": yb, "h1T": hmid_core[j], "vec": vecs_for(1, b, None)})
    res = _run(_prog("D", lambda: build_D(T1)), maps, "D")
    out = np.empty((NB, SEQ, D), np.float32)
    for j, (b, half) in enumerate(cores):
        out[b, half * (SEQ // 2):(half + 1) * (SEQ // 2)] = res[j]["oT"].T
    return out
```

```python
import numpy as np
import ml_dtypes
from contextlib import ExitStack
import concourse.bass as bass
import concourse.mybir as mybir
from concourse.bass_utils import run_bass_kernel_spmd

F32 = mybir.dt.float32
BF16 = mybir.dt.bfloat16
AF = mybir.ActivationFunctionType
ALU = mybir.AluOpType
AX = mybir.AxisListType
NPBF = ml_dtypes.bfloat16

D = 2048
KD = 16
DFF = 7168
KF = 56
NCORE = 8
ALPHA = 4.0 ** 0.25
LN_EPS = 1e-6
GRID = 64
CTX = 256
SEQ = 4096
NB = 4
W_RG = 2048
W_M = 2048
C_RGG = W_RG
C_M = 2 * W_RG
C_G = C_M + 4 * W_M + 32
N_IN = C_G + 2 * D


class Res:
    __slots__ = ("last_w", "readers", "excl")

    def __init__(self, excl=False):
        self.last_w = None
        self.readers = []
        self.excl = excl


class Op:
    __slots__ = ("eng", "fn", "deps", "dma", "idx", "signal", "count", "slot", "slot_count")

    def __init__(self, eng, fn, dma):
        self.eng = eng
        self.fn = fn
        self.deps = []
        self.dma = dma
        self.signal = False
        self.count = 0
        self.slot = None
        self.slot_count = 0


class Prog:
    ENGS = ("pe", "act", "dve", "pool", "sp")
    NSLOT = 8

    def __init__(self, nc):
        self.nc = nc
        self.ops = []
        self.es = ExitStack()
        self._n = 0

    def sbuf(self, shape, dtype):
        self._n += 1
        return self.es.enter_context(self.nc.sbuf_tensor(f"sb{self._n}_{id(self) % 9973}", list(shape), dtype))

    def psum(self, shape, dtype):
        self._n += 1
        return self.es.enter_context(self.nc.psum_tensor(f"ps{self._n}_{id(self) % 9973}", list(shape), dtype))

    def add(self, eng, fn, reads=(), writes=(), dma=False):
        op = Op(eng, fn, dma)
        op.idx = len(self.ops)
        deps = {}
        for r in reads:
            if r.last_w is not None:
                deps[r.last_w.idx] = r.last_w
            if r.excl:
                for rd in r.readers:
                    if rd.eng != eng:
                        deps[rd.idx] = rd
        for w in writes:
            if w.last_w is not None:
                deps[w.last_w.idx] = w.last_w
            for rd in w.readers:
                deps[rd.idx] = rd
        last = {}
        for d in deps.values():
            if d.dma:
                op.deps.append(d)
                continue
            if d.eng == "pe" and eng == "pe" and not dma:
                continue
            if d.eng not in last or last[d.eng].idx < d.idx:
                last[d.eng] = d
        op.deps.extend(last.values())
        for r in reads:
            r.readers.append(op)
        for w in writes:
            w.last_w = op
            w.readers = []
        self.ops.append(op)
        return op

    def dma(self, q, out, in_, reads=(), writes=()):
        return self.add(q, lambda e: e.dma_start(out=out, in_=in_), reads, writes, dma=True)

    def emit(self):
        nc = self.nc
        ops = self.ops
        for op in ops:
            for d in op.deps:
                d.signal = True
        counts = {e: 0 for e in self.ENGS}
        dcount = {e: 0 for e in self.ENGS}
        slot_counts = {e: [0] * self.NSLOT for e in self.ENGS}
        for op in ops:
            if op.dma:
                k = dcount[op.eng]
                dcount[op.eng] += 1
                op.slot = k % self.NSLOT
                slot_counts[op.eng][op.slot] += 1
                op.slot_count = slot_counts[op.eng][op.slot]
            elif op.signal:
                counts[op.eng] += 1
                op.count = counts[op.eng]
        tag = id(self) % 9973
        sems = {e: self.es.enter_context(nc.semaphore(f"s_{e}_{tag}")) for e in self.ENGS}
        dsems = {}
        for e in self.ENGS:
            if dcount[e]:
                dsems[e] = [self.es.enter_context(nc.semaphore(f"d_{e}{i}_{tag}"))
                            for i in range(min(self.NSLOT, dcount[e]))]
        by_eng = {e: [op for op in ops if op.eng == e] for e in self.ENGS}
        block = self.es.enter_context(nc.Block())

        def run(e, h):
            seen = {}
            for op in by_eng[e]:
                waits = {}
                for d in op.deps:
                    if d.dma:
                        key = ("d", d.eng, d.slot)
                        val = 16 * d.slot_count
                        sem = dsems[d.eng][d.slot]
                    else:
                        key = ("c", d.eng)
                        val = d.count
                        sem = sems[d.eng]
                    if seen.get(key, 0) >= val:
                        continue
                    if key not in waits or waits[key][1] < val:
                        waits[key] = (sem, val)
                if op.dma and op.slot_count > 1:
                    key = ("d", e, op.slot)
                    val = 16 * (op.slot_count - 1)
                    if seen.get(key, 0) < val and (key not in waits or waits[key][1] < val):
                        waits[key] = (dsems[e][op.slot], val)
                for key, (sem, val) in waits.items():
                    h.wait_ge(sem, val)
                    seen[key] = val
                ins = op.fn(h)
                if op.dma:
                    ins.then_inc(dsems[e][op.slot], 16)
                elif op.signal:
                    ins.then_inc(sems[e], 1)
            if e in dsems:
                for s in range(len(dsems[e])):
                    val = 16 * slot_counts[e][s]
                    if seen.get(("d", e, s), 0) < val:
                        h.wait_ge(dsems[e][s], val)

        @block.tensor
        def _(h):
            run("pe", h)

        @block.scalar
        def _(h):
            run("act", h)

        @block.vector
        def _(h):
            run("dve", h)

        @block.gpsimd
        def _(h):
            run("pool", h)

        @block.sync
        def _(h):
            run("sp", h)

        self.es.close()


class Rot:
    def __init__(self, P, n, shape, dtype, psum=False):
        self.bufs = [(P.psum(shape, dtype) if psum else P.sbuf(shape, dtype)) for _ in range(n)]
        self.res = [Res(excl=psum) for _ in range(n)]
        self.i = 0
        self.n = n

    def next(self):
        j = self.i % self.n
        self.i += 1
        return self.bufs[j], self.res[j]


def new_nc():
    return bass.Bass("TRN2", target_bir_lowering=False)


def din(nc, name, shape, dt):
    return nc.dram_tensor(name, list(shape), dt, kind="ExternalInput").ap()


def dout(nc, name, shape, dt):
    return nc.dram_tensor(name, list(shape), dt, kind="ExternalOutput").ap()


class Ctx:
    def __init__(self, P, wslots=6, wcols=4096, ntmp=6, nmm=6, ln=True):
        self.P = P
        self.mm = Rot(P, nmm, [128, 512], F32, psum=True)
        self.st = Rot(P, 2, [128, 512], F32, psum=True)
        self.tmp = Rot(P, ntmp, [128, 512], F32)
        self.wp = Rot(P, wslots, [128, wcols], BF16) if wslots else None
        self.ones = P.sbuf([128, 128], F32)
        self.r_ones = Res()
        P.add("dve", lambda e: e.memset(self.ones[:], 1.0), writes=[self.r_ones])
        self.stat = Rot(P, 4, [128, 512], F32) if ln else None

    def wtile(self, src, nk, ncols):
        buf, res = self.wp.next()
        view = buf[:, 0:nk * ncols].rearrange("p (k c) -> p k c", k=nk)
        self.P.dma("pool", view, src.rearrange("(k p) c -> p k c", p=128), writes=[res])
        return view, res


def fm_mm(cx, wview, wres, c0, xs, xres, G, nk=None, koff=0, ps=None, first=True, last=True):
    P = cx.P
    if ps is None:
        ps = cx.mm.next()
    pbuf, pres = ps
    nk = nk if nk is not None else KD
    for k in range(nk):
        P.add("pe", lambda e, k=k: e.matmul(pbuf[:, 0:G], wview[:, k, c0:c0 + 128], xs(koff + k),
                                            start=(first and k == 0), stop=(last and k == nk - 1)),
              reads=[wres, xres(koff + k)], writes=[pres])
    return ps


def layer_norm_fm(cx, src, src_res, G, outs):
    P = cx.P
    s1, r1 = cx.st.next()
    s2, r2 = cx.st.next()
    for c in range(KD):
        sq, rsq = cx.tmp.next()
        P.add("act", lambda e, c=c, sq=sq: e.activation(sq[:, 0:G], src(c), AF.Square), reads=[src_res(c)], writes=[rsq])
        P.add("pe", lambda e, c=c: e.matmul(s1[:, 0:G], cx.ones[:], src(c), start=(c == 0), stop=(c == KD - 1)),
              reads=[cx.r_ones, src_res(c)], writes=[r1])
        P.add("pe", lambda e, c=c, sq=sq: e.matmul(s2[:, 0:G], cx.ones[:], sq[:, 0:G], start=(c == 0), stop=(c == KD - 1)),
              reads=[cx.r_ones, rsq], writes=[r2])
    mean, rm = cx.stat.next()
    msq, rq = cx.stat.next()
    var, rv = cx.stat.next()
    rstd, rr = cx.stat.next()
    P.add("act", lambda e: e.mul(mean[:, 0:G], s1[:, 0:G], 1.0 / D), reads=[r1], writes=[rm])
    P.add("dve", lambda e: e.tensor_tensor(msq[:, 0:G], mean[:, 0:G], mean[:, 0:G], ALU.mult), reads=[rm], writes=[rq])
    P.add("dve", lambda e: e.scalar_tensor_tensor(var[:, 0:G], s2[:, 0:G], 1.0 / D, msq[:, 0:G], ALU.mult, ALU.subtract),
          reads=[r2, rq], writes=[rv])
    P.add("act", lambda e: e.activation(var[:, 0:G], var[:, 0:G], AF.Sqrt, bias=LN_EPS, scale=1.0), reads=[rv], writes=[rv])
    P.add("dve", lambda e: e.reciprocal(rstd[:, 0:G], var[:, 0:G]), reads=[rv], writes=[rr])
    for c in range(KD):
        t1, rt1 = cx.tmp.next()
        P.add("dve", lambda e, c=c, t1=t1: e.tensor_tensor(t1[:, 0:G], src(c), mean[:, 0:G], ALU.subtract),
              reads=[src_res(c), rm], writes=[rt1])
        P.add("dve", lambda e, t1=t1: e.tensor_tensor(t1[:, 0:G], t1[:, 0:G], rstd[:, 0:G], ALU.mult),
              reads=[rt1, rr], writes=[rt1])
        for (dst, dres, sc, bi, extra) in outs:
            P.add("act", lambda e, c=c, t1=t1, dst=dst, sc=sc, bi=bi: e.activation(dst(c), t1[:, 0:G], AF.Identity, bias=bi(c), scale=sc(c)),
                  reads=[rt1] + list(extra), writes=[dres(c)])


def ffn_fm(cx, w1, w3, w2, vview, vres, G, act_view, act_res, consume):
    P = cx.P
    for f2 in range(KF // 2):
        t1, rw1 = cx.wtile(w1[:, f2 * 256:(f2 + 1) * 256], KD, 256)
        t3, rw3 = cx.wtile(w3[:, f2 * 256:(f2 + 1) * 256], KD, 256)
        for cc in range(2):
            f = 2 * f2 + cc
            p1, rp1 = fm_mm(cx, t1, rw1, cc * 128, vview, vres, G)
            p3, rp3 = fm_mm(cx, t3, rw3, cc * 128, vview, vres, G)
            s, rs = cx.tmp.next()
            P.add("act", lambda e, p1=p1, s=s: e.activation(s[:, 0:G], p1[:, 0:G], AF.Silu), reads=[rp1], writes=[rs])
            P.add("dve", lambda e, f=f, p3=p3, s=s: e.tensor_tensor(act_view(f), s[:, 0:G], p3[:, 0:G], ALU.mult),
                  reads=[rs, rp3], writes=[act_res(f)])
    for c in range(KD):
        ta, ra = cx.wtile(w2[0:(KF // 2) * 128, c * 128:(c + 1) * 128], KF // 2, 128)
        tb, rb = cx.wtile(w2[(KF // 2) * 128:KF * 128, c * 128:(c + 1) * 128], KF // 2, 128)
        ps = cx.mm.next()
        fm_mm(cx, ta, ra, 0, act_view, act_res, G, nk=KF // 2, koff=0, ps=ps, first=True, last=False)
        fm_mm(cx, tb, rb, 0, act_view, act_res, G, nk=KF // 2, koff=KF // 2, ps=ps, first=False, last=True)
        consume(c, ps[0], ps[1])


V_G1, V_SC2, V_SH2, V_G2, V_NSC1, V_NSH1 = range(6)
V_LN = 12


def build_B(groups, T, ffn, router):
    nc = new_nc()
    yrT = din(nc, "yrT", [D, T], BF16)
    ymT = din(nc, "ymT", [D, T], BF16)
    uT = din(nc, "uT", [D, T], BF16)
    hT = din(nc, "hT", [D, T], F32)
    w_g = din(nc, "w_g", [D, 2 * D], F32)
    p_rg = din(nc, "p_rg", [W_RG, D], F32)
    p_m = din(nc, "p_m", [W_M, D], F32)
    w_out = din(nc, "w_out", [D, D], F32)
    vec = din(nc, "vec", [128, 16, KD], F32)
    if ffn:
        w1 = din(nc, "w1", [D, DFF], F32)
        w3 = din(nc, "w3", [D, DFF], F32)
        w2 = din(nc, "w2", [DFF, D], F32)
        hT_out = dout(nc, "hT_out", [D, T], F32)
        uT_out = dout(nc, "uT_out", [D, T], BF16)
    if router:
        rw = din(nc, "rw", [D, 8], F32)
        rb = din(nc, "rb", [128, 8], F32)
        h1T_out = dout(nc, "h1T_out", [D, T], F32)
        vT_out = dout(nc, "vT_out", [D, T], BF16)
        gw_out = dout(nc, "gw", [T, 8], F32)

    P = Prog(nc)
    cx = Ctx(P, wslots=5, ntmp=4) if router else Ctx(P)
    vs = P.sbuf([128, 16, KD], F32)
    r_vs = Res()
    P.dma("sp", vs[:], vec, writes=[r_vs])
    for kind in range(2):
        for slot in (V_SC2, V_NSC1):
            j = kind * 6 + slot
            P.add("dve", lambda e, j=j: e.tensor_scalar_add(vs[:, j, :], vs[:, j, :], 1.0), reads=[r_vs], writes=[r_vs])

    def vcol(kind, slot):
        j = (kind * 6 + slot) if slot < 6 else slot
        return lambda c: vs[:, j, c:c + 1]

    scr = P.sbuf([128, 64 * 512], BF16)
    rj = [Res() for _ in range(64)]
    hbuf = P.sbuf([128, KD, 512], F32)
    rh = [Res() for _ in range(KD)]
    vbuf = P.sbuf([128, KD, 512], BF16)
    rv = [Res() for _ in range(KD)]
    if router:
        v32 = P.sbuf([128, KD, 512], F32)
        rv32 = [Res() for _ in range(KD)]
        rws = P.sbuf([128, KD, 8], F32)
        r_rws = Res()
        P.dma("sp", rws[:], rw.rearrange("(k p) e -> p k e", p=128), writes=[r_rws])
        rbs = P.sbuf([128, 8], F32)
        r_rbs = Res()
        P.dma("sp", rbs[:], rb, writes=[r_rbs])
        small = Rot(P, 12, [128, 8], F32)

    def do_group(tok0, G, kind):
        def q(qi, k, G=G):
            return scr[:, (qi * 16 + k) * 512:(qi * 16 + k) * 512 + G]

        def qall(qi, G=G):
            return scr[:, qi * 16 * 512:(qi + 1) * 16 * 512].rearrange("p (k t) -> p k t", k=16)[:, :, 0:G]

        for qi, src in enumerate((yrT, ymT, uT)):
            P.dma("sp", qall(qi), src[:, tok0:tok0 + G].rearrange("(k p) t -> p k t", p=128),
                  writes=rj[qi * 16:(qi + 1) * 16])
        P.dma("sp", hbuf[:, :, 0:G], hT[:, tok0:tok0 + G].rearrange("(k p) t -> p k t", p=128), writes=rh)
        for c in range(KD):
            P.add("act", lambda e, c=c, G=G: e.mul(hbuf[:, c, 0:G], hbuf[:, c, 0:G], ALPHA), reads=[rh[c]], writes=[rh[c]])
        for c2 in range(KD // 2):
            cs = slice(c2 * 256, (c2 + 1) * 256)
            tg1, rg1 = cx.wtile(w_g[:, cs], KD, 256)
            tg2, rg2 = cx.wtile(w_g[:, D + c2 * 256:D + (c2 + 1) * 256], KD, 256)
            tp1, rp1 = cx.wtile(p_rg[:, cs], KD, 256)
            tp2, rp2 = cx.wtile(p_m[:, cs], KD, 256)
            for cc in range(2):
                c = 2 * c2 + cc
                pg1, rpg1 = fm_mm(cx, tg1, rg1, cc * 128, lambda k: q(2, k), lambda k: rj[32 + k], G)
                pg2, rpg2 = fm_mm(cx, tg2, rg2, cc * 128, lambda k: q(2, k), lambda k: rj[32 + k], G)
                pa1, rpa1 = fm_mm(cx, tp1, rp1, cc * 128, lambda k: q(0, k), lambda k: rj[k], G)
                pa2, rpa2 = fm_mm(cx, tp2, rp2, cc * 128, lambda k: q(1, k), lambda k: rj[16 + k], G)
                s1, rs1 = cx.tmp.next()
                s2, rs2 = cx.tmp.next()
                P.add("act", lambda e, pg1=pg1, s1=s1, G=G: e.activation(s1[:, 0:G], pg1[:, 0:G], AF.Sigmoid), reads=[rpg1], writes=[rs1])
                P.add("act", lambda e, pg2=pg2, s2=s2, G=G: e.activation(s2[:, 0:G], pg2[:, 0:G], AF.Sigmoid), reads=[rpg2], writes=[rs2])
                P.add("dve", lambda e, pa1=pa1, s1=s1, G=G: e.tensor_tensor(s1[:, 0:G], s1[:, 0:G], pa1[:, 0:G], ALU.mult), reads=[rs1, rpa1], writes=[rs1])
                P.add("dve", lambda e, pa2=pa2, s2=s2, G=G: e.tensor_tensor(s2[:, 0:G], s2[:, 0:G], pa2[:, 0:G], ALU.mult), reads=[rs2, rpa2], writes=[rs2])
                P.add("dve", lambda e, c=c, s1=s1, s2=s2, G=G: e.tensor_tensor(q(3, c, G), s1[:, 0:G], s2[:, 0:G], ALU.add),
                      reads=[rs1, rs2], writes=[rj[48 + c]])
        g1 = vcol(kind, V_G1)
        for c2 in range(KD // 2):
            to, ro = cx.wtile(w_out[:, c2 * 256:(c2 + 1) * 256], KD, 256)
            for cc in range(2):
                c = 2 * c2 + cc
                po, rpo = fm_mm(cx, to, ro, cc * 128, lambda k: q(3, k), lambda k: rj[48 + k], G)
                P.add("dve", lambda e, c=c, po=po, G=G, g1=g1: e.scalar_tensor_tensor(hbuf[:, c, 0:G], po[:, 0:G], g1(c), hbuf[:, c, 0:G], ALU.mult, ALU.add),
                      reads=[rpo, rh[c], r_vs], writes=[rh[c]])
        hsrc = lambda c, G=G: hbuf[:, c, 0:G]
        layer_norm_fm(cx, hsrc, lambda c: rh[c], G,
                      [(hsrc, lambda c: rh[c], vcol(0, V_LN + 0), vcol(0, V_LN + 1), [r_vs])])
        outs = [(lambda c, G=G: vbuf[:, c, 0:G], lambda c: rv[c], vcol(kind, V_SC2), vcol(kind, V_SH2), [r_vs])]
        if router:
            outs.append((lambda c, G=G: v32[:, c, 0:G], lambda c: rv32[c], vcol(kind, V_SC2), vcol(kind, V_SH2), [r_vs]))
        layer_norm_fm(cx, hsrc, lambda c: rh[c], G, outs)
        if ffn:
            g2 = vcol(kind, V_G2)
            for c in range(KD):
                P.add("act", lambda e, c=c, G=G: e.mul(hbuf[:, c, 0:G], hbuf[:, c, 0:G], ALPHA), reads=[rh[c]], writes=[rh[c]])

            def consume(c, pbuf, pres, G=G, g2=g2):
                P.add("dve", lambda e: e.scalar_tensor_tensor(hbuf[:, c, 0:G], pbuf[:, 0:G], g2(c), hbuf[:, c, 0:G], ALU.mult, ALU.add),
                      reads=[pres, rh[c], r_vs], writes=[rh[c]])

            ffn_fm(cx, w1, w3, w2, lambda k, G=G: vbuf[:, k, 0:G], lambda k: rv[k], G,
                   lambda f, G=G: scr[:, f * 512:f * 512 + G], lambda f: rj[f], consume)
            layer_norm_fm(cx, hsrc, lambda c: rh[c], G,
                          [(hsrc, lambda c: rh[c], vcol(0, V_LN + 2), vcol(0, V_LN + 3), [r_vs])])
            P.dma("sp", hT_out[:, tok0:tok0 + G].rearrange("(k p) t -> p k t", p=128), hbuf[:, :, 0:G], reads=rh)
            layer_norm_fm(cx, hsrc, lambda c: rh[c], G,
                          [(lambda c, G=G: vbuf[:, c, 0:G], lambda c: rv[c], vcol(kind, V_NSC1), vcol(kind, V_NSH1), [r_vs])])
            P.dma("sp", uT_out[:, tok0:tok0 + G].rearrange("(k p) t -> p k t", p=128), vbuf[:, :, 0:G], reads=rv)
        if router:
            P.dma("sp", h1T_out[:, tok0:tok0 + G].rearrange("(k p) t -> p k t", p=128), hbuf[:, :, 0:G], reads=rh)
            P.dma("sp", vT_out[:, tok0:tok0 + G].rearrange("(k p) t -> p k t", p=128), vbuf[:, :, 0:G], reads=rv)
            for t0 in range(0, G, 128):
                pl, rpl = cx.mm.next()
                for k in range(KD):
                    P.add("pe", lambda e, k=k, t0=t0, pl=pl: e.matmul(pl[:, 0:8], v32[:, k, t0:t0 + 128], rws[:, k, :], start=(k == 0), stop=(k == KD - 1)),
                          reads=[rv32[k], r_rws], writes=[rpl])
                lg, rlg = small.next()
                m1, rm1 = small.next()
                e1, re1 = small.next()
                l2, rl2 = small.next()
                m2, rm2 = small.next()
                e2, re2 = small.next()
                dd, rdd = small.next()
                gwt, rgw = small.next()
                P.add("dve", lambda e, pl=pl, lg=lg: e.tensor_tensor(lg[:], pl[:, 0:8], rbs[:], ALU.add), reads=[rpl, r_rbs], writes=[rlg])
                P.add("dve", lambda e, lg=lg, m1=m1: e.reduce_max(m1[:, 0:1], lg[:], AX.X), reads=[rlg], writes=[rm1])
                P.add("dve", lambda e, lg=lg, m1=m1, e1=e1: e.tensor_scalar(e1[:], lg[:], m1[:, 0:1], None, ALU.is_equal), reads=[rlg, rm1], writes=[re1])
                P.add("dve", lambda e, lg=lg, e1=e1, l2=l2: e.scalar_tensor_tensor(l2[:], e1[:], -1e30, lg[:], ALU.mult, ALU.add), reads=[rlg, re1], writes=[rl2])
                P.add("dve", lambda e, l2=l2, m2=m2: e.reduce_max(m2[:, 0:1], l2[:], AX.X), reads=[rl2], writes=[rm2])
                P.add("dve", lambda e, l2=l2, m2=m2, e2=e2: e.tensor_scalar(e2[:], l2[:], m2[:, 0:1], None, ALU.is_equal), reads=[rl2, rm2], writes=[re2])
                P.add("dve", lambda e, m1=m1, m2=m2, dd=dd: e.tensor_tensor(dd[:, 0:1], m2[:, 0:1], m1[:, 0:1], ALU.subtract), reads=[rm1, rm2], writes=[rdd])
                P.add("act", lambda e, dd=dd: e.activation(dd[:, 1:2], dd[:, 0:1], AF.Exp), reads=[rdd], writes=[rdd])
                P.add("dve", lambda e, dd=dd: e.tensor_scalar_add(dd[:, 2:3], dd[:, 1:2], 1.0), reads=[rdd], writes=[rdd])
                P.add("dve", lambda e, dd=dd: e.reciprocal(dd[:, 2:3], dd[:, 2:3]), reads=[rdd], writes=[rdd])
                P.add("dve", lambda e, dd=dd: e.tensor_tensor(dd[:, 3:4], dd[:, 1:2], dd[:, 2:3], ALU.mult), reads=[rdd], writes=[rdd])
                P.add("dve", lambda e, e1=e1, dd=dd, gwt=gwt: e.tensor_scalar(gwt[:], e1[:], dd[:, 2:3], None, ALU.mult), reads=[re1, rdd], writes=[rgw])
                P.add("dve", lambda e, e2=e2, dd=dd, gwt=gwt: e.scalar_tensor_tensor(gwt[:], e2[:], dd[:, 3:4], gwt[:], ALU.mult, ALU.add), reads=[re2, rdd, rgw], writes=[rgw])
                P.dma("sp", gw_out[tok0 + t0:tok0 + t0 + 128, :], gwt[:], reads=[rgw])
    for (tok0, G, kind) in groups:
        do_group(tok0, G, kind)
    P.emit()
    return nc


def build_C(NS, K1):
    T = NS * 512
    nc = new_nc()
    vT = din(nc, "vT", [D, T], BF16)
    wrow = din(nc, "wrow", [128, T], F32)
    wts = [(din(nc, "w1", [D, DFF], F32), din(nc, "w3", [D, DFF], F32), din(nc, "w2", [DFF, D], F32))]
    if K1 < NS:
        wts.append((din(nc, "w1b", [D, DFF], F32), din(nc, "w3b", [D, DFF], F32), din(nc, "w2b", [DFF, D], F32)))
    yT = dout(nc, "yT", [D, T], F32)
    P = Prog(nc)
    cx = Ctx(P, wslots=8)
    vb = Rot(P, 2, [128, KD, 512], BF16)
    wr = Rot(P, 2, [128, 512], F32)
    scr = P.sbuf([128, KF * 512], BF16)
    rj = [Res() for _ in range(KF)]

    def do_group(tok0, G, w1, w3, w2):
        vbuf, rvb = vb.next()
        wrt, rwr = wr.next()
        P.dma("sp", vbuf[:, :, 0:G], vT[:, tok0:tok0 + G].rearrange("(k p) t -> p k t", p=128), writes=[rvb])
        P.dma("sp", wrt[:, 0:G], wrow[:, tok0:tok0 + G], writes=[rwr])

        def consume(c, pbuf, pres):
            o, ro = cx.tmp.next()
            P.add("dve", lambda e: e.tensor_tensor(o[:, 0:G], pbuf[:, 0:G], wrt[:, 0:G], ALU.mult), reads=[pres, rwr], writes=[ro])
            P.dma("sp", yT[c * 128:(c + 1) * 128, tok0:tok0 + G], o[:, 0:G], reads=[ro])

        ffn_fm(cx, w1, w3, w2, lambda k: vbuf[:, k, 0:G], lambda k: rvb, G,
               lambda f: scr[:, f * 512:f * 512 + G], lambda f: rj[f], consume)

    for sl in range(NS):
        w1, w3, w2 = wts[0] if sl < K1 else wts[1]
        do_group(sl * 512, 512, w1, w3, w2)
    P.emit()
    return nc


def build_D(T):
    nc = new_nc()
    yaT = din(nc, "yaT", [D, T], F32)
    ybT = din(nc, "ybT", [D, T], F32)
    h1T = din(nc, "h1T", [D, T], F32)
    vec = din(nc, "vec", [128, 16, KD], F32)
    oT = dout(nc, "oT", [D, T], F32)
    P = Prog(nc)
    cx = Ctx(P, wslots=0)
    vs = P.sbuf([128, 16, KD], F32)
    r_vs = Res()
    P.dma("sp", vs[:], vec, writes=[r_vs])
    ya = P.sbuf([128, KD, 512], F32)
    yb = P.sbuf([128, KD, 512], F32)
    hb = P.sbuf([128, KD, 512], F32)
    rya = [Res() for _ in range(KD)]
    ryb = [Res() for _ in range(KD)]
    rh = [Res() for _ in range(KD)]

    def do_group(tok0, G):
        for buf, res, src in ((ya, rya, yaT), (yb, ryb, ybT), (hb, rh, h1T)):
            P.dma("sp", buf[:, :, 0:G], src[:, tok0:tok0 + G].rearrange("(k p) t -> p k t", p=128), writes=res)
        for c in range(KD):
            P.add("act", lambda e, c=c: e.mul(hb[:, c, 0:G], hb[:, c, 0:G], ALPHA), reads=[rh[c]], writes=[rh[c]])
            P.add("dve", lambda e, c=c: e.tensor_tensor(ya[:, c, 0:G], ya[:, c, 0:G], yb[:, c, 0:G], ALU.add), reads=[rya[c], ryb[c]], writes=[rya[c]])
            P.add("dve", lambda e, c=c: e.scalar_tensor_tensor(hb[:, c, 0:G], ya[:, c, 0:G], vs[:, V_G2, c:c + 1], hb[:, c, 0:G], ALU.mult, ALU.add),
                  reads=[rya[c], rh[c], r_vs], writes=[rh[c]])
        hsrc = lambda c: hb[:, c, 0:G]
        layer_norm_fm(cx, hsrc, lambda c: rh[c], G,
                      [(hsrc, lambda c: rh[c], lambda c: vs[:, V_LN + 2, c:c + 1], lambda c: vs[:, V_LN + 3, c:c + 1], [r_vs])])
        P.dma("sp", oT[:, tok0:tok0 + G].rearrange("(k p) t -> p k t", p=128), hb[:, :, 0:G], reads=rh)

    for tok0 in range(0, T, 512):
        do_group(tok0, min(512, T - tok0))
    P.emit()
    return nc


def build_Kln(groups, T):
    nc = new_nc()
    hT = din(nc, "hT", [D, T], F32)
    vec = din(nc, "vec", [128, 16, KD], F32)
    uT_out = dout(nc, "uT_out", [D, T], BF16)
    P = Prog(nc)
    cx = Ctx(P, wslots=0)
    vs = P.sbuf([128, 16, KD], F32)
    r_vs = Res()
    P.dma("sp", vs[:], vec, writes=[r_vs])
    for kind in range(2):
        j = kind * 6 + V_NSC1
        P.add("dve", lambda e, j=j: e.tensor_scalar_add(vs[:, j, :], vs[:, j, :], 1.0), reads=[r_vs], writes=[r_vs])
    hb = Rot(P, 2, [128, KD, 512], F32)
    ub = Rot(P, 2, [128, KD, 512], BF16)

    def do_group(tok0, G, kind):
        hbuf, rhb = hb.next()
        ubuf, rub = ub.next()
        P.dma("sp", hbuf[:, :, 0:G], hT[:, tok0:tok0 + G].rearrange("(k p) t -> p k t", p=128), writes=[rhb])
        layer_norm_fm(cx, lambda c: hbuf[:, c, 0:G], lambda c: rhb, G,
                      [(lambda c: ubuf[:, c, 0:G], lambda c: rub, lambda c: vs[:, kind * 6 + V_NSC1, c:c + 1],
                        lambda c: vs[:, kind * 6 + V_NSH1, c:c + 1], [r_vs])])
        P.dma("sp", uT_out[:, tok0:tok0 + G].rearrange("(k p) t -> p k t", p=128), ubuf[:, :, 0:G], reads=[rub])

    for (tok0, G, kind) in groups:
        do_group(tok0, G, kind)
    P.emit()
    return nc


MOD_COLS = 2 * 6 * D // NCORE


def build_Kmod():
    nc = new_nc()
    cT = din(nc, "cT", [D, 5], F32)
    wm = din(nc, "wm", [D, MOD_COLS], F32)
    bm = din(nc, "bm", [5, MOD_COLS], F32)
    out = dout(nc, "mod", [5, MOD_COLS], F32)
    P = Prog(nc)
    ps = Rot(P, 2, [128, 512], F32, psum=True)
    wt = Rot(P, 2, [128, KD, 512], F32)
    cs = P.sbuf([128, KD, 5], F32)
    r_cs = Res()
    P.dma("sp", cs[:], cT.rearrange("(k p) r -> p k r", p=128), writes=[r_cs])
    P.add("act", lambda e: e.activation(cs[:], cs[:], AF.Silu), reads=[r_cs], writes=[r_cs])
    bs = P.sbuf([5, MOD_COLS], F32)
    r_bs = Res()
    P.dma("sp", bs[:], bm, writes=[r_bs])
    ob = P.sbuf([5, MOD_COLS], F32)
    r_ob = Res()
    for n in range(MOD_COLS // 512):
        w, rw_ = wt.next()
        P.dma("sp" if n % 2 == 0 else "act", w[:], wm[:, n * 512:(n + 1) * 512].rearrange("(k p) c -> p k c", p=128), writes=[rw_])
        p, rp = ps.next()
        for k in range(KD):
            P.add("pe", lambda e, k=k, p=p, w=w: e.matmul(p[0:5, :], cs[:, k, :], w[:, k, :], start=(k == 0), stop=(k == KD - 1)),
                  reads=[r_cs, rw_], writes=[rp])
        P.add("dve", lambda e, n=n, p=p: e.tensor_tensor(ob[:, n * 512:(n + 1) * 512], p[0:5, :], bs[:, n * 512:(n + 1) * 512], ALU.add),
              reads=[rp, r_bs], writes=[r_ob])
    P.dma("sp", out, ob[:], reads=[r_ob])
    P.emit()
    return nc


TPB = CTX + SEQ
GELU_C = 0.044715
GELU_S = 1.5957691216057308


def batch_groups():
    return [(0, CTX)] + [(CTX + 512 * i, 512) for i in range(SEQ // 512)]


def build_Arg(nbatch, nc=None):
    NT = nbatch * TPB
    nc = nc or new_nc()
    uT = din(nc, "uT", [D, NT], BF16)
    wx = din(nc, "wx", [D, 256], F32)
    wg = din(nc, "wg", [D, 256], F32)
    cw = din(nc, "rcw", [128, 2, 4], F32)
    cb = din(nc, "rcb", [128, 2], F32)
    rgw = din(nc, "rgw", [128, 8, 128], F32)
    rgb = din(nc, "rgb", [128, 8], F32)
    lam = din(nc, "lam", [128, 4], F32)
    yrT = dout(nc, "yrT", [256, NT], BF16)
    P = Prog(nc)
    cx = Ctx(P, wslots=2, wcols=KD * 256, ntmp=8)
    twx, rwx = cx.wtile(wx, KD, 256)
    twg, rwg = cx.wtile(wg, KD, 256)
    small = {}
    r_small = Res()
    for name, src, shape in (("cw", cw, [128, 2, 4]), ("cb", cb, [128, 2]), ("rgw", rgw, [128, 8, 128]), ("rgb", rgb, [128, 8]), ("lam", lam, [128, 4])):
        small[name] = P.sbuf(shape, F32)
        P.dma("sp", small[name][:], src, writes=[r_small])
    clam = P.sbuf([128, 4], F32)
    clam2 = P.sbuf([128, 4], F32)
    r_cl = Res()
    P.add("act", lambda e: e.activation(clam[:], small["lam"][:], AF.Exp, scale=-1.0), reads=[r_small], writes=[r_cl])
    P.add("act", lambda e: e.activation(clam[:], clam[:], AF.Ln, bias=1.0), reads=[r_cl], writes=[r_cl])
    P.add("dve", lambda e: e.tensor_scalar_mul(clam2[:], clam[:], -16.0), reads=[r_cl], writes=[r_cl])
    P.add("dve", lambda e: e.tensor_scalar_mul(clam[:], clam[:], -8.0), reads=[r_cl], writes=[r_cl])

    ub = Rot(P, 2, [128, KD, 512], BF16)
    XR = [P.sbuf([128, TPB + 6], F32) for _ in range(2)]
    rxr = [Res(), Res()]
    XC = [P.sbuf([128, TPB], F32) for _ in range(2)]
    rxc = [Res(), Res()]
    GB = [P.sbuf([128, TPB], BF16) for _ in range(2)]
    rgbuf = [Res(), Res()]
    H = [P.sbuf([128, TPB + 1], F32) for _ in range(2)]
    rH = [Res(), Res()]
    for ch in range(2):
        P.add("dve", lambda e, ch=ch: e.memset(XR[ch][:], 0.0), writes=[rxr[ch]])
    A, B = XR[0], XR[1]
    segs = ((0, 0, CTX), (CTX + 3, CTX, SEQ))

    def posraw(tok):
        return tok + 2 if tok < CTX else tok + 5

    def do_batch(b):
        base = b * TPB
        if b > 0:
            for ch in range(2):
                for col in (0, CTX + 2, TPB + 5):
                    w = 2 if col != TPB + 5 else 1
                    if col == CTX + 2:
                        w = 3
                    P.add("dve", lambda e, ch=ch, col=col, w=w: e.memset(XR[ch][:, col:col + w], 0.0), writes=[rxr[ch]])
        for (tok0, G) in batch_groups():
            ubuf, rub = ub.next()
            P.dma("sp", ubuf[:, :, 0:G], uT[:, base + tok0:base + tok0 + G].rearrange("(k p) t -> p k t", p=128), writes=[rub])
            for ch in range(2):
                px, rpx = fm_mm(cx, twx, rwx, ch * 128, lambda k, ubuf=ubuf, G=G: ubuf[:, k, 0:G], lambda k, rub=rub: rub, G)
                P.add("act", lambda e, ch=ch, px=px, tok0=tok0, G=G: e.copy(XR[ch][:, posraw(tok0):posraw(tok0) + G], px[:, 0:G]),
                      reads=[rpx], writes=[rxr[ch]])
                pg, rpg = fm_mm(cx, twg, rwg, ch * 128, lambda k, ubuf=ubuf, G=G: ubuf[:, k, 0:G], lambda k, rub=rub: rub, G)
                t, rt = cx.tmp.next()
                P.add("act", lambda e, pg=pg, t=t, G=G: e.activation(t[:, 0:G], pg[:, 0:G], AF.Square), reads=[rpg], writes=[rt])
                P.add("dve", lambda e, t=t, G=G: e.tensor_scalar(t[:, 0:G], t[:, 0:G], GELU_C, 1.0, ALU.mult, ALU.add), reads=[rt], writes=[rt])
                P.add("dve", lambda e, pg=pg, t=t, G=G: e.tensor_tensor(t[:, 0:G], t[:, 0:G], pg[:, 0:G], ALU.mult), reads=[rt, rpg], writes=[rt])
                P.add("act", lambda e, t=t, G=G: e.activation(t[:, 0:G], t[:, 0:G], AF.Sigmoid, scale=GELU_S), reads=[rt], writes=[rt])
                P.add("dve", lambda e, ch=ch, pg=pg, t=t, tok0=tok0, G=G: e.tensor_tensor(GB[ch][:, tok0:tok0 + G], t[:, 0:G], pg[:, 0:G], ALU.mult),
                      reads=[rt, rpg], writes=[rgbuf[ch]])
        for ch in range(2):
            for (praw, ptok, L) in segs:
                for h0 in range(0, L, 2048):
                    hl = min(2048, L - h0)
                    o = XC[ch][:, ptok + h0:ptok + h0 + hl]
                    P.add("dve", lambda e, ch=ch, o=o, praw=praw, h0=h0, hl=hl: e.tensor_scalar(
                        o, XR[ch][:, praw + h0:praw + h0 + hl], small["cw"][:, ch, 0:1], small["cb"][:, ch:ch + 1], ALU.mult, ALU.add),
                        reads=[rxr[ch], r_small], writes=[rxc[ch]])
                    for j in range(1, 4):
                        P.add("dve", lambda e, ch=ch, o=o, praw=praw, h0=h0, hl=hl, j=j: e.scalar_tensor_tensor(
                            o, XR[ch][:, praw + h0 + j:praw + h0 + j + hl], small["cw"][:, ch, j:j + 1], o, ALU.mult, ALU.add),
                            reads=[rxr[ch], r_small, rxc[ch]], writes=[rxc[ch]])
        for ch in range(2):
            for d in range(2):
                ia = (d * 2 + ch) * 2
                for (tok0, G) in batch_groups():
                    sb_, sl = (0, CTX) if tok0 < CTX else (CTX, SEQ)
                    if d == 0:
                        oa = A[:, 1 + tok0:1 + tok0 + G]
                        obb = B[:, 1 + tok0:1 + tok0 + G]
                    else:
                        phi = sb_ + sl - 1 - (tok0 - sb_)
                        oa = A[:, 1 + phi:1 + phi - G:-1]
                        obb = B[:, 1 + phi:1 + phi - G:-1]
                    xcs = XC[ch][:, tok0:tok0 + G]
                    pr, rpr = cx.mm.next()
                    P.add("pe", lambda e, pr=pr, ia=ia, xcs=xcs, G=G: e.matmul(pr[:, 0:G], small["rgw"][:, ia, :], xcs, start=True, stop=True),
                          reads=[r_small, rxc[ch]], writes=[rpr])
                    pi, rpi = cx.mm.next()
                    P.add("pe", lambda e, pi=pi, ia=ia, xcs=xcs, G=G: e.matmul(pi[:, 0:G], small["rgw"][:, ia + 1, :], xcs, start=True, stop=True),
                          reads=[r_small, rxc[ch]], writes=[rpi])
                    r_, rr = cx.tmp.next()
                    s_, rs = cx.tmp.next()
                    i_, ri = cx.tmp.next()
                    P.add("act", lambda e, pr=pr, r_=r_, ia=ia, G=G: e.activation(r_[:, 0:G], pr[:, 0:G], AF.Sigmoid, bias=small["rgb"][:, ia:ia + 1]),
                          reads=[rpr, r_small], writes=[rr])
                    P.add("act", lambda e, r_=r_, oa=oa, d=d, ch=ch, G=G: e.activation(oa, r_[:, 0:G], AF.Exp, scale=clam[:, d * 2 + ch:d * 2 + ch + 1]),
                          reads=[rr, r_cl], writes=[rxr[0]])
                    P.add("act", lambda e, r_=r_, s_=s_, d=d, ch=ch, G=G: e.activation(s_[:, 0:G], r_[:, 0:G], AF.Exp, scale=clam2[:, d * 2 + ch:d * 2 + ch + 1]),
                          reads=[rr, r_cl], writes=[rs])
                    P.add("act", lambda e, s_=s_, G=G: e.activation(s_[:, 0:G], s_[:, 0:G], AF.Sqrt, bias=1.0, scale=-1.0), reads=[rs], writes=[rs])
                    P.add("act", lambda e, pi=pi, i_=i_, ia=ia, G=G: e.activation(i_[:, 0:G], pi[:, 0:G], AF.Sigmoid, bias=small["rgb"][:, ia + 1:ia + 2]),
                          reads=[rpi, r_small], writes=[ri])
                    P.add("dve", lambda e, s_=s_, i_=i_, G=G: e.tensor_tensor(s_[:, 0:G], s_[:, 0:G], i_[:, 0:G], ALU.mult), reads=[rs, ri], writes=[rs])
                    P.add("dve", lambda e, s_=s_, obb=obb, xcs=xcs, G=G: e.tensor_tensor(obb, s_[:, 0:G], xcs, ALU.mult), reads=[rs, rxc[ch]], writes=[rxr[1]])
                half = TPB // 2
                P.add("dve", lambda e, d=d: e.tensor_tensor_scan(H[d][:, 1:1 + half], A[:, 1:1 + half], B[:, 1:1 + half], 0.0, ALU.mult, ALU.add),
                      reads=[rxr[0], rxr[1]], writes=[rH[d]])
                P.add("dve", lambda e, d=d: e.tensor_tensor_scan(H[d][:, 1 + half:1 + TPB], A[:, 1 + half:1 + TPB], B[:, 1 + half:1 + TPB],
                                                                 H[d][:, half:half + 1], ALU.mult, ALU.add),
                      reads=[rxr[0], rxr[1], rH[d]], writes=[rH[d]])
            for (praw, ptok, L) in segs:
                P.add("dve", lambda e, ptok=ptok, L=L: e.tensor_tensor(H[0][:, 1 + ptok:1 + ptok + L], H[1][:, ptok + L:ptok:-1],
                                                                       H[0][:, 1 + ptok:1 + ptok + L], ALU.add),
                      reads=[rH[0], rH[1]], writes=[rH[0]])
            P.add("dve", lambda e, ch=ch: e.tensor_tensor(GB[ch][:], H[0][:, 1:1 + TPB], GB[ch][:], ALU.mult), reads=[rH[0], rgbuf[ch]], writes=[rgbuf[ch]])
            P.dma("sp", yrT[ch * 128:(ch + 1) * 128, base:base + TPB], GB[ch][:], reads=[rgbuf[ch]])

    for b in range(nbatch):
        do_batch(b)
    P.emit()
    return nc


AM2_POOL = "pool"
AM2_BLK = 4
AM2_LAG = 2
MG = 256
NCH = TPB // 128
DH = 256


def build_Am(nbatch, nc=None):
    NT = nbatch * TPB
    nc = nc or new_nc()
    uT = din(nc, "uTm", [D, NT], BF16)
    wq = din(nc, "wq", [D, 256], F32)
    wk = din(nc, "wk", [D, 256], F32)
    wvo = din(nc, "wvo", [D, 512], F32)
    wgt = din(nc, "wgt", [D, 4], F32)
    gb = din(nc, "gb", [128, 4], F32)
    cwd = din(nc, "cw", [128, 4, 4], F32)
    cbd = din(nc, "cb", [128, 4], F32)
    gn = din(nc, "gn", [128, 256], F32)
    ident = din(nc, "ident", [128, 128], BF16)
    tri = din(nc, "tri", [128, 2, 128], F32)
    negm = din(nc, "negm", [128, 2, 128], F32)
    ym = dout(nc, "ym", [NT, 256], BF16)
    P = Prog(nc)
    ps = Rot(P, 6, [128, 512], F32, psum=True)
    pt = Rot(P, 2, [128, 512], BF16, psum=True)
    tmp = Rot(P, 6, [128, MG + 3], F32)
    wqs = P.sbuf([128, KD, 256], BF16)
    wks = P.sbuf([128, KD, 256], BF16)
    wvos = P.sbuf([128, KD, 512], BF16)
    wgts = P.sbuf([128, KD, 4], BF16)
    r_w = Res()
    for dst, src in ((wqs, wq), (wks, wk), (wvos, wvo), (wgts, wgt)):
        P.dma("pool", dst[:], src.rearrange("(k p) c -> p k c", p=128), writes=[r_w])
    cst = {}
    r_c = Res()
    for name, src, shape, dt in (("gb", gb, [128, 4], F32), ("cw", cwd, [128, 4, 4], F32), ("cb", cbd, [128, 4], F32), ("gn", gn, [128, 256], F32),
                                 ("ident", ident, [128, 128], BF16), ("tri", tri, [128, 2, 128], F32), ("negm", negm, [128, 2, 128], F32)):
        cst[name] = P.sbuf(shape, dt)
        P.dma("sp", cst[name][:], src, writes=[r_c])
    ones = P.sbuf([128, 128], F32)
    P.add("dve", lambda e: e.memset(ones[:], 1.0), writes=[r_c])

    ub = Rot(P, 2, [128, KD, MG + 3], BF16)
    QK = [P.sbuf([128, TPB], BF16) for _ in range(4)]
    rqk = [[Res() for _ in range(NCH // 2)] for _ in range(4)]
    VA = P.sbuf([128, NCH, DH + 1], BF16)
    rva = [Res() for _ in range(NCH)]
    SO = P.sbuf([128, NCH, DH], BF16)
    rso = [Res() for _ in range(NCH)]
    GT = P.sbuf([128, NCH, 4], F32)
    rgt = Res()
    HF = P.sbuf([128, NCH, DH], F32)
    rhf = [Res() for _ in range(NCH)]
    C32 = [P.sbuf([128, DH + 1], F32) for _ in range(2)]
    CB = [P.sbuf([128, DH + 1], BF16) for _ in range(2)]
    rc32 = [Res(), Res()]
    rcb = [Res(), Res()]
    t128 = Rot(P, 8, [128, 128], F32)
    b128 = Rot(P, 8, [128, 128], BF16)
    kwr = Rot(P, 2, [128, DH], BF16)
    vsm = Rot(P, 16, [128, 8], F32)
    hsr = Rot(P, 3, [128, DH], F32)
    obr = Rot(P, 3, [128, DH], BF16)
    P.add("dve", lambda e: e.memset(VA[:, :, DH:DH + 1], 1.0), writes=rva)
    QSCALE = DH ** -0.5

    def do_group(base, g, t0, first, last):
        ubuf, rub = ub.next()
        hl = 0 if first else 2
        hr = 0 if last else 1
        if first:
            P.add("dve", lambda e: e.memset(ubuf[:, :, 0:2], 0.0), writes=[rub])
        if last:
            P.add("dve", lambda e: e.memset(ubuf[:, :, MG + 2:MG + 3], 0.0), writes=[rub])
        P.dma("sp", ubuf[:, :, 2 - hl:2 + MG + hr],
              uT[:, base + t0 - hl:base + t0 + MG + hr].rearrange("(k p) t -> p k t", p=128), writes=[rub])
        for ci in range(4):
            wsb = wqs if ci < 2 else wks
            c0 = (ci % 2) * 128
            pb, rp = ps.next()
            for k in range(KD):
                P.add("pe", lambda e, k=k, pb=pb, wsb=wsb, c0=c0: e.matmul(pb[:, 0:MG + 3], wsb[:, k, c0:c0 + 128], ubuf[:, k, :], start=(k == 0), stop=(k == KD - 1)),
                      reads=[r_w, rub], writes=[rp])
            cv, rcv = tmp.next()
            sg, rsg = tmp.next()
            P.add("dve", lambda e, pb=pb, cv=cv, ci=ci: e.tensor_scalar(cv[:, 0:MG], pb[:, 0:MG], cst["cw"][:, ci, 0:1], cst["cb"][:, ci:ci + 1], ALU.mult, ALU.add),
                  reads=[rp, r_c], writes=[rcv])
            for j in range(1, 4):
                P.add("dve", lambda e, pb=pb, cv=cv, ci=ci, j=j: e.scalar_tensor_tensor(cv[:, 0:MG], pb[:, j:j + MG], cst["cw"][:, ci, j:j + 1], cv[:, 0:MG], ALU.mult, ALU.add),
                      reads=[rp, r_c, rcv], writes=[rcv])
            P.add("act", lambda e, cv=cv, sg=sg: e.activation(sg[:, 0:MG], cv[:, 0:MG], AF.Sigmoid), reads=[rcv], writes=[rsg])
            sc = QSCALE if ci < 2 else 1.0
            P.add("dve", lambda e, cv=cv, sg=sg, ci=ci, sc=sc: e.scalar_tensor_tensor(QK[ci][:, t0:t0 + MG], cv[:, 0:MG], sc, sg[:, 0:MG], ALU.mult, ALU.mult),
                  reads=[rcv, rsg], writes=[rqk[ci][g]])
        for i in range(2):
            c = 2 * g + i
            pv, rpv = ps.next()
            pg, rpg = ps.next()
            for k in range(KD):
                P.add("pe", lambda e, k=k, pv=pv, i=i: e.matmul(pv[:, 0:512], ubuf[:, k, 2 + i * 128:2 + (i + 1) * 128], wvos[:, k, :], start=(k == 0), stop=(k == KD - 1)),
                      reads=[r_w, rub], writes=[rpv])
            for k in range(KD):
                P.add("pe", lambda e, k=k, pg=pg, i=i: e.matmul(pg[:, 0:4], ubuf[:, k, 2 + i * 128:2 + (i + 1) * 128], wgts[:, k, :], start=(k == 0), stop=(k == KD - 1)),
                      reads=[r_w, rub], writes=[rpg])
            P.add("act", lambda e, pv=pv, c=c: e.copy(VA[:, c, 0:DH], pv[:, 0:DH]), reads=[rpv], writes=[rva[c]])
            P.add("act", lambda e, pv=pv, c=c: e.activation(SO[:, c, :], pv[:, DH:2 * DH], AF.Sigmoid), reads=[rpv], writes=[rso[c]])
            P.add("dve", lambda e, pg=pg, c=c: e.tensor_tensor(GT[:, c, :], pg[:, 0:4], cst["gb"][:], ALU.add), reads=[rpg, r_c], writes=[rgt])

    def do_chunk(base, c, d, finalize):
        cs = slice(c * 128, (c + 1) * 128)
        g = c // 2
        ig = GT[:, c, 2 * d:2 * d + 1]
        lf = GT[:, c, 2 * d + 1:2 * d + 2]
        lastcol = 127 if d == 0 else 0
        LU, rLU = t128.next()
        P.add("dve", lambda e: e.tensor_scalar(LU[:], cst["tri"][:, d, :], lf, None, ALU.mult), reads=[r_c, rgt], writes=[rLU])
        prow, rprow = ps.next()
        P.add("pe", lambda e: e.matmul(prow[:, 0:128], ones[:], LU[:], start=True, stop=True), reads=[r_c, rLU], writes=[rprow])
        pcol, rpcol = ps.next()
        P.add("pe", lambda e: e.matmul(pcol[:, 0:1], cst["tri"][:, d, :], lf, start=True, stop=True), reads=[r_c, rgt], writes=[rpcol])
        sv, rsv = vsm.next()
        P.add("dve", lambda e: e.tensor_tensor(sv[:, 0:1], ig, pcol[:, 0:1], ALU.subtract), reads=[rgt, rpcol], writes=[rsv])
        P.add("dve", lambda e: e.tensor_copy(sv[:, 1:2], prow[:, lastcol:lastcol + 1]), reads=[rprow], writes=[rsv])
        DT, rDT = t128.next()
        P.add("dve", lambda e: e.tensor_tensor(DT[:], prow[:, 0:128], cst["negm"][:, d, :], ALU.add), reads=[rprow, r_c], writes=[rDT])
        P.add("act", lambda e: e.activation(DT[:], DT[:], AF.Exp, bias=sv[:, 0:1]), reads=[rDT, rsv], writes=[rDT])
        EB, rEB = t128.next()
        P.add("act", lambda e: e.activation(EB[:], prow[:, 0:128], AF.Exp), reads=[rprow], writes=[rEB])
        QS = []
        for i in range(2):
            qs, rqs = b128.next()
            P.add("dve", lambda e, i=i, qs=qs: e.tensor_tensor(qs[:], QK[i][:, cs], EB[:], ALU.mult), reads=[rqk[i][g], rEB], writes=[rqs])
            QS.append((qs, rqs))
        pst, rpst = ps.next()
        for i in range(2):
            P.add("pe", lambda e, i=i: e.matmul(pst[:, 0:128], QK[2 + i][:, cs], QK[i][:, cs], start=(i == 0), stop=(i == 1)),
                  reads=[rqk[2 + i][g], rqk[i][g]], writes=[rpst])
        ST, rST = b128.next()
        P.add("dve", lambda e: e.tensor_tensor(ST[:], pst[:, 0:128], DT[:], ALU.mult), reads=[rpst, rDT], writes=[rST])
        pnum, rpnum = ps.next()
        P.add("pe", lambda e: e.matmul(pnum[:, 0:DH + 1], ST[:], VA[:, c, :], start=True, stop=False), reads=[rST, rva[c]], writes=[rpnum])
        for i in range(2):
            P.add("pe", lambda e, i=i: e.matmul(pnum[:, 0:DH + 1], QS[i][0][:], CB[i][:], start=False, stop=(i == 1)),
                  reads=[QS[i][1], rcb[i]], writes=[rpnum])
        P.add("act", lambda e: e.activation(sv[:, 4:5], pnum[:, DH:DH + 1], AF.Abs), reads=[rpnum], writes=[rsv])
        P.add("dve", lambda e: e.tensor_scalar_max(sv[:, 4:5], sv[:, 4:5], 1.0), reads=[rsv], writes=[rsv])
        P.add("dve", lambda e: e.reciprocal(sv[:, 5:6], sv[:, 4:5]), reads=[rsv], writes=[rsv])
        if not finalize:
            P.add("act", lambda e: e.activation(HF[:, c, :], pnum[:, 0:DH], AF.Copy, scale=sv[:, 5:6]), reads=[rpnum, rsv], writes=[rhf[c]])
        else:
            hs, rhs = hsr.next()
            P.add("dve", lambda e: e.scalar_tensor_tensor(hs[:], pnum[:, 0:DH], sv[:, 5:6], HF[:, c, :], ALU.mult, ALU.add), reads=[rpnum, rsv, rhf[c]], writes=[rhs])
            st6, rst6 = vsm.next()
            P.add("dve", lambda e: e.bn_stats(st6[:, 0:6], hs[:]), reads=[rhs], writes=[rst6])
            mv, rmv = vsm.next()
            P.add("dve", lambda e: e.bn_aggr(mv[:, 0:2], st6[:, 0:6]), reads=[rst6], writes=[rmv])
            P.add("act", lambda e: e.activation(mv[:, 2:3], mv[:, 1:2], AF.Sqrt, bias=LN_EPS, scale=1.0), reads=[rmv], writes=[rmv])
            P.add("dve", lambda e: e.reciprocal(mv[:, 2:3], mv[:, 2:3]), reads=[rmv], writes=[rmv])
            P.add("dve", lambda e: e.tensor_scalar(hs[:], hs[:], mv[:, 0:1], mv[:, 2:3], ALU.subtract, ALU.mult), reads=[rhs, rmv], writes=[rhs])
            P.add("dve", lambda e: e.tensor_tensor(hs[:], hs[:], cst["gn"][:], ALU.mult), reads=[rhs, r_c], writes=[rhs])
            ob, rob = obr.next()
            P.add("dve", lambda e: e.tensor_tensor(ob[:], hs[:], SO[:, c, :], ALU.mult), reads=[rhs, rso[c]], writes=[rob])
            P.dma("sp", ym[base + c * 128:base + (c + 1) * 128, :], ob[:], reads=[rob])
        P.add("act", lambda e: e.activation(sv[:, 2:3], sv[:, 0:1], AF.Exp, bias=sv[:, 1:2]), reads=[rsv], writes=[rsv])
        P.add("act", lambda e: e.activation(sv[:, 3:4], sv[:, 1:2], AF.Exp), reads=[rsv], writes=[rsv])
        kw, rkw = kwr.next()
        for i in range(2):
            ptb, rpt = pt.next()
            P.add("pe", lambda e, i=i, ptb=ptb: e.transpose(ptb[:, 0:128], QK[2 + i][:, cs], cst["ident"][:]), reads=[rqk[2 + i][g], r_c], writes=[rpt])
            P.add("dve", lambda e, i=i, ptb=ptb: e.tensor_scalar(kw[:, i * 128:(i + 1) * 128], ptb[:, 0:128], sv[:, 2:3], None, ALU.mult), reads=[rpt, rsv], writes=[rkw])
        pcus = [ps.next(), ps.next()]
        for i in range(2):
            P.add("pe", lambda e, i=i: e.matmul(pcus[i][0][:, 0:DH + 1], kw[:, i * 128:(i + 1) * 128], VA[:, c, :], start=True, stop=True),
                  reads=[rkw, rva[c]], writes=[pcus[i][1]])
        for i in range(2):
            P.add("dve", lambda e, i=i: e.scalar_tensor_tensor(C32[i][:], C32[i][:], sv[:, 3:4], pcus[i][0][:, 0:DH + 1], ALU.mult, ALU.add),
                  reads=[rc32[i], rsv, pcus[i][1]], writes=[rc32[i]])
            P.add("act", lambda e, i=i: e.copy(CB[i][:], C32[i][:]), reads=[rc32[i]], writes=[rcb[i]])

    def zero_state():
        for i in range(2):
            P.add("dve", lambda e, i=i: e.memset(C32[i][:], 0.0), writes=[rc32[i]])
            P.add("dve", lambda e, i=i: e.memset(CB[i][:], 0.0), writes=[rcb[i]])

    def do_batch(b):
        base = b * TPB
        ngrp = TPB // MG
        for g in range(ngrp):
            first = g in (0, 1)
            last = g in (0, ngrp - 1)
            do_group(base, g, g * MG, first, last)
        for col in (1, 3):
            P.add("act", lambda e, col=col: e.activation(GT[:, :, col], GT[:, :, col], AF.Exp, scale=-1.0), reads=[rgt], writes=[rgt])
            P.add("act", lambda e, col=col: e.activation(GT[:, :, col], GT[:, :, col], AF.Ln, bias=1.0), reads=[rgt], writes=[rgt])
            P.add("dve", lambda e, col=col: e.tensor_scalar_mul(GT[:, :, col], GT[:, :, col], -1.0), reads=[rgt], writes=[rgt])
        zero_state()
        for c in range(NCH):
            do_chunk(base, c, 0, False)
        zero_state()
        for c in [1, 0] + list(range(NCH - 1, 1, -1)):
            do_chunk(base, c, 1, True)

    for b in range(nbatch):
        do_batch(b)
    P.emit()
    return nc


def build_Am2(nbatch, nc=None):
    NT = nbatch * TPB
    nc = nc or new_nc()
    uT = din(nc, "uTm", [D, NT], BF16)
    wq = din(nc, "wq", [D, 256], F32)
    wk = din(nc, "wk", [D, 256], F32)
    wvo = din(nc, "wvo", [D, 512], F32)
    wgt = din(nc, "wgt", [D, 4], F32)
    gb = din(nc, "gb", [128, 4], F32)
    cwd = din(nc, "cw", [128, 4, 4], F32)
    cbd = din(nc, "cb", [128, 4], F32)
    gn = din(nc, "gn", [128, 256], F32)
    ident = din(nc, "ident", [128, 128], BF16)
    tri = din(nc, "tri", [128, 2, 128], F32)
    negm = din(nc, "negm", [128, 2, 128], F32)
    ym = dout(nc, "ym", [NT, 256], BF16)
    P = Prog(nc)
    banks = [P.psum([128, 512], F32) for _ in range(7)]
    rbk = [Res(excl=True) for _ in range(7)]
    tbank = P.psum([128, 1024], BF16)
    rtb = Res(excl=True)
    ps = Rot(P, 0, None, None)
    ps.bufs, ps.res, ps.n = banks[0:6], rbk[0:6], 6
    tmp = Rot(P, 6, [128, MG + 3], F32)
    wqs = P.sbuf([128, KD, 256], BF16)
    wks = P.sbuf([128, KD, 256], BF16)
    wvos = P.sbuf([128, KD, 512], BF16)
    wgts = P.sbuf([128, KD, 4], BF16)
    r_w = Res()
    for dst, src in ((wqs, wq), (wks, wk), (wvos, wvo), (wgts, wgt)):
        P.dma("pool", dst[:], src.rearrange("(k p) c -> p k c", p=128), writes=[r_w])
    cst = {}
    r_c = Res()
    for name, src, shape, dt in (("gb", gb, [128, 4], F32), ("cw", cwd, [128, 4, 4], F32), ("cb", cbd, [128, 4], F32), ("gn", gn, [128, 256], F32),
                                 ("ident", ident, [128, 128], BF16), ("tri", tri, [128, 2, 128], F32), ("negm", negm, [128, 2, 128], F32)):
        cst[name] = P.sbuf(shape, dt)
        P.dma("sp", cst[name][:], src, writes=[r_c])
    ones = P.sbuf([128, 128], F32)
    P.add("dve", lambda e: e.memset(ones[:], 1.0), writes=[r_c])

    ub = Rot(P, 2, [128, KD, MG + 3], BF16)
    QK = [P.sbuf([128, TPB], BF16) for _ in range(4)]
    rqk = [[Res() for _ in range(NCH // 2)] for _ in range(4)]
    VA = P.sbuf([128, NCH, DH + 1], BF16)
    rva = [Res() for _ in range(NCH)]
    SO = P.sbuf([128, NCH, DH], BF16)
    rso = [Res() for _ in range(NCH)]
    GT = P.sbuf([128, NCH, 4], F32)
    rgt = Res()
    HF = P.sbuf([128, NCH, DH], F32)
    rhf = [Res() for _ in range(NCH)]
    C32 = P.sbuf([128, 2, DH + 1], F32)
    rc32 = Res()
    NS = 8
    LUa = P.sbuf([128, NS, 128], F32)
    PRa = P.sbuf([128, NS, 128], F32)
    rPR = [Res() for _ in range(NS)]
    DTa = P.sbuf([128, NS, 128], F32)
    EBa = P.sbuf([128, NS, 128], F32)
    QSa = P.sbuf([128, NS, 2, 128], BF16)
    STa = P.sbuf([128, NS, 128], BF16)
    KWa = P.sbuf([128, NS, DH], BF16)
    SVa = P.sbuf([128, NS, 8], F32)
    rLU = [Res() for _ in range(NS)]
    rDT = [Res() for _ in range(NS)]
    rEB = [Res() for _ in range(NS)]
    rQS = [Res() for _ in range(NS)]
    rST = [Res() for _ in range(NS)]
    rKW = [Res() for _ in range(NS)]
    rSV = [Res() for _ in range(NS)]
    CUs = Rot(P, 4, [128, 2, DH + 1], F32)
    CBr = [P.sbuf([128, 2, DH + 16], BF16) for _ in range(4)]
    rCBr = [Res() for _ in range(4)]
    vsm = Rot(P, 16, [128, 8], F32)
    hsr = Rot(P, 3, [128, DH], F32)
    obr = Rot(P, 3, [128, DH], BF16)
    P.add("dve", lambda e: e.memset(VA[:, :, DH:DH + 1], 1.0), writes=rva)
    QSCALE = DH ** -0.5

    def do_group(base, g, t0, first, last):
        ubuf, rub = ub.next()
        hl = 0 if first else 2
        hr = 0 if last else 1
        if first:
            P.add("dve", lambda e: e.memset(ubuf[:, :, 0:2], 0.0), writes=[rub])
        if last:
            P.add("dve", lambda e: e.memset(ubuf[:, :, MG + 2:MG + 3], 0.0), writes=[rub])
        P.dma("sp", ubuf[:, :, 2 - hl:2 + MG + hr],
              uT[:, base + t0 - hl:base + t0 + MG + hr].rearrange("(k p) t -> p k t", p=128), writes=[rub])
        for ci in range(4):
            wsb = wqs if ci < 2 else wks
            c0 = (ci % 2) * 128
            pb, rp = ps.next()
            for k in range(KD):
                P.add("pe", lambda e, k=k, pb=pb, wsb=wsb, c0=c0: e.matmul(pb[:, 0:MG + 3], wsb[:, k, c0:c0 + 128], ubuf[:, k, :], start=(k == 0), stop=(k == KD - 1)),
                      reads=[r_w, rub], writes=[rp])
            cv, rcv = tmp.next()
            sg, rsg = tmp.next()
            P.add("dve", lambda e, pb=pb, cv=cv, ci=ci: e.tensor_scalar(cv[:, 0:MG], pb[:, 0:MG], cst["cw"][:, ci, 0:1], cst["cb"][:, ci:ci + 1], ALU.mult, ALU.add),
                  reads=[rp, r_c], writes=[rcv])
            for j in range(1, 4):
                P.add("dve", lambda e, pb=pb, cv=cv, ci=ci, j=j: e.scalar_tensor_tensor(cv[:, 0:MG], pb[:, j:j + MG], cst["cw"][:, ci, j:j + 1], cv[:, 0:MG], ALU.mult, ALU.add),
                      reads=[rp, r_c, rcv], writes=[rcv])
            P.add("act", lambda e, cv=cv, sg=sg: e.activation(sg[:, 0:MG], cv[:, 0:MG], AF.Sigmoid), reads=[rcv], writes=[rsg])
            sc = QSCALE if ci < 2 else 1.0
            P.add("dve", lambda e, cv=cv, sg=sg, ci=ci, sc=sc: e.scalar_tensor_tensor(QK[ci][:, t0:t0 + MG], cv[:, 0:MG], sc, sg[:, 0:MG], ALU.mult, ALU.mult),
                  reads=[rcv, rsg], writes=[rqk[ci][g]])
        for i in range(2):
            c = 2 * g + i
            pv, rpv = ps.next()
            pg, rpg = ps.next()
            for k in range(KD):
                P.add("pe", lambda e, k=k, pv=pv, i=i: e.matmul(pv[:, 0:512], ubuf[:, k, 2 + i * 128:2 + (i + 1) * 128], wvos[:, k, :], start=(k == 0), stop=(k == KD - 1)),
                      reads=[r_w, rub], writes=[rpv])
            for k in range(KD):
                P.add("pe", lambda e, k=k, pg=pg, i=i: e.matmul(pg[:, 0:4], ubuf[:, k, 2 + i * 128:2 + (i + 1) * 128], wgts[:, k, :], start=(k == 0), stop=(k == KD - 1)),
                      reads=[r_w, rub], writes=[rpg])
            P.add("act", lambda e, pv=pv, c=c: e.copy(VA[:, c, 0:DH], pv[:, 0:DH]), reads=[rpv], writes=[rva[c]])
            P.add("act", lambda e, pv=pv, c=c: e.activation(SO[:, c, :], pv[:, DH:2 * DH], AF.Sigmoid), reads=[rpv], writes=[rso[c]])
            P.add("dve", lambda e, pg=pg, c=c: e.tensor_tensor(GT[:, c, :], pg[:, 0:4], cst["gb"][:], ALU.add), reads=[rpg, r_c], writes=[rgt])

    def block_X(base, chunks, d, par):
        lastcol = 127 if d == 0 else 0
        sl = [par * 4 + i for i in range(len(chunks))]

        def part0():
            for i, c in enumerate(chunks):
                P.add("dve", lambda e, i=i, c=c: e.tensor_scalar(LUa[:, sl[i], :], cst["tri"][:, d, :], GT[:, c, 2 * d + 1:2 * d + 2], None, ALU.mult),
                      reads=[r_c, rgt], writes=[rLU[sl[i]]])

        def part1():
            for i, c in enumerate(chunks):
                P.add("pe", lambda e, i=i: e.matmul(banks[0][:, i * 128:(i + 1) * 128], ones[:], LUa[:, sl[i], :], start=True, stop=True),
                      reads=[r_c, rLU[sl[i]]], writes=[rbk[0]])
                P.add("pe", lambda e, i=i, c=c: e.matmul(banks[2][:, 16 * i:16 * i + 1], cst["tri"][:, d, :], GT[:, c, 2 * d + 1:2 * d + 2], start=True, stop=True),
                      reads=[r_c, rgt], writes=[rbk[2]])

        def part2():
            for i, c in enumerate(chunks):
                P.add("act", lambda e, i=i: e.copy(PRa[:, sl[i], :], banks[0][:, i * 128:(i + 1) * 128]), reads=[rbk[0]], writes=[rPR[sl[i]]])
            for i, c in enumerate(chunks):
                sv = SVa[:, sl[i], :]
                P.add("dve", lambda e, i=i, c=c, sv=sv: e.tensor_tensor(sv[:, 0:1], GT[:, c, 2 * d:2 * d + 1], banks[2][:, 16 * i:16 * i + 1], ALU.subtract),
                      reads=[rgt, rbk[2]], writes=[rSV[sl[i]]])
                P.add("dve", lambda e, i=i: e.tensor_tensor(DTa[:, sl[i], :], PRa[:, sl[i], :], cst["negm"][:, d, :], ALU.add),
                      reads=[rPR[sl[i]], r_c], writes=[rDT[sl[i]]])
            for i, c in enumerate(chunks):
                sv = SVa[:, sl[i], :]
                P.add("act", lambda e, i=i, sv=sv: e.activation(DTa[:, sl[i], :], DTa[:, sl[i], :], AF.Exp, bias=sv[:, 0:1]), reads=[rDT[sl[i]], rSV[sl[i]]], writes=[rDT[sl[i]]])
                P.add("act", lambda e, i=i: e.activation(EBa[:, sl[i], :], PRa[:, sl[i], :], AF.Exp), reads=[rPR[sl[i]]], writes=[rEB[sl[i]]])
                P.add("act", lambda e, i=i, sv=sv: e.activation(sv[:, 2:3], sv[:, 0:1], AF.Exp, bias=PRa[:, sl[i], lastcol:lastcol + 1]), reads=[rSV[sl[i]], rPR[sl[i]]], writes=[rSV[sl[i]]])
                P.add("act", lambda e, i=i, sv=sv: e.activation(sv[:, 3:4], PRa[:, sl[i], lastcol:lastcol + 1], AF.Exp), reads=[rPR[sl[i]]], writes=[rSV[sl[i]]])

        def part3():
            for i, c in enumerate(chunks):
                cs = slice(c * 128, (c + 1) * 128)
                for h in range(2):
                    P.add(AM2_POOL, lambda e, i=i, h=h, cs=cs: e.tensor_tensor(QSa[:, sl[i], h, :], QK[h][:, cs], EBa[:, sl[i], :], ALU.mult),
                          reads=[rqk[h][c // 2], rEB[sl[i]]], writes=[rQS[sl[i]]])
            for i, c in enumerate(chunks):
                cs = slice(c * 128, (c + 1) * 128)
                for h in range(2):
                    P.add("pe", lambda e, i=i, h=h, cs=cs: e.matmul(banks[1][:, i * 128:(i + 1) * 128], QK[2 + h][:, cs], QK[h][:, cs], start=(h == 0), stop=(h == 1)),
                          reads=[rqk[2 + h][c // 2], rqk[h][c // 2]], writes=[rbk[1]])
                for h in range(2):
                    P.add("pe", lambda e, i=i, h=h, cs=cs: e.transpose(tbank[:, (2 * i + h) * 128:(2 * i + h + 1) * 128], QK[2 + h][:, cs], cst["ident"][:]),
                          reads=[rqk[2 + h][c // 2], r_c], writes=[rtb])
            for i, c in enumerate(chunks):
                sv = SVa[:, sl[i], :]
                P.add("dve", lambda e, i=i: e.tensor_tensor(STa[:, sl[i], :], banks[1][:, i * 128:(i + 1) * 128], DTa[:, sl[i], :], ALU.mult),
                      reads=[rbk[1], rDT[sl[i]]], writes=[rST[sl[i]]])
                P.add("dve", lambda e, i=i, sv=sv: e.tensor_scalar(KWa[:, sl[i], :], tbank[:, 2 * i * 128:(2 * i + 2) * 128], sv[:, 2:3], None, ALU.mult),
                      reads=[rtb, rSV[sl[i]]], writes=[rKW[sl[i]]])

        return [part0, part1, part2, part3]

    def block_YA(base, chunks, d, finalize, par, n0, i):
        sl = [par * 4 + t for t in range(len(chunks))]
        c = chunks[i]
        n = n0 + i
        sv = SVa[:, sl[i], :]
        cu, rcu = CUs.next()
        for h in range(2):
            P.add("pe", lambda e, i=i, h=h, c=c: e.matmul(banks[5 + h][:, 0:DH + 1], KWa[:, sl[i], h * 128:(h + 1) * 128], VA[:, c, :], start=True, stop=True),
                  reads=[rKW[sl[i]], rva[c]], writes=[rbk[5 + h]])
        for h in range(2):
            P.add("act", lambda e, h=h, cu=cu: e.copy(cu[:, h, :], banks[5 + h][:, 0:DH + 1]), reads=[rbk[5 + h]], writes=[rcu])
        P.add("dve", lambda e, cu=cu, sv=sv: e.scalar_tensor_tensor(C32[:], C32[:], sv[:, 3:4], cu[:], ALU.mult, ALU.add),
              reads=[rc32, rSV[sl[i]], rcu], writes=[rc32])
        P.add("act", lambda e, n=n: e.copy(CBr[(n + 1) % 4][:, :, 0:DH + 1], C32[:]), reads=[rc32], writes=[rCBr[(n + 1) % 4]])

    def block_YB(base, chunks, d, finalize, par, n0, i):
        sl = [par * 4 + t for t in range(len(chunks))]
        c = chunks[i]
        n = n0 + i
        sv = SVa[:, sl[i], :]
        pn = banks[3 + (n % 2)]
        rpn = rbk[3 + (n % 2)]
        P.add("pe", lambda e, i=i, c=c, pn=pn: e.matmul(pn[:, 0:DH + 1], STa[:, sl[i], :], VA[:, c, :], start=True, stop=False), reads=[rST[sl[i]], rva[c]], writes=[rpn])
        for h in range(2):
            P.add("pe", lambda e, i=i, h=h, pn=pn, n=n: e.matmul(pn[:, 0:DH + 1], QSa[:, sl[i], h, :], CBr[n % 4][:, h, 0:DH + 1], start=False, stop=(h == 1)),
                  reads=[rQS[sl[i]], rCBr[n % 4]], writes=[rpn])
        dn, rdn = vsm.next()
        P.add("act", lambda e, pn=pn, dn=dn: e.activation(dn[:, 0:1], pn[:, DH:DH + 1], AF.Abs), reads=[rpn], writes=[rdn])
        P.add("dve", lambda e, dn=dn: e.tensor_scalar_max(dn[:, 0:1], dn[:, 0:1], 1.0), reads=[rdn], writes=[rdn])
        P.add("dve", lambda e, dn=dn: e.reciprocal(dn[:, 1:2], dn[:, 0:1]), reads=[rdn], writes=[rdn])
        if not finalize:
            P.add("act", lambda e, pn=pn, dn=dn, c=c: e.activation(HF[:, c, :], pn[:, 0:DH], AF.Copy, scale=dn[:, 1:2]), reads=[rpn, rdn], writes=[rhf[c]])
        else:
            hs, rhs = hsr.next()
            P.add("dve", lambda e, pn=pn, dn=dn, c=c, hs=hs: e.scalar_tensor_tensor(hs[:], pn[:, 0:DH], dn[:, 1:2], HF[:, c, :], ALU.mult, ALU.add),
                  reads=[rpn, rdn, rhf[c]], writes=[rhs])
            P.add("dve", lambda e, dn=dn, hs=hs: e.bn_stats(dn[:, 2:8], hs[:]), reads=[rhs], writes=[rdn])
            mv, rmv = vsm.next()
            P.add("dve", lambda e, dn=dn, mv=mv: e.bn_aggr(mv[:, 0:2], dn[:, 2:8]), reads=[rdn], writes=[rmv])
            P.add("act", lambda e, mv=mv: e.activation(mv[:, 2:3], mv[:, 1:2], AF.Sqrt, bias=LN_EPS, scale=1.0), reads=[rmv], writes=[rmv])
            P.add("dve", lambda e, mv=mv: e.reciprocal(mv[:, 2:3], mv[:, 2:3]), reads=[rmv], writes=[rmv])
            P.add("dve", lambda e, mv=mv, hs=hs: e.tensor_scalar(hs[:], hs[:], mv[:, 0:1], mv[:, 2:3], ALU.subtract, ALU.mult), reads=[rhs, rmv], writes=[rhs])
            P.add(AM2_POOL, lambda e, hs=hs: e.tensor_tensor(hs[:], hs[:], cst["gn"][:], ALU.mult), reads=[rhs, r_c], writes=[rhs])
            ob, rob = obr.next()
            P.add(AM2_POOL, lambda e, hs=hs, ob=ob, c=c: e.tensor_tensor(ob[:], hs[:], SO[:, c, :], ALU.mult), reads=[rhs, rso[c]], writes=[rob])
            P.dma("sp", ym[base + c * 128:base + (c + 1) * 128, :], ob[:], reads=[rob])


    def zero_state():
        P.add("dve", lambda e: e.memset(C32[:], 0.0), writes=[rc32])
        P.add("dve", lambda e: e.memset(CBr[0][:], 0.0), writes=[rCBr[0]])

    blk_counter = [0]

    def do_dir(base, order, d, finalize):
        zero_state()
        blocks = [order[s0:s0 + AM2_BLK] for s0 in range(0, len(order), AM2_BLK)]
        par0 = blk_counter[0]
        for p in block_X(base, blocks[0], d, par0 % 2):
            p()
        pending = []
        for k, chunks in enumerate(blocks):
            nxt = block_X(base, blocks[k + 1], d, (par0 + k + 1) % 2) if k + 1 < len(blocks) else []
            for t in range(max(4, len(chunks))):
                if t < len(nxt):
                    nxt[t]()
                if t < len(chunks):
                    args = (base, chunks, d, finalize, (par0 + k) % 2, k * AM2_BLK, t)
                    block_YA(*args)
                    pending.append(args)
                    if len(pending) > AM2_LAG:
                        block_YB(*pending.pop(0))
        while pending:
            block_YB(*pending.pop(0))
        blk_counter[0] += len(blocks)

    def do_batch(b):
        base = b * TPB
        ngrp = TPB // MG
        for g in range(ngrp):
            first = g in (0, 1)
            last = g in (0, ngrp - 1)
            do_group(base, g, g * MG, first, last)
        for col in (1, 3):
            P.add("act", lambda e, col=col: e.activation(GT[:, :, col], GT[:, :, col], AF.Exp, scale=-1.0), reads=[rgt], writes=[rgt])
            P.add("act", lambda e, col=col: e.activation(GT[:, :, col], GT[:, :, col], AF.Ln, bias=1.0), reads=[rgt], writes=[rgt])
            P.add("dve", lambda e, col=col: e.tensor_scalar_mul(GT[:, :, col], GT[:, :, col], -1.0), reads=[rgt], writes=[rgt])
        do_dir(base, list(range(NCH)), 0, False)
        do_dir(base, [1, 0] + list(range(NCH - 1, 1, -1)), 1, True)

    for b in range(nbatch):
        do_batch(b)
    P.emit()
    return nc


def build_A(nbatch):
    nc = new_nc()
    build_Arg(nbatch, nc)
    nc.all_engine_barrier()
    build_Am2(nbatch, nc)
    return nc


_PROGS = {}


def _prog(key, builder):
    if key not in _PROGS:
        _PROGS[key] = builder()
    return _PROGS[key]


def _run(nc, in_maps, tag=""):
    r = run_bass_kernel_spmd(nc, in_maps, core_ids=list(range(NCORE)))
    if getattr(r, "exec_time_ns", None):
        print(f"[kernel] launch {tag} exec_time_ns={r.exec_time_ns}", flush=True)
    return r.results


def _vec_pack(vecs):
    return np.ascontiguousarray(np.asarray(vecs, np.float32).reshape(16, KD, 128).transpose(2, 0, 1))


def _cm_perm():
    t = np.arange(SEQ)
    return (t % GRID) * GRID + (t // GRID)


def _core_tokens(b, half, with_ctx):
    lat = CTX + np.arange(half * (SEQ // 2), (half + 1) * (SEQ // 2))
    if with_ctx:
        return np.concatenate([np.arange(half * (CTX // 2), (half + 1) * (CTX // 2)), lat])
    return lat


def kernel(x, c, ctx, c_ctx, w_mod, b_mod, w_in, conv_rg_w, conv_rg_b, conv_m_w, conv_m_b, rg_wa, rg_ba,
           rg_wx, rg_bx, rg_lam, m_gate_b, m_gn_g, p_rg, p_m, w_out, ln_g, ln_b, ff_w1, ff_w3, ff_w2,
           router_w, router_b, ex_w1, ex_w3, ex_w2):
    f32 = lambda a: np.asarray(a, np.float32)
    x, c, ctx, c_ctx = f32(x), f32(c), f32(ctx), f32(c_ctx)
    w_mod, b_mod, w_in = f32(w_mod), f32(b_mod), f32(w_in)
    ncores = NCORE
    cores = [(j // 2, j % 2) for j in range(ncores)]

    cT = np.ascontiguousarray(np.concatenate([c, c_ctx[None]], 0).T)
    maps = []
    for j in range(ncores):
        l, q = divmod(j, 4)
        cs = slice(q * MOD_COLS, (q + 1) * MOD_COLS)
        maps.append({"cT": cT, "wm": np.ascontiguousarray(w_mod[l][:, cs]),
                     "bm": np.ascontiguousarray(np.broadcast_to(b_mod[l][cs], (5, MOD_COLS)))})
    res = _run(_prog("Kmod", build_Kmod), maps, "Kmod")
    mod = [np.concatenate([res[l * 4 + q]["mod"] for q in range(4)], 1).reshape(5, 6, D) for l in range(2)]
    SH1, SC1, G1, SH2, SC2, G2 = range(6)

    def vecs_for(l, b, nxt):
        v = np.zeros((16, D), np.float32)
        for kind, row in ((0, b), (1, 4)):
            o = kind * 6
            v[o + V_G1] = mod[l][row, G1]
            v[o + V_SC2] = mod[l][row, SC2]
            v[o + V_SH2] = mod[l][row, SH2]
            v[o + V_G2] = mod[l][row, G2]
            if nxt is not None:
                v[o + V_NSC1] = mod[nxt][row, SC1]
                v[o + V_NSH1] = mod[nxt][row, SH1]
        v[V_LN + 0], v[V_LN + 1] = ln_g[l, 0], ln_b[l, 0]
        v[V_LN + 2], v[V_LN + 3] = ln_g[l, 1], ln_b[l, 1]
        return _vec_pack(v)

    hfull = [np.concatenate([ctx[b], x[b]], 0) for b in range(NB)]

    groups0 = [(0, 128, 1)] + [(128 + 512 * i, 512, 0) for i in range(4)]
    T0 = 128 + 2048
    hT_core = []
    maps = []
    for (b, half) in cores:
        hT = np.ascontiguousarray(hfull[b][_core_tokens(b, half, True)].T)
        hT_core.append(hT)
        maps.append({"hT": hT, "vec": vecs_for(0, b, 0)})
    res = _run(_prog("Kln", lambda: build_Kln(groups0, T0)), maps, "Kln")
    u_core = [r["uT_out"] for r in res]

    perm = _cm_perm()
    tri_s = np.arange(128)
    U = (tri_s[:, None] <= tri_s[None, :]).astype(np.float32)
    Lo = (tri_s[:, None] >= tri_s[None, :]).astype(np.float32)
    tri = np.ascontiguousarray(np.stack([U, Lo], 1))
    negm = np.ascontiguousarray(((tri - 1.0) * 30000.0).astype(np.float32))
    ident = np.eye(128).astype(NPBF)

    def assemble_u(u_core):
        uT = np.empty((D, NB * TPB), NPBF)
        for j, (b, half) in enumerate(cores):
            uT[:, b * TPB + _core_tokens(b, half, True)] = u_core[j]
        uTm = uT.copy()
        for b in range(NB):
            lat = uT[:, b * TPB + CTX:(b + 1) * TPB]
            uTm[:, b * TPB + CTX:(b + 1) * TPB] = lat[:, perm]
        return uT, uTm

    def mixer(l, uT, uTm):
        wl = w_in[l]
        maps = []
        for j in range(ncores):
            cs = slice(j * 256, (j + 1) * 256)
            rgw = np.zeros((128, 8, 128), np.float32)
            rgb = np.zeros((128, 8), np.float32)
            lm = np.zeros((128, 4), np.float32)
            for d in range(2):
                for ch in range(2):
                    ia = (d * 2 + ch) * 2
                    blk = 2 * j + ch
                    rgw[:, ia, :] = rg_wa[l, d, blk]
                    rgw[:, ia + 1, :] = rg_wx[l, d, blk]
                    rgb[:, ia] = rg_ba[l, d, blk * 128:(blk + 1) * 128]
                    rgb[:, ia + 1] = rg_bx[l, d, blk * 128:(blk + 1) * 128]
                    lm[:, d * 2 + ch] = rg_lam[l, d, blk * 128:(blk + 1) * 128]
            maps.append({"uT": uT, "wx": np.ascontiguousarray(wl[:, cs]), "wg": np.ascontiguousarray(wl[:, C_RGG + j * 256:C_RGG + (j + 1) * 256]),
                         "rcw": np.ascontiguousarray(f32(conv_rg_w[l])[:, cs].reshape(4, 2, 128).transpose(2, 1, 0)),
                         "rcb": np.ascontiguousarray(f32(conv_rg_b[l])[cs].reshape(2, 128).T),
                         "rgw": rgw, "rgb": rgb, "lam": lm})
        for j in range(ncores):
            qs = slice(C_M + j * 256, C_M + (j + 1) * 256)
            ks = slice(C_M + W_M + j * 256, C_M + W_M + (j + 1) * 256)
            vs = slice(C_M + 2 * W_M + j * 256, C_M + 2 * W_M + (j + 1) * 256)
            os_ = slice(C_M + 3 * W_M + j * 256, C_M + 3 * W_M + (j + 1) * 256)
            gcols = [C_M + 4 * W_M + d * 16 + g * 8 + j for d in range(2) for g in range(2)]
            gbv = np.array([m_gate_b[l][d, g, j] for d in range(2) for g in range(2)], np.float32)
            cwm = f32(conv_m_w[l])
            cbm = f32(conv_m_b[l])
            cwj = np.concatenate([cwm[:, j * 256:(j + 1) * 256], cwm[:, W_M + j * 256:W_M + (j + 1) * 256]], 1)
            cbj = np.concatenate([cbm[j * 256:(j + 1) * 256], cbm[W_M + j * 256:W_M + (j + 1) * 256]])
            maps[j].update({"uTm": uTm, "wq": np.ascontiguousarray(wl[:, qs]), "wk": np.ascontiguousarray(wl[:, ks]),
                         "wvo": np.ascontiguousarray(np.concatenate([wl[:, vs], wl[:, os_]], 1)),
                         "wgt": np.ascontiguousarray(wl[:, gcols]),
                         "gb": np.ascontiguousarray(np.broadcast_to(gbv, (128, 4))),
                         "cw": np.ascontiguousarray(cwj.reshape(4, 4, 128).transpose(2, 1, 0)),
                         "cb": np.ascontiguousarray(cbj.reshape(4, 128).T),
                         "gn": np.ascontiguousarray(np.broadcast_to(f32(m_gn_g[l])[j * 256:(j + 1) * 256], (128, 256))),
                         "ident": ident, "tri": tri, "negm": negm})
        res = _run(_prog("A", lambda: build_A(NB)), maps, "A")
        yrT = np.concatenate([r["yrT"] for r in res], 0)
        ymT = np.empty((D, NB * TPB), NPBF)
        for j in range(ncores):
            ymj = res[j]["ym"]
            for b in range(NB):
                blk = ymj[b * TPB:(b + 1) * TPB]
                ymT[j * 256:(j + 1) * 256, b * TPB:b * TPB + CTX] = blk[:CTX].T
                lat = np.empty((SEQ, 256), NPBF)
                lat[perm] = blk[CTX:]
                ymT[j * 256:(j + 1) * 256, b * TPB + CTX:(b + 1) * TPB] = lat.T
        return yrT, ymT

    uT, uTm = assemble_u(u_core)
    yrT, ymT = mixer(0, uT, uTm)
    w_g0 = np.ascontiguousarray(w_in[0][:, C_G:])
    maps = []
    for j, (b, half) in enumerate(cores):
        tk = b * TPB + _core_tokens(b, half, True)
        maps.append({"yrT": np.ascontiguousarray(yrT[:, tk]), "ymT": np.ascontiguousarray(ymT[:, tk]), "uT": u_core[j], "hT": hT_core[j],
                     "w_g": w_g0, "p_rg": f32(p_rg[0]), "p_m": f32(p_m[0]), "w_out": f32(w_out[0]), "vec": vecs_for(0, b, 1),
                     "w1": f32(ff_w1[0]), "w3": f32(ff_w3[0]), "w2": f32(ff_w2[0])})
    res = _run(_prog("B0", lambda: build_B(groups0, T0, True, False)), maps, "B0")
    h1_core = [r["hT_out"] for r in res]
    u1_core = [r["uT_out"] for r in res]

    uT, uTm = assemble_u(u1_core)
    yrT, ymT = mixer(1, uT, uTm)
    groups1 = [(512 * i, 512, 0) for i in range(4)]
    T1 = 2048
    w_g1 = np.ascontiguousarray(w_in[1][:, C_G:])
    rb = np.ascontiguousarray(np.broadcast_to(f32(router_b[0]), (128, 8)))
    maps = []
    for j, (b, half) in enumerate(cores):
        tk = b * TPB + _core_tokens(b, half, False)
        maps.append({"yrT": np.ascontiguousarray(yrT[:, tk]), "ymT": np.ascontiguousarray(ymT[:, tk]),
                     "uT": np.ascontiguousarray(u1_core[j][:, 128:]), "hT": np.ascontiguousarray(h1_core[j][:, 128:]),
                     "w_g": w_g1, "p_rg": f32(p_rg[1]), "p_m": f32(p_m[1]), "w_out": f32(w_out[1]), "vec": vecs_for(1, b, None),
                     "rw": f32(router_w[0]), "rb": rb})
    res = _run(_prog("B1", lambda: build_B(groups1, T1, False, True)), maps, "B1")
    hmid_core = [r["h1T_out"] for r in res]
    vT_all = np.concatenate([r["vT_out"] for r in res], 1)
    gw_all = np.concatenate([r["gw"] for r in res], 0)

    NTOK = gw_all.shape[0]
    top2 = np.argsort(-gw_all, axis=1, kind="stable")[:, :2]
    top2.sort(axis=1)
    lists = [np.nonzero((top2 == e).any(1))[0] for e in range(8)]
    nblk = [-(-len(l) // 512) for l in lists]
    plan = None
    for NS in range(max(1, -(-sum(nblk) // 8)), max(nblk) + 1):
        for K1 in range(NS, -1, -1):
            over = [max(0, n - K1) for n in nblk]
            if K1 == NS:
                ok = sum(over) == 0
                need = 0
            else:
                need = sum(-(-o // (NS - K1)) for o in over)
                ok = need <= 8
            if ok:
                plan = (NS, K1)
                break
        if plan:
            break
    NS, K1 = plan
    print(f"[kernel] moe counts={[len(l) for l in lists]} NS={NS} K1={K1}", flush=True)
    sec = []
    for e in range(8):
        o = max(0, nblk[e] - K1)
        b0 = K1
        while o > 0:
            nb_ = min(o, NS - K1)
            sec.append((e, b0, nb_))
            b0 += nb_
            o -= nb_
    while len(sec) < 8:
        sec.append((len(sec), 0, 0))
    T_C = NS * 512
    pos = np.zeros((NTOK, 8), np.int64)
    for e in range(8):
        pos[lists[e], e] = np.arange(len(lists[e]))
    maps = []
    slot_src = []
    for ci in range(8):
        vT = np.zeros((D, T_C), NPBF)
        wr = np.zeros((T_C,), np.float32)
        srcs = []
        blocks = [(ci, bb) for bb in range(min(nblk[ci], K1))]
        blocks += [None] * (K1 - len(blocks))
        se, sb0, snb = sec[ci]
        blocks += [(se, sb0 + i) for i in range(snb)] + [None] * (NS - K1 - snb)
        for sl, blk in enumerate(blocks):
            if blk is None:
                continue
            e, bb = blk
            idx = lists[e][bb * 512:(bb + 1) * 512]
            vT[:, sl * 512:sl * 512 + len(idx)] = vT_all[:, idx]
            wr[sl * 512:sl * 512 + len(idx)] = gw_all[idx, e]
        slot_src.append(blocks)
        m = {"vT": vT, "wrow": np.ascontiguousarray(np.broadcast_to(wr, (128, T_C))),
             "w1": f32(ex_w1[0][ci]), "w3": f32(ex_w3[0][ci]), "w2": f32(ex_w2[0][ci])}
        if K1 < NS:
            m.update({"w1b": f32(ex_w1[0][se]), "w3b": f32(ex_w3[0][se]), "w2b": f32(ex_w2[0][se])})
        maps.append(m)
    res = _run(_prog(("C", NS, K1), lambda: build_C(NS, K1)), maps, f"C{NS}_{K1}")
    Y = [np.zeros((D, nblk[e] * 512), np.float32) for e in range(8)]
    for ci in range(8):
        for sl, blk in enumerate(slot_src[ci]):
            if blk is None:
                continue
            e, bb = blk
            Y[e][:, bb * 512:(bb + 1) * 512] = res[ci]["yT"][:, sl * 512:(sl + 1) * 512]

    maps = []
    for j, (b, half) in enumerate(cores):
        tl = np.arange(j * T1, (j + 1) * T1)
        ya = np.empty((D, T1), np.float32)
        yb = np.empty((D, T1), np.float32)
        for e in range(8):
            ma = top2[tl, 0] == e
            mb = top2[tl, 1] == e
            ya[:, ma] = Y[e][:, pos[tl[ma], e]]
            yb[:, mb] = Y[e][:, pos[tl[mb], e]]
        maps.append({"yaT": ya, "ybT": yb, "h1T": hmid_core[j], "vec": vecs_for(1, b, None)})
    res = _run(_prog("D", lambda: build_D(T1)), maps, "D")
    out = np.empty((NB, SEQ, D), np.float32)
    for j, (b, half) in enumerate(cores):
        out[b, half * (SEQ // 2):(half + 1) * (SEQ // 2)] = res[j]["oT"].T
    return out
```

```python
import numpy as np
import ml_dtypes
from contextlib import ExitStack
import concourse.bass as bass
import concourse.mybir as mybir
from concourse.bass_utils import run_bass_kernel_spmd

F32 = mybir.dt.float32
BF16 = mybir.dt.bfloat16
AF = mybir.ActivationFunctionType
ALU = mybir.AluOpType
AX = mybir.AxisListType
NPBF = ml_dtypes.bfloat16

D = 2048
KD = 16
DFF = 7168
KF = 56
NCORE = 8
ALPHA = 4.0 ** 0.25
LN_EPS = 1e-6
GRID = 64
CTX = 256
SEQ = 4096
NB = 4
W_RG = 2048
W_M = 2048
C_RGG = W_RG
C_M = 2 * W_RG
C_G = C_M + 4 * W_M + 32
N_IN = C_G + 2 * D


class Res:
    __slots__ = ("last_w", "readers", "excl")

    def __init__(self, excl=False):
        self.last_w = None
        self.readers = []
        self.excl = excl


class Op:
    __slots__ = ("eng", "fn", "deps", "dma", "idx", "signal", "count", "slot", "slot_count")

    def __init__(self, eng, fn, dma):
        self.eng = eng
        self.fn = fn
        self.deps = []
        self.dma = dma
        self.signal = False
        self.count = 0
        self.slot = None
        self.slot_count = 0


class Prog:
    ENGS = ("pe", "act", "dve", "pool", "sp")
    NSLOT = 8

    def __init__(self, nc):
        self.nc = nc
        self.ops = []
        self.es = ExitStack()
        self._n = 0

    def sbuf(self, shape, dtype):
        self._n += 1
        return self.es.enter_context(self.nc.sbuf_tensor(f"sb{self._n}_{id(self) % 9973}", list(shape), dtype))

    def psum(self, shape, dtype):
        self._n += 1
        return self.es.enter_context(self.nc.psum_tensor(f"ps{self._n}_{id(self) % 9973}", list(shape), dtype))

    def add(self, eng, fn, reads=(), writes=(), dma=False):
        op = Op(eng, fn, dma)
        op.idx = len(self.ops)
        deps = {}
        for r in reads:
            if r.last_w is not None:
                deps[r.last_w.idx] = r.last_w
            if r.excl:
                for rd in r.readers:
                    if rd.eng != eng:
                        deps[rd.idx] = rd
        for w in writes:
            if w.last_w is not None:
                deps[w.last_w.idx] = w.last_w
            for rd in w.readers:
                deps[rd.idx] = rd
        last = {}
        for d in deps.values():
            if d.dma:
                op.deps.append(d)
                continue
            if d.eng == "pe" and eng == "pe" and not dma:
                continue
            if d.eng not in last or last[d.eng].idx < d.idx:
                last[d.eng] = d
        op.deps.extend(last.values())
        for r in reads:
            r.readers.append(op)
        for w in writes:
            w.last_w = op
            w.readers = []
        self.ops.append(op)
        return op

    def dma(self, q, out, in_, reads=(), writes=()):
        return self.add(q, lambda e: e.dma_start(out=out, in_=in_), reads, writes, dma=True)

    def emit(self):
        nc = self.nc
        ops = self.ops
        for op in ops:
            for d in op.deps:
                d.signal = True
        counts = {e: 0 for e in self.ENGS}
        dcount = {e: 0 for e in self.ENGS}
        slot_counts = {e: [0] * self.NSLOT for e in self.ENGS}
        for op in ops:
            if op.dma:
                k = dcount[op.eng]
                dcount[op.eng] += 1
                op.slot = k % self.NSLOT
                slot_counts[op.eng][op.slot] += 1
                op.slot_count = slot_counts[op.eng][op.slot]
            elif op.signal:
                counts[op.eng] += 1
                op.count = counts[op.eng]
        tag = id(self) % 9973
        sems = {e: self.es.enter_context(nc.semaphore(f"s_{e}_{tag}")) for e in self.ENGS}
        dsems = {}
        for e in self.ENGS:
            if dcount[e]:
                dsems[e] = [self.es.enter_context(nc.semaphore(f"d_{e}{i}_{tag}"))
                            for i in range(min(self.NSLOT, dcount[e]))]
        by_eng = {e: [op for op in ops if op.eng == e] for e in self.ENGS}
        block = self.es.enter_context(nc.Block())

        def run(e, h):
            seen = {}
            for op in by_eng[e]:
                waits = {}
                for d in op.deps:
                    if d.dma:
                        key = ("d", d.eng, d.slot)
                        val = 16 * d.slot_count
                        sem = dsems[d.eng][d.slot]
                    else:
                        key = ("c", d.eng)
                        val = d.count
                        sem = sems[d.eng]
                    if seen.get(key, 0) >= val:
                        continue
                    if key not in waits or waits[key][1] < val:
                        waits[key] = (sem, val)
                if op.dma and op.slot_count > 1:
                    key = ("d", e, op.slot)
                    val = 16 * (op.slot_count - 1)
                    if seen.get(key, 0) < val and (key not in waits or waits[key][1] < val):
                        waits[key] = (dsems[e][op.slot], val)
                for key, (sem, val) in waits.items():
                    h.wait_ge(sem, val)
                    seen[key] = val
                ins = op.fn(h)
                if op.dma:
                    ins.then_inc(dsems[e][op.slot], 16)
                elif op.signal:
                    ins.then_inc(sems[e], 1)
            if e in dsems:
                for s in range(len(dsems[e])):
                    val = 16 * slot_counts[e][s]
                    if seen.get(("d", e, s), 0) < val:
                        h.wait_ge(dsems[e][s], val)

        @block.tensor
        def _(h):
            run("pe", h)

        @block.scalar
        def _(h):
            run("act", h)

        @block.vector
        def _(h):
            run("dve", h)

        @block.gpsimd
        def _(h):
            run("pool", h)

        @block.sync
        def _(h):
            run("sp", h)

        self.es.close()


class Rot:
    def __init__(self, P, n, shape, dtype, psum=False):
        self.bufs = [(P.psum(shape, dtype) if psum else P.sbuf(shape, dtype)) for _ in range(n)]
        self.res = [Res(excl=psum) for _ in range(n)]
        self.i = 0
        self.n = n

    def next(self):
        j = self.i % self.n
        self.i += 1
        return self.bufs[j], self.res[j]


def new_nc():
    return bass.Bass("TRN2", target_bir_lowering=False)


def din(nc, name, shape, dt):
    return nc.dram_tensor(name, list(shape), dt, kind="ExternalInput").ap()


def dout(nc, name, shape, dt):
    return nc.dram_tensor(name, list(shape), dt, kind="ExternalOutput").ap()


class Ctx:
    def __init__(self, P, wslots=6, wcols=4096, ntmp=6, nmm=6, ln=True):
        self.P = P
        self.mm = Rot(P, nmm, [128, 512], F32, psum=True)
        self.st = Rot(P, 2, [128, 512], F32, psum=True)
        self.tmp = Rot(P, ntmp, [128, 512], F32)
        self.wp = Rot(P, wslots, [128, wcols], BF16) if wslots else None
        self.ones = P.sbuf([128, 128], F32)
        self.r_ones = Res()
        P.add("dve", lambda e: e.memset(self.ones[:], 1.0), writes=[self.r_ones])
        self.stat = Rot(P, 5, [128, 512], F32) if ln else None

    def wtile(self, src, nk, ncols):
        buf, res = self.wp.next()
        view = buf[:, 0:nk * ncols].rearrange("p (k c) -> p k c", k=nk)
        self.P.dma("pool", view, src.rearrange("(k p) c -> p k c", p=128), writes=[res])
        return view, res


def fm_mm(cx, wview, wres, c0, xs, xres, G, nk=None, koff=0, ps=None, first=True, last=True):
    P = cx.P
    if ps is None:
        ps = cx.mm.next()
    pbuf, pres = ps
    nk = nk if nk is not None else KD
    for k in range(nk):
        P.add("pe", lambda e, k=k: e.matmul(pbuf[:, 0:G], wview[:, k, c0:c0 + 128], xs(koff + k),
                                            start=(first and k == 0), stop=(last and k == nk - 1)),
              reads=[wres, xres(koff + k)], writes=[pres])
    return ps


def layer_norm_fm(cx, src, src_res, G, outs):
    P = cx.P
    s1, r1 = cx.st.next()
    s2, r2 = cx.st.next()
    accq, racq = cx.stat.next()
    for c in range(KD):
        P.add("pe", lambda e, c=c: e.matmul(s1[:, 0:G], cx.ones[:], src(c), start=(c == 0), stop=(c == KD - 1)),
              reads=[cx.r_ones, src_res(c)], writes=[r1])
        if c == 0:
            P.add("act", lambda e, c=c: e.activation(accq[:, 0:G], src(c), AF.Square), reads=[src_res(c)], writes=[racq])
        else:
            sq, rsq = cx.tmp.next()
            P.add("act", lambda e, c=c, sq=sq: e.activation(sq[:, 0:G], src(c), AF.Square), reads=[src_res(c)], writes=[rsq])
            P.add("dve", lambda e, sq=sq: e.tensor_tensor(accq[:, 0:G], accq[:, 0:G], sq[:, 0:G], ALU.add), reads=[rsq, racq], writes=[racq])
    P.add("pe", lambda e: e.matmul(s2[:, 0:G], cx.ones[:], accq[:, 0:G], start=True, stop=True), reads=[cx.r_ones, racq], writes=[r2])
    mean, rm = cx.stat.next()
    msq, rq = cx.stat.next()
    var, rv = cx.stat.next()
    rstd, rr = cx.stat.next()
    P.add("act", lambda e: e.mul(mean[:, 0:G], s1[:, 0:G], 1.0 / D), reads=[r1], writes=[rm])
    P.add("dve", lambda e: e.tensor_tensor(msq[:, 0:G], mean[:, 0:G], mean[:, 0:G], ALU.mult), reads=[rm], writes=[rq])
    P.add("dve", lambda e: e.scalar_tensor_tensor(var[:, 0:G], s2[:, 0:G], 1.0 / D, msq[:, 0:G], ALU.mult, ALU.subtract),
          reads=[r2, rq], writes=[rv])
    P.add("act", lambda e: e.activation(var[:, 0:G], var[:, 0:G], AF.Sqrt, bias=LN_EPS, scale=1.0), reads=[rv], writes=[rv])
    P.add("dve", lambda e: e.reciprocal(rstd[:, 0:G], var[:, 0:G]), reads=[rv], writes=[rr])
    for c in range(KD):
        t1, rt1 = cx.tmp.next()
        P.add("dve", lambda e, c=c, t1=t1: e.tensor_tensor(t1[:, 0:G], src(c), mean[:, 0:G], ALU.subtract),
              reads=[src_res(c), rm], writes=[rt1])
        P.add("dve", lambda e, t1=t1: e.tensor_tensor(t1[:, 0:G], t1[:, 0:G], rstd[:, 0:G], ALU.mult),
              reads=[rt1, rr], writes=[rt1])
        for (dst, dres, sc, bi, extra) in outs:
            P.add("act", lambda e, c=c, t1=t1, dst=dst, sc=sc, bi=bi: e.activation(dst(c), t1[:, 0:G], AF.Identity, bias=bi(c), scale=sc(c)),
                  reads=[rt1] + list(extra), writes=[dres(c)])


def ffn_fm(cx, w1, w3, w2, vview, vres, G, act_view, act_res, consume):
    P = cx.P
    for f2 in range(KF // 2):
        t1, rw1 = cx.wtile(w1[:, f2 * 256:(f2 + 1) * 256], KD, 256)
        t3, rw3 = cx.wtile(w3[:, f2 * 256:(f2 + 1) * 256], KD, 256)
        for cc in range(2):
            f = 2 * f2 + cc
            p1, rp1 = fm_mm(cx, t1, rw1, cc * 128, vview, vres, G)
            p3, rp3 = fm_mm(cx, t3, rw3, cc * 128, vview, vres, G)
            s, rs = cx.tmp.next()
            P.add("act", lambda e, p1=p1, s=s: e.activation(s[:, 0:G], p1[:, 0:G], AF.Silu), reads=[rp1], writes=[rs])
            P.add("dve", lambda e, f=f, p3=p3, s=s: e.tensor_tensor(act_view(f), s[:, 0:G], p3[:, 0:G], ALU.mult),
                  reads=[rs, rp3], writes=[act_res(f)])
    for c in range(KD):
        ta, ra = cx.wtile(w2[0:(KF // 2) * 128, c * 128:(c + 1) * 128], KF // 2, 128)
        tb, rb = cx.wtile(w2[(KF // 2) * 128:KF * 128, c * 128:(c + 1) * 128], KF // 2, 128)
        ps = cx.mm.next()
        fm_mm(cx, ta, ra, 0, act_view, act_res, G, nk=KF // 2, koff=0, ps=ps, first=True, last=False)
        fm_mm(cx, tb, rb, 0, act_view, act_res, G, nk=KF // 2, koff=KF // 2, ps=ps, first=False, last=True)
        consume(c, ps[0], ps[1])


V_G1, V_SC2, V_SH2, V_G2, V_NSC1, V_NSH1 = range(6)
V_LN = 12


def build_B(groups, T, ffn, router):
    nc = new_nc()
    yrT = din(nc, "yrT", [D, T], BF16)
    ymT = din(nc, "ymT", [D, T], BF16)
    uT = din(nc, "uT", [D, T], BF16)
    hT = din(nc, "hT", [D, T], F32)
    w_g = din(nc, "w_g", [D, 2 * D], F32)
    p_rg = din(nc, "p_rg", [W_RG, D], F32)
    p_m = din(nc, "p_m", [W_M, D], F32)
    w_out = din(nc, "w_out", [D, D], F32)
    vec = din(nc, "vec", [128, 16, KD], F32)
    if ffn:
        w1 = din(nc, "w1", [D, DFF], F32)
        w3 = din(nc, "w3", [D, DFF], F32)
        w2 = din(nc, "w2", [DFF, D], F32)
        hT_out = dout(nc, "hT_out", [D, T], F32)
        uT_out = dout(nc, "uT_out", [D, T], BF16)
    if router:
        rw = din(nc, "rw", [D, 8], F32)
        rb = din(nc, "rb", [128, 8], F32)
        h1T_out = dout(nc, "h1T_out", [D, T], F32)
        vT_out = dout(nc, "vT_out", [D, T], BF16)
        gw_out = dout(nc, "gw", [T, 8], F32)

    P = Prog(nc)
    cx = Ctx(P, wslots=5, ntmp=5) if router else Ctx(P, wslots=8, ntmp=8)
    vs = P.sbuf([128, 16, KD], F32)
    r_vs = Res()
    P.dma("sp", vs[:], vec, writes=[r_vs])
    for kind in range(2):
        for slot in (V_SC2, V_NSC1):
            j = kind * 6 + slot
            P.add("dve", lambda e, j=j: e.tensor_scalar_add(vs[:, j, :], vs[:, j, :], 1.0), reads=[r_vs], writes=[r_vs])

    def vcol(kind, slot):
        j = (kind * 6 + slot) if slot < 6 else slot
        return lambda c: vs[:, j, c:c + 1]

    scr = P.sbuf([128, 64 * 512], BF16)
    rj = [Res() for _ in range(64)]
    hbuf = P.sbuf([128, KD, 512], F32)
    rh = [Res() for _ in range(KD)]
    vbuf = P.sbuf([128, KD, 512], BF16)
    rv = [Res() for _ in range(KD)]
    if router:
        v32 = P.sbuf([128, KD, 512], F32)
        rv32 = [Res() for _ in range(KD)]
        rws = P.sbuf([128, KD, 8], F32)
        r_rws = Res()
        P.dma("sp", rws[:], rw.rearrange("(k p) e -> p k e", p=128), writes=[r_rws])
        rbs = P.sbuf([128, 8], F32)
        r_rbs = Res()
        P.dma("sp", rbs[:], rb, writes=[r_rbs])
        small = Rot(P, 12, [128, 8], F32)

    def do_group(tok0, G, kind):
        def q(qi, k, G=G):
            return scr[:, (qi * 16 + k) * 512:(qi * 16 + k) * 512 + G]

        def qall(qi, G=G):
            return scr[:, qi * 16 * 512:(qi + 1) * 16 * 512].rearrange("p (k t) -> p k t", k=16)[:, :, 0:G]

        for qi, src in enumerate((yrT, ymT, uT)):
            P.dma("sp", qall(qi), src[:, tok0:tok0 + G].rearrange("(k p) t -> p k t", p=128),
                  writes=rj[qi * 16:(qi + 1) * 16])
        P.dma("sp", hbuf[:, :, 0:G], hT[:, tok0:tok0 + G].rearrange("(k p) t -> p k t", p=128), writes=rh)
        for c in range(KD):
            P.add("act", lambda e, c=c, G=G: e.mul(hbuf[:, c, 0:G], hbuf[:, c, 0:G], ALPHA), reads=[rh[c]], writes=[rh[c]])
        for c2 in range(KD // 2):
            cs = slice(c2 * 256, (c2 + 1) * 256)
            sa = [cx.tmp.next() for _ in range(2)]
            sb_ = [cx.tmp.next() for _ in range(2)]
            tg1, rg1 = cx.wtile(w_g[:, cs], KD, 256)
            for cc in range(2):
                pg, rpg = fm_mm(cx, tg1, rg1, cc * 128, lambda k: q(2, k), lambda k: rj[32 + k], G)
                P.add("act", lambda e, pg=pg, t=sa[cc][0]: e.activation(t[:, 0:G], pg[:, 0:G], AF.Sigmoid), reads=[rpg], writes=[sa[cc][1]])
            tg2, rg2 = cx.wtile(w_g[:, D + c2 * 256:D + (c2 + 1) * 256], KD, 256)
            for cc in range(2):
                pg, rpg = fm_mm(cx, tg2, rg2, cc * 128, lambda k: q(2, k), lambda k: rj[32 + k], G)
                P.add("act", lambda e, pg=pg, t=sb_[cc][0]: e.activation(t[:, 0:G], pg[:, 0:G], AF.Sigmoid), reads=[rpg], writes=[sb_[cc][1]])
            tp1, rp1 = cx.wtile(p_rg[:, cs], KD, 256)
            for cc in range(2):
                pa, rpa = fm_mm(cx, tp1, rp1, cc * 128, lambda k: q(0, k), lambda k: rj[k], G)
                P.add("dve", lambda e, pa=pa, t=sa[cc][0]: e.tensor_tensor(t[:, 0:G], t[:, 0:G], pa[:, 0:G], ALU.mult), reads=[sa[cc][1], rpa], writes=[sa[cc][1]])
            tp2, rp2 = cx.wtile(p_m[:, cs], KD, 256)
            for cc in range(2):
                c = 2 * c2 + cc
                pa, rpa = fm_mm(cx, tp2, rp2, cc * 128, lambda k: q(1, k), lambda k: rj[16 + k], G)
                P.add("dve", lambda e, pa=pa, t=sb_[cc][0]: e.tensor_tensor(t[:, 0:G], t[:, 0:G], pa[:, 0:G], ALU.mult), reads=[sb_[cc][1], rpa], writes=[sb_[cc][1]])
                P.add("dve", lambda e, c=c, t1=sa[cc][0], t2=sb_[cc][0]: e.tensor_tensor(q(3, c, G), t1[:, 0:G], t2[:, 0:G], ALU.add),
                      reads=[sa[cc][1], sb_[cc][1]], writes=[rj[48 + c]])
        g1 = vcol(kind, V_G1)
        for c2 in range(KD // 2):
            to, ro = cx.wtile(w_out[:, c2 * 256:(c2 + 1) * 256], KD, 256)
            for cc in range(2):
                c = 2 * c2 + cc
                po, rpo = fm_mm(cx, to, ro, cc * 128, lambda k: q(3, k), lambda k: rj[48 + k], G)
                P.add("dve", lambda e, c=c, po=po, G=G, g1=g1: e.scalar_tensor_tensor(hbuf[:, c, 0:G], po[:, 0:G], g1(c), hbuf[:, c, 0:G], ALU.mult, ALU.add),
                      reads=[rpo, rh[c], r_vs], writes=[rh[c]])
        hsrc = lambda c, G=G: hbuf[:, c, 0:G]
        layer_norm_fm(cx, hsrc, lambda c: rh[c], G,
                      [(hsrc, lambda c: rh[c], vcol(0, V_LN + 0), vcol(0, V_LN + 1), [r_vs])])
        outs = [(lambda c, G=G: vbuf[:, c, 0:G], lambda c: rv[c], vcol(kind, V_SC2), vcol(kind, V_SH2), [r_vs])]
        if router:
            outs.append((lambda c, G=G: v32[:, c, 0:G], lambda c: rv32[c], vcol(kind, V_SC2), vcol(kind, V_SH2), [r_vs]))
        layer_norm_fm(cx, hsrc, lambda c: rh[c], G, outs)
        if ffn:
            g2 = vcol(kind, V_G2)
            for c in range(KD):
                P.add("act", lambda e, c=c, G=G: e.mul(hbuf[:, c, 0:G], hbuf[:, c, 0:G], ALPHA), reads=[rh[c]], writes=[rh[c]])

            def consume(c, pbuf, pres, G=G, g2=g2):
                P.add("dve", lambda e: e.scalar_tensor_tensor(hbuf[:, c, 0:G], pbuf[:, 0:G], g2(c), hbuf[:, c, 0:G], ALU.mult, ALU.add),
                      reads=[pres, rh[c], r_vs], writes=[rh[c]])

            ffn_fm(cx, w1, w3, w2, lambda k, G=G: vbuf[:, k, 0:G], lambda k: rv[k], G,
                   lambda f, G=G: scr[:, f * 512:f * 512 + G], lambda f: rj[f], consume)
            layer_norm_fm(cx, hsrc, lambda c: rh[c], G,
                          [(hsrc, lambda c: rh[c], vcol(0, V_LN + 2), vcol(0, V_LN + 3), [r_vs])])
            P.dma("sp", hT_out[:, tok0:tok0 + G].rearrange("(k p) t -> p k t", p=128), hbuf[:, :, 0:G], reads=rh)
            layer_norm_fm(cx, hsrc, lambda c: rh[c], G,
                          [(lambda c, G=G: vbuf[:, c, 0:G], lambda c: rv[c], vcol(kind, V_NSC1), vcol(kind, V_NSH1), [r_vs])])
            P.dma("sp", uT_out[:, tok0:tok0 + G].rearrange("(k p) t -> p k t", p=128), vbuf[:, :, 0:G], reads=rv)
        if router:
            P.dma("sp", h1T_out[:, tok0:tok0 + G].rearrange("(k p) t -> p k t", p=128), hbuf[:, :, 0:G], reads=rh)
            P.dma("sp", vT_out[:, tok0:tok0 + G].rearrange("(k p) t -> p k t", p=128), vbuf[:, :, 0:G], reads=rv)
            for t0 in range(0, G, 128):
                pl, rpl = cx.mm.next()
                for k in range(KD):
                    P.add("pe", lambda e, k=k, t0=t0, pl=pl: e.matmul(pl[:, 0:8], v32[:, k, t0:t0 + 128], rws[:, k, :], start=(k == 0), stop=(k == KD - 1)),
                          reads=[rv32[k], r_rws], writes=[rpl])
                lg, rlg = small.next()
                m1, rm1 = small.next()
                e1, re1 = small.next()
                l2, rl2 = small.next()
                m2, rm2 = small.next()
                e2, re2 = small.next()
                dd, rdd = small.next()
                gwt, rgw = small.next()
                P.add("dve", lambda e, pl=pl, lg=lg: e.tensor_tensor(lg[:], pl[:, 0:8], rbs[:], ALU.add), reads=[rpl, r_rbs], writes=[rlg])
                P.add("dve", lambda e, lg=lg, m1=m1: e.reduce_max(m1[:, 0:1], lg[:], AX.X), reads=[rlg], writes=[rm1])
                P.add("dve", lambda e, lg=lg, m1=m1, e1=e1: e.tensor_scalar(e1[:], lg[:], m1[:, 0:1], None, ALU.is_equal), reads=[rlg, rm1], writes=[re1])
                P.add("dve", lambda e, lg=lg, e1=e1, l2=l2: e.scalar_tensor_tensor(l2[:], e1[:], -1e30, lg[:], ALU.mult, ALU.add), reads=[rlg, re1], writes=[rl2])
                P.add("dve", lambda e, l2=l2, m2=m2: e.reduce_max(m2[:, 0:1], l2[:], AX.X), reads=[rl2], writes=[rm2])
                P.add("dve", lambda e, l2=l2, m2=m2, e2=e2: e.tensor_scalar(e2[:], l2[:], m2[:, 0:1], None, ALU.is_equal), reads=[rl2, rm2], writes=[re2])
                P.add("dve", lambda e, m1=m1, m2=m2, dd=dd: e.tensor_tensor(dd[:, 0:1], m2[:, 0:1], m1[:, 0:1], ALU.subtract), reads=[rm1, rm2], writes=[rdd])
                P.add("act", lambda e, dd=dd: e.activation(dd[:, 1:2], dd[:, 0:1], AF.Exp), reads=[rdd], writes=[rdd])
                P.add("dve", lambda e, dd=dd: e.tensor_scalar_add(dd[:, 2:3], dd[:, 1:2], 1.0), reads=[rdd], writes=[rdd])
                P.add("dve", lambda e, dd=dd: e.reciprocal(dd[:, 2:3], dd[:, 2:3]), reads=[rdd], writes=[rdd])
                P.add("dve", lambda e, dd=dd: e.tensor_tensor(dd[:, 3:4], dd[:, 1:2], dd[:, 2:3], ALU.mult), reads=[rdd], writes=[rdd])
                P.add("dve", lambda e, e1=e1, dd=dd, gwt=gwt: e.tensor_scalar(gwt[:], e1[:], dd[:, 2:3], None, ALU.mult), reads=[re1, rdd], writes=[rgw])
                P.add("dve", lambda e, e2=e2, dd=dd, gwt=gwt: e.scalar_tensor_tensor(gwt[:], e2[:], dd[:, 3:4], gwt[:], ALU.mult, ALU.add), reads=[re2, rdd, rgw], writes=[rgw])
                P.dma("sp", gw_out[tok0 + t0:tok0 + t0 + 128, :], gwt[:], reads=[rgw])
    for (tok0, G, kind) in groups:
        do_group(tok0, G, kind)
    P.emit()
    return nc


def build_C(NS, K1):
    T = NS * 512
    nc = new_nc()
    vT = din(nc, "vT", [D, T], BF16)
    wrow = din(nc, "wrow", [128, T], F32)
    wts = [(din(nc, "w1", [D, DFF], F32), din(nc, "w3", [D, DFF], F32), din(nc, "w2", [DFF, D], F32))]
    if K1 < NS:
        wts.append((din(nc, "w1b", [D, DFF], F32), din(nc, "w3b", [D, DFF], F32), din(nc, "w2b", [DFF, D], F32)))
    yT = dout(nc, "yT", [D, T], F32)
    P = Prog(nc)
    cx = Ctx(P, wslots=8)
    vb = Rot(P, 2, [128, KD, 512], BF16)
    wr = Rot(P, 2, [128, 512], F32)
    scr = P.sbuf([128, KF * 512], BF16)
    rj = [Res() for _ in range(KF)]

    def do_group(tok0, G, w1, w3, w2):
        vbuf, rvb = vb.next()
        wrt, rwr = wr.next()
        P.dma("sp", vbuf[:, :, 0:G], vT[:, tok0:tok0 + G].rearrange("(k p) t -> p k t", p=128), writes=[rvb])
        P.dma("sp", wrt[:, 0:G], wrow[:, tok0:tok0 + G], writes=[rwr])

        def consume(c, pbuf, pres):
            o, ro = cx.tmp.next()
            P.add("dve", lambda e: e.tensor_tensor(o[:, 0:G], pbuf[:, 0:G], wrt[:, 0:G], ALU.mult), reads=[pres, rwr], writes=[ro])
            P.dma("sp", yT[c * 128:(c + 1) * 128, tok0:tok0 + G], o[:, 0:G], reads=[ro])

        ffn_fm(cx, w1, w3, w2, lambda k: vbuf[:, k, 0:G], lambda k: rvb, G,
               lambda f: scr[:, f * 512:f * 512 + G], lambda f: rj[f], consume)

    for sl in range(NS):
        w1, w3, w2 = wts[0] if sl < K1 else wts[1]
        do_group(sl * 512, 512, w1, w3, w2)
    P.emit()
    return nc


def build_D(T):
    nc = new_nc()
    yaT = din(nc, "yaT", [D, T], F32)
    ybT = din(nc, "ybT", [D, T], F32)
    h1T = din(nc, "h1T", [D, T], F32)
    vec = din(nc, "vec", [128, 16, KD], F32)
    oT = dout(nc, "oT", [D, T], F32)
    P = Prog(nc)
    cx = Ctx(P, wslots=0)
    vs = P.sbuf([128, 16, KD], F32)
    r_vs = Res()
    P.dma("sp", vs[:], vec, writes=[r_vs])
    ya = P.sbuf([128, KD, 512], F32)
    yb = P.sbuf([128, KD, 512], F32)
    hb = P.sbuf([128, KD, 512], F32)
    rya = [Res() for _ in range(KD)]
    ryb = [Res() for _ in range(KD)]
    rh = [Res() for _ in range(KD)]

    def do_group(tok0, G):
        for buf, res, src in ((ya, rya, yaT), (yb, ryb, ybT), (hb, rh, h1T)):
            P.dma("sp", buf[:, :, 0:G], src[:, tok0:tok0 + G].rearrange("(k p) t -> p k t", p=128), writes=res)
        for c in range(KD):
            P.add("act", lambda e, c=c: e.mul(hb[:, c, 0:G], hb[:, c, 0:G], ALPHA), reads=[rh[c]], writes=[rh[c]])
            P.add("dve", lambda e, c=c: e.tensor_tensor(ya[:, c, 0:G], ya[:, c, 0:G], yb[:, c, 0:G], ALU.add), reads=[rya[c], ryb[c]], writes=[rya[c]])
            P.add("dve", lambda e, c=c: e.scalar_tensor_tensor(hb[:, c, 0:G], ya[:, c, 0:G], vs[:, V_G2, c:c + 1], hb[:, c, 0:G], ALU.mult, ALU.add),
                  reads=[rya[c], rh[c], r_vs], writes=[rh[c]])
        hsrc = lambda c: hb[:, c, 0:G]
        layer_norm_fm(cx, hsrc, lambda c: rh[c], G,
                      [(hsrc, lambda c: rh[c], lambda c: vs[:, V_LN + 2, c:c + 1], lambda c: vs[:, V_LN + 3, c:c + 1], [r_vs])])
        P.dma("sp", oT[:, tok0:tok0 + G].rearrange("(k p) t -> p k t", p=128), hb[:, :, 0:G], reads=rh)

    for tok0 in range(0, T, 512):
        do_group(tok0, min(512, T - tok0))
    P.emit()
    return nc


def build_Kln(groups, T):
    nc = new_nc()
    hT = din(nc, "hT", [D, T], F32)
    vec = din(nc, "vec", [128, 16, KD], F32)
    uT_out = dout(nc, "uT_out", [D, T], BF16)
    P = Prog(nc)
    cx = Ctx(P, wslots=0)
    vs = P.sbuf([128, 16, KD], F32)
    r_vs = Res()
    P.dma("sp", vs[:], vec, writes=[r_vs])
    for kind in range(2):
        j = kind * 6 + V_NSC1
        P.add("dve", lambda e, j=j: e.tensor_scalar_add(vs[:, j, :], vs[:, j, :], 1.0), reads=[r_vs], writes=[r_vs])
    hb = Rot(P, 2, [128, KD, 512], F32)
    ub = Rot(P, 2, [128, KD, 512], BF16)

    def do_group(tok0, G, kind):
        hbuf, rhb = hb.next()
        ubuf, rub = ub.next()
        P.dma("sp", hbuf[:, :, 0:G], hT[:, tok0:tok0 + G].rearrange("(k p) t -> p k t", p=128), writes=[rhb])
        layer_norm_fm(cx, lambda c: hbuf[:, c, 0:G], lambda c: rhb, G,
                      [(lambda c: ubuf[:, c, 0:G], lambda c: rub, lambda c: vs[:, kind * 6 + V_NSC1, c:c + 1],
                        lambda c: vs[:, kind * 6 + V_NSH1, c:c + 1], [r_vs])])
        P.dma("sp", uT_out[:, tok0:tok0 + G].rearrange("(k p) t -> p k t", p=128), ubuf[:, :, 0:G], reads=[rub])

    for (tok0, G, kind) in groups:
        do_group(tok0, G, kind)
    P.emit()
    return nc


MOD_COLS = 2 * 6 * D // NCORE


def build_Kmod():
    nc = new_nc()
    cT = din(nc, "cT", [D, 5], F32)
    wm = din(nc, "wm", [D, MOD_COLS], F32)
    bm = din(nc, "bm", [5, MOD_COLS], F32)
    out = dout(nc, "mod", [5, MOD_COLS], F32)
    P = Prog(nc)
    ps = Rot(P, 2, [128, 512], F32, psum=True)
    wt = Rot(P, 2, [128, KD, 512], F32)
    cs = P.sbuf([128, KD, 5], F32)
    r_cs = Res()
    P.dma("sp", cs[:], cT.rearrange("(k p) r -> p k r", p=128), writes=[r_cs])
    P.add("act", lambda e: e.activation(cs[:], cs[:], AF.Silu), reads=[r_cs], writes=[r_cs])
    bs = P.sbuf([5, MOD_COLS], F32)
    r_bs = Res()
    P.dma("sp", bs[:], bm, writes=[r_bs])
    ob = P.sbuf([5, MOD_COLS], F32)
    r_ob = Res()
    for n in range(MOD_COLS // 512):
        w, rw_ = wt.next()
        P.dma("sp" if n % 2 == 0 else "act", w[:], wm[:, n * 512:(n + 1) * 512].rearrange("(k p) c -> p k c", p=128), writes=[rw_])
        p, rp = ps.next()
        for k in range(KD):
            P.add("pe", lambda e, k=k, p=p, w=w: e.matmul(p[0:5, :], cs[:, k, :], w[:, k, :], start=(k == 0), stop=(k == KD - 1)),
                  reads=[r_cs, rw_], writes=[rp])
        P.add("dve", lambda e, n=n, p=p: e.tensor_tensor(ob[:, n * 512:(n + 1) * 512], p[0:5, :], bs[:, n * 512:(n + 1) * 512], ALU.add),
              reads=[rp, r_bs], writes=[r_ob])
    P.dma("sp", out, ob[:], reads=[r_ob])
    P.emit()
    return nc


TPB = CTX + SEQ
GELU_C = 0.044715
GELU_S = 1.5957691216057308


def batch_groups():
    return [(0, CTX)] + [(CTX + 512 * i, 512) for i in range(SEQ // 512)]


def build_Arg(nbatch, nc=None):
    NT = nbatch * TPB
    nc = nc or new_nc()
    uT = din(nc, "uT", [D, NT], BF16)
    wx = din(nc, "wx", [D, 256], F32)
    wg = din(nc, "wg", [D, 256], F32)
    cw = din(nc, "rcw", [128, 2, 4], F32)
    cb = din(nc, "rcb", [128, 2], F32)
    rgw = din(nc, "rgw", [128, 8, 128], F32)
    rgb = din(nc, "rgb", [128, 8], F32)
    lam = din(nc, "lam", [128, 4], F32)
    yrT = dout(nc, "yrT", [256, NT], BF16)
    P = Prog(nc)
    cx = Ctx(P, wslots=2, wcols=KD * 256, ntmp=10, ln=False)
    twx, rwx = cx.wtile(wx, KD, 256)
    twg, rwg = cx.wtile(wg, KD, 256)
    small = {}
    r_small = Res()
    for name, src, shape in (("cw", cw, [128, 2, 4]), ("cb", cb, [128, 2]), ("rgw", rgw, [128, 8, 128]), ("rgb", rgb, [128, 8]), ("lam", lam, [128, 4])):
        small[name] = P.sbuf(shape, F32)
        P.dma("sp", small[name][:], src, writes=[r_small])
    clam = P.sbuf([128, 4], F32)
    clam2 = P.sbuf([128, 4], F32)
    r_cl = Res()
    P.add("act", lambda e: e.activation(clam[:], small["lam"][:], AF.Exp, scale=-1.0), reads=[r_small], writes=[r_cl])
    P.add("act", lambda e: e.activation(clam[:], clam[:], AF.Ln, bias=1.0), reads=[r_cl], writes=[r_cl])
    P.add("dve", lambda e: e.tensor_scalar_mul(clam2[:], clam[:], -16.0), reads=[r_cl], writes=[r_cl])
    P.add("dve", lambda e: e.tensor_scalar_mul(clam[:], clam[:], -8.0), reads=[r_cl], writes=[r_cl])

    ub = Rot(P, 2, [128, KD, 512], BF16)
    XR = [P.sbuf([128, TPB + 6], F32) for _ in range(2)]
    rxr = [Res(), Res()]
    XC = [P.sbuf([128, TPB], F32) for _ in range(2)]
    rxc = [Res(), Res()]
    GB = [P.sbuf([128, TPB], BF16) for _ in range(2)]
    rgbuf = [Res(), Res()]
    H = [P.sbuf([128, TPB + 1], F32) for _ in range(2)]
    rH = [Res(), Res()]
    for ch in range(2):
        P.add("dve", lambda e, ch=ch: e.memset(XR[ch][:], 0.0), writes=[rxr[ch]])
    A, B = XR[0], XR[1]
    segs = ((0, 0, CTX), (CTX + 3, CTX, SEQ))

    def posraw(tok):
        return tok + 2 if tok < CTX else tok + 5

    def do_batch(b):
        base = b * TPB
        if b > 0:
            for ch in range(2):
                for col in (0, CTX + 2, TPB + 5):
                    w = 2 if col != TPB + 5 else 1
                    if col == CTX + 2:
                        w = 3
                    P.add("dve", lambda e, ch=ch, col=col, w=w: e.memset(XR[ch][:, col:col + w], 0.0), writes=[rxr[ch]])
        for (tok0, G) in batch_groups():
            ubuf, rub = ub.next()
            P.dma("sp", ubuf[:, :, 0:G], uT[:, base + tok0:base + tok0 + G].rearrange("(k p) t -> p k t", p=128), writes=[rub])
            for ch in range(2):
                px, rpx = fm_mm(cx, twx, rwx, ch * 128, lambda k, ubuf=ubuf, G=G: ubuf[:, k, 0:G], lambda k, rub=rub: rub, G)
                P.add("act", lambda e, ch=ch, px=px, tok0=tok0, G=G: e.copy(XR[ch][:, posraw(tok0):posraw(tok0) + G], px[:, 0:G]),
                      reads=[rpx], writes=[rxr[ch]])
                pg, rpg = fm_mm(cx, twg, rwg, ch * 128, lambda k, ubuf=ubuf, G=G: ubuf[:, k, 0:G], lambda k, rub=rub: rub, G)
                t, rt = cx.tmp.next()
                P.add("act", lambda e, pg=pg, t=t, G=G: e.activation(t[:, 0:G], pg[:, 0:G], AF.Square), reads=[rpg], writes=[rt])
                P.add("dve", lambda e, t=t, G=G: e.tensor_scalar(t[:, 0:G], t[:, 0:G], GELU_C, 1.0, ALU.mult, ALU.add), reads=[rt], writes=[rt])
                P.add("dve", lambda e, pg=pg, t=t, G=G: e.tensor_tensor(t[:, 0:G], t[:, 0:G], pg[:, 0:G], ALU.mult), reads=[rt, rpg], writes=[rt])
                P.add("act", lambda e, t=t, G=G: e.activation(t[:, 0:G], t[:, 0:G], AF.Sigmoid, scale=GELU_S), reads=[rt], writes=[rt])
                P.add("dve", lambda e, ch=ch, pg=pg, t=t, tok0=tok0, G=G: e.tensor_tensor(GB[ch][:, tok0:tok0 + G], t[:, 0:G], pg[:, 0:G], ALU.mult),
                      reads=[rt, rpg], writes=[rgbuf[ch]])
        for ch in range(2):
            for (praw, ptok, L) in segs:
                for h0 in range(0, L, 2048):
                    hl = min(2048, L - h0)
                    o = XC[ch][:, ptok + h0:ptok + h0 + hl]
                    P.add("dve", lambda e, ch=ch, o=o, praw=praw, h0=h0, hl=hl: e.tensor_scalar(
                        o, XR[ch][:, praw + h0:praw + h0 + hl], small["cw"][:, ch, 0:1], small["cb"][:, ch:ch + 1], ALU.mult, ALU.add),
                        reads=[rxr[ch], r_small], writes=[rxc[ch]])
                    for j in range(1, 4):
                        P.add("dve", lambda e, ch=ch, o=o, praw=praw, h0=h0, hl=hl, j=j: e.scalar_tensor_tensor(
                            o, XR[ch][:, praw + h0 + j:praw + h0 + j + hl], small["cw"][:, ch, j:j + 1], o, ALU.mult, ALU.add),
                            reads=[rxr[ch], r_small, rxc[ch]], writes=[rxc[ch]])
        for ch in range(2):
            for d in range(2):
                ia = (d * 2 + ch) * 2
                grps = batch_groups()
                for g0 in range(0, len(grps), 3):
                    items = []
                    for (tok0, G) in grps[g0:g0 + 3]:
                        sb_, sl = (0, CTX) if tok0 < CTX else (CTX, SEQ)
                        if d == 0:
                            oa = A[:, 1 + tok0:1 + tok0 + G]
                            obb = B[:, 1 + tok0:1 + tok0 + G]
                        else:
                            phi = sb_ + sl - 1 - (tok0 - sb_)
                            oa = A[:, 1 + phi:1 + phi - G:-1]
                            obb = B[:, 1 + phi:1 + phi - G:-1]
                        xcs = XC[ch][:, tok0:tok0 + G]
                        pr, rpr = cx.mm.next()
                        P.add("pe", lambda e, pr=pr, xcs=xcs, G=G, ia=ia: e.matmul(pr[:, 0:G], small["rgw"][:, ia, :], xcs, start=True, stop=True),
                              reads=[r_small, rxc[ch]], writes=[rpr])
                        pi, rpi = cx.mm.next()
                        P.add("pe", lambda e, pi=pi, xcs=xcs, G=G, ia=ia: e.matmul(pi[:, 0:G], small["rgw"][:, ia + 1, :], xcs, start=True, stop=True),
                              reads=[r_small, rxc[ch]], writes=[rpi])
                        r_, rr = cx.tmp.next()
                        s_, rs = cx.tmp.next()
                        i_, ri = cx.tmp.next()
                        items.append((G, oa, obb, xcs, pr, rpr, pi, rpi, r_, rr, s_, rs, i_, ri))
                    for (G, oa, obb, xcs, pr, rpr, pi, rpi, r_, rr, s_, rs, i_, ri) in items:
                        P.add("act", lambda e, pr=pr, r_=r_, G=G, ia=ia: e.activation(r_[:, 0:G], pr[:, 0:G], AF.Sigmoid, bias=small["rgb"][:, ia:ia + 1]),
                              reads=[rpr, r_small], writes=[rr])
                        P.add("act", lambda e, pi=pi, i_=i_, G=G, ia=ia: e.activation(i_[:, 0:G], pi[:, 0:G], AF.Sigmoid, bias=small["rgb"][:, ia + 1:ia + 2]),
                              reads=[rpi, r_small], writes=[ri])
                    for (G, oa, obb, xcs, pr, rpr, pi, rpi, r_, rr, s_, rs, i_, ri) in items:
                        P.add("act", lambda e, r_=r_, oa=oa, G=G, d=d, ch=ch: e.activation(oa, r_[:, 0:G], AF.Exp, scale=clam[:, d * 2 + ch:d * 2 + ch + 1]),
                              reads=[rr, r_cl], writes=[rxr[0]])
                        P.add("act", lambda e, r_=r_, s_=s_, G=G, d=d, ch=ch: e.activation(s_[:, 0:G], r_[:, 0:G], AF.Exp, scale=clam2[:, d * 2 + ch:d * 2 + ch + 1]),
                              reads=[rr, r_cl], writes=[rs])
                    for (G, oa, obb, xcs, pr, rpr, pi, rpi, r_, rr, s_, rs, i_, ri) in items:
                        P.add("act", lambda e, s_=s_, G=G: e.activation(s_[:, 0:G], s_[:, 0:G], AF.Ln, bias=1.0, scale=-1.0), reads=[rs], writes=[rs])
                    for (G, oa, obb, xcs, pr, rpr, pi, rpi, r_, rr, s_, rs, i_, ri) in items:
                        P.add("act", lambda e, s_=s_, G=G: e.activation(s_[:, 0:G], s_[:, 0:G], AF.Exp, scale=0.5), reads=[rs], writes=[rs])
                    for (G, oa, obb, xcs, pr, rpr, pi, rpi, r_, rr, s_, rs, i_, ri) in items:
                        P.add("dve", lambda e, s_=s_, i_=i_, G=G: e.tensor_tensor(s_[:, 0:G], s_[:, 0:G], i_[:, 0:G], ALU.mult), reads=[rs, ri], writes=[rs])
                        P.add("dve", lambda e, s_=s_, obb=obb, xcs=xcs, G=G: e.tensor_tensor(obb, s_[:, 0:G], xcs, ALU.mult), reads=[rs, rxc[ch]], writes=[rxr[1]])
                half = TPB // 2
                P.add("dve", lambda e, d=d: e.tensor_tensor_scan(H[d][:, 1:1 + half], A[:, 1:1 + half], B[:, 1:1 + half], 0.0, ALU.mult, ALU.add),
                      reads=[rxr[0], rxr[1]], writes=[rH[d]])
                P.add("dve", lambda e, d=d: e.tensor_tensor_scan(H[d][:, 1 + half:1 + TPB], A[:, 1 + half:1 + TPB], B[:, 1 + half:1 + TPB],
                                                                 H[d][:, half:half + 1], ALU.mult, ALU.add),
                      reads=[rxr[0], rxr[1], rH[d]], writes=[rH[d]])
            for (praw, ptok, L) in segs:
                P.add("dve", lambda e, ptok=ptok, L=L: e.tensor_tensor(H[0][:, 1 + ptok:1 + ptok + L], H[1][:, ptok + L:ptok:-1],
                                                                       H[0][:, 1 + ptok:1 + ptok + L], ALU.add),
                      reads=[rH[0], rH[1]], writes=[rH[0]])
            P.add("dve", lambda e, ch=ch: e.tensor_tensor(GB[ch][:], H[0][:, 1:1 + TPB], GB[ch][:], ALU.mult), reads=[rH[0], rgbuf[ch]], writes=[rgbuf[ch]])
            P.dma("sp", yrT[ch * 128:(ch + 1) * 128, base:base + TPB], GB[ch][:], reads=[rgbuf[ch]])

    for b in range(nbatch):
        do_batch(b)
    P.emit()
    return nc


AM2_POOL = "pool"
AM2_BLK = 4
AM2_LAG = 2
MG = 256
NCH = TPB // 128
DH = 256


def build_Am(nbatch, nc=None):
    NT = nbatch * TPB
    nc = nc or new_nc()
    uT = din(nc, "uTm", [D, NT], BF16)
    wq = din(nc, "wq", [D, 256], F32)
    wk = din(nc, "wk", [D, 256], F32)
    wvo = din(nc, "wvo", [D, 512], F32)
    wgt = din(nc, "wgt", [D, 4], F32)
    gb = din(nc, "gb", [128, 4], F32)
    cwd = din(nc, "cw", [128, 4, 4], F32)
    cbd = din(nc, "cb", [128, 4], F32)
    gn = din(nc, "gn", [128, 256], F32)
    ident = din(nc, "ident", [128, 128], BF16)
    tri = din(nc, "tri", [128, 2, 128], F32)
    negm = din(nc, "negm", [128, 2, 128], F32)
    ym = dout(nc, "ym", [NT, 256], BF16)
    P = Prog(nc)
    ps = Rot(P, 6, [128, 512], F32, psum=True)
    pt = Rot(P, 2, [128, 512], BF16, psum=True)
    tmp = Rot(P, 6, [128, MG + 3], F32)
    wqs = P.sbuf([128, KD, 256], BF16)
    wks = P.sbuf([128, KD, 256], BF16)
    wvos = P.sbuf([128, KD, 512], BF16)
    wgts = P.sbuf([128, KD, 4], BF16)
    r_w = Res()
    for dst, src in ((wqs, wq), (wks, wk), (wvos, wvo), (wgts, wgt)):
        P.dma("pool", dst[:], src.rearrange("(k p) c -> p k c", p=128), writes=[r_w])
    cst = {}
    r_c = Res()
    for name, src, shape, dt in (("gb", gb, [128, 4], F32), ("cw", cwd, [128, 4, 4], F32), ("cb", cbd, [128, 4], F32), ("gn", gn, [128, 256], F32),
                                 ("ident", ident, [128, 128], BF16), ("tri", tri, [128, 2, 128], F32), ("negm", negm, [128, 2, 128], F32)):
        cst[name] = P.sbuf(shape, dt)
        P.dma("sp", cst[name][:], src, writes=[r_c])
    ones = P.sbuf([128, 128], F32)
    P.add("dve", lambda e: e.memset(ones[:], 1.0), writes=[r_c])

    ub = Rot(P, 2, [128, KD, MG + 3], BF16)
    QK = [P.sbuf([128, TPB], BF16) for _ in range(4)]
    rqk = [[Res() for _ in range(NCH // 2)] for _ in range(4)]
    VA = P.sbuf([128, NCH, DH + 1], BF16)
    rva = [Res() for _ in range(NCH)]
    SO = P.sbuf([128, NCH, DH], BF16)
    rso = [Res() for _ in range(NCH)]
    GT = P.sbuf([128, NCH, 4], F32)
    rgt = Res()
    HF = P.sbuf([128, NCH, DH], F32)
    rhf = [Res() for _ in range(NCH)]
    C32 = [P.sbuf([128, DH + 1], F32) for _ in range(2)]
    CB = [P.sbuf([128, DH + 1], BF16) for _ in range(2)]
    rc32 = [Res(), Res()]
    rcb = [Res(), Res()]
    t128 = Rot(P, 8, [128, 128], F32)
    b128 = Rot(P, 8, [128, 128], BF16)
    kwr = Rot(P, 2, [128, DH], BF16)
    vsm = Rot(P, 16, [128, 8], F32)
    hsr = Rot(P, 3, [128, DH], F32)
    obr = Rot(P, 3, [128, DH], BF16)
    P.add("dve", lambda e: e.memset(VA[:, :, DH:DH + 1], 1.0), writes=rva)
    QSCALE = DH ** -0.5

    def do_group(base, g, t0, first, last):
        ubuf, rub = ub.next()
        hl = 0 if first else 2
        hr = 0 if last else 1
        if first:
            P.add("dve", lambda e: e.memset(ubuf[:, :, 0:2], 0.0), writes=[rub])
        if last:
            P.add("dve", lambda e: e.memset(ubuf[:, :, MG + 2:MG + 3], 0.0), writes=[rub])
        P.dma("sp", ubuf[:, :, 2 - hl:2 + MG + hr],
              uT[:, base + t0 - hl:base + t0 + MG + hr].rearrange("(k p) t -> p k t", p=128), writes=[rub])
        for ci in range(4):
            wsb = wqs if ci < 2 else wks
            c0 = (ci % 2) * 128
            pb, rp = ps.next()
            for k in range(KD):
                P.add("pe", lambda e, k=k, pb=pb, wsb=wsb, c0=c0: e.matmul(pb[:, 0:MG + 3], wsb[:, k, c0:c0 + 128], ubuf[:, k, :], start=(k == 0), stop=(k == KD - 1)),
                      reads=[r_w, rub], writes=[rp])
            cv, rcv = tmp.next()
            sg, rsg = tmp.next()
            P.add("dve", lambda e, pb=pb, cv=cv, ci=ci: e.tensor_scalar(cv[:, 0:MG], pb[:, 0:MG], cst["cw"][:, ci, 0:1], cst["cb"][:, ci:ci + 1], ALU.mult, ALU.add),
                  reads=[rp, r_c], writes=[rcv])
            for j in range(1, 4):
                P.add("dve", lambda e, pb=pb, cv=cv, ci=ci, j=j: e.scalar_tensor_tensor(cv[:, 0:MG], pb[:, j:j + MG], cst["cw"][:, ci, j:j + 1], cv[:, 0:MG], ALU.mult, ALU.add),
                      reads=[rp, r_c, rcv], writes=[rcv])
            P.add("act", lambda e, cv=cv, sg=sg: e.activation(sg[:, 0:MG], cv[:, 0:MG], AF.Sigmoid), reads=[rcv], writes=[rsg])
            sc = QSCALE if ci < 2 else 1.0
            P.add("dve", lambda e, cv=cv, sg=sg, ci=ci, sc=sc: e.scalar_tensor_tensor(QK[ci][:, t0:t0 + MG], cv[:, 0:MG], sc, sg[:, 0:MG], ALU.mult, ALU.mult),
                  reads=[rcv, rsg], writes=[rqk[ci][g]])
        for i in range(2):
            c = 2 * g + i
            pv, rpv = ps.next()
            pg, rpg = ps.next()
            for k in range(KD):
                P.add("pe", lambda e, k=k, pv=pv, i=i: e.matmul(pv[:, 0:512], ubuf[:, k, 2 + i * 128:2 + (i + 1) * 128], wvos[:, k, :], start=(k == 0), stop=(k == KD - 1)),
                      reads=[r_w, rub], writes=[rpv])
            for k in range(KD):
                P.add("pe", lambda e, k=k, pg=pg, i=i: e.matmul(pg[:, 0:4], ubuf[:, k, 2 + i * 128:2 + (i + 1) * 128], wgts[:, k, :], start=(k == 0), stop=(k == KD - 1)),
                      reads=[r_w, rub], writes=[rpg])
            P.add("act", lambda e, pv=pv, c=c: e.copy(VA[:, c, 0:DH], pv[:, 0:DH]), reads=[rpv], writes=[rva[c]])
            P.add("act", lambda e, pv=pv, c=c: e.activation(SO[:, c, :], pv[:, DH:2 * DH], AF.Sigmoid), reads=[rpv], writes=[rso[c]])
            P.add("dve", lambda e, pg=pg, c=c: e.tensor_tensor(GT[:, c, :], pg[:, 0:4], cst["gb"][:], ALU.add), reads=[rpg, r_c], writes=[rgt])

    def do_chunk(base, c, d, finalize):
        cs = slice(c * 128, (c + 1) * 128)
        g = c // 2
        ig = GT[:, c, 2 * d:2 * d + 1]
        lf = GT[:, c, 2 * d + 1:2 * d + 2]
        lastcol = 127 if d == 0 else 0
        LU, rLU = t128.next()
        P.add("dve", lambda e: e.tensor_scalar(LU[:], cst["tri"][:, d, :], lf, None, ALU.mult), reads=[r_c, rgt], writes=[rLU])
        prow, rprow = ps.next()
        P.add("pe", lambda e: e.matmul(prow[:, 0:128], ones[:], LU[:], start=True, stop=True), reads=[r_c, rLU], writes=[rprow])
        pcol, rpcol = ps.next()
        P.add("pe", lambda e: e.matmul(pcol[:, 0:1], cst["tri"][:, d, :], lf, start=True, stop=True), reads=[r_c, rgt], writes=[rpcol])
        sv, rsv = vsm.next()
        P.add("dve", lambda e: e.tensor_tensor(sv[:, 0:1], ig, pcol[:, 0:1], ALU.subtract), reads=[rgt, rpcol], writes=[rsv])
        P.add("dve", lambda e: e.tensor_copy(sv[:, 1:2], prow[:, lastcol:lastcol + 1]), reads=[rprow], writes=[rsv])
        DT, rDT = t128.next()
        P.add("dve", lambda e: e.tensor_tensor(DT[:], prow[:, 0:128], cst["negm"][:, d, :], ALU.add), reads=[rprow, r_c], writes=[rDT])
        P.add("act", lambda e: e.activation(DT[:], DT[:], AF.Exp, bias=sv[:, 0:1]), reads=[rDT, rsv], writes=[rDT])
        EB, rEB = t128.next()
        P.add("act", lambda e: e.activation(EB[:], prow[:, 0:128], AF.Exp), reads=[rprow], writes=[rEB])
        QS = []
        for i in range(2):
            qs, rqs = b128.next()
            P.add("dve", lambda e, i=i, qs=qs: e.tensor_tensor(qs[:], QK[i][:, cs], EB[:], ALU.mult), reads=[rqk[i][g], rEB], writes=[rqs])
            QS.append((qs, rqs))
        pst, rpst = ps.next()
        for i in range(2):
            P.add("pe", lambda e, i=i: e.matmul(pst[:, 0:128], QK[2 + i][:, cs], QK[i][:, cs], start=(i == 0), stop=(i == 1)),
                  reads=[rqk[2 + i][g], rqk[i][g]], writes=[rpst])
        ST, rST = b128.next()
        P.add("dve", lambda e: e.tensor_tensor(ST[:], pst[:, 0:128], DT[:], ALU.mult), reads=[rpst, rDT], writes=[rST])
        pnum, rpnum = ps.next()
        P.add("pe", lambda e: e.matmul(pnum[:, 0:DH + 1], ST[:], VA[:, c, :], start=True, stop=False), reads=[rST, rva[c]], writes=[rpnum])
        for i in range(2):
            P.add("pe", lambda e, i=i: e.matmul(pnum[:, 0:DH + 1], QS[i][0][:], CB[i][:], start=False, stop=(i == 1)),
                  reads=[QS[i][1], rcb[i]], writes=[rpnum])
        P.add("act", lambda e: e.activation(sv[:, 4:5], pnum[:, DH:DH + 1], AF.Abs), reads=[rpnum], writes=[rsv])
        P.add("dve", lambda e: e.tensor_scalar_max(sv[:, 4:5], sv[:, 4:5], 1.0), reads=[rsv], writes=[rsv])
        P.add("dve", lambda e: e.reciprocal(sv[:, 5:6], sv[:, 4:5]), reads=[rsv], writes=[rsv])
        if not finalize:
            P.add("act", lambda e: e.activation(HF[:, c, :], pnum[:, 0:DH], AF.Copy, scale=sv[:, 5:6]), reads=[rpnum, rsv], writes=[rhf[c]])
        else:
            hs, rhs = hsr.next()
            P.add("dve", lambda e: e.scalar_tensor_tensor(hs[:], pnum[:, 0:DH], sv[:, 5:6], HF[:, c, :], ALU.mult, ALU.add), reads=[rpnum, rsv, rhf[c]], writes=[rhs])
            st6, rst6 = vsm.next()
            P.add("dve", lambda e: e.bn_stats(st6[:, 0:6], hs[:]), reads=[rhs], writes=[rst6])
            mv, rmv = vsm.next()
            P.add("dve", lambda e: e.bn_aggr(mv[:, 0:2], st6[:, 0:6]), reads=[rst6], writes=[rmv])
            P.add("act", lambda e: e.activation(mv[:, 2:3], mv[:, 1:2], AF.Sqrt, bias=LN_EPS, scale=1.0), reads=[rmv], writes=[rmv])
            P.add("dve", lambda e: e.reciprocal(mv[:, 2:3], mv[:, 2:3]), reads=[rmv], writes=[rmv])
            P.add("dve", lambda e: e.tensor_scalar(hs[:], hs[:], mv[:, 0:1], mv[:, 2:3], ALU.subtract, ALU.mult), reads=[rhs, rmv], writes=[rhs])
            P.add("dve", lambda e: e.tensor_tensor(hs[:], hs[:], cst["gn"][:], ALU.mult), reads=[rhs, r_c], writes=[rhs])
            ob, rob = obr.next()
            P.add("dve", lambda e: e.tensor_tensor(ob[:], hs[:], SO[:, c, :], ALU.mult), reads=[rhs, rso[c]], writes=[rob])
            P.dma("sp", ym[base + c * 128:base + (c + 1) * 128, :], ob[:], reads=[rob])
        P.add("act", lambda e: e.activation(sv[:, 2:3], sv[:, 0:1], AF.Exp, bias=sv[:, 1:2]), reads=[rsv], writes=[rsv])
        P.add("act", lambda e: e.activation(sv[:, 3:4], sv[:, 1:2], AF.Exp), reads=[rsv], writes=[rsv])
        kw, rkw = kwr.next()
        for i in range(2):
            ptb, rpt = pt.next()
            P.add("pe", lambda e, i=i, ptb=ptb: e.transpose(ptb[:, 0:128], QK[2 + i][:, cs], cst["ident"][:]), reads=[rqk[2 + i][g], r_c], writes=[rpt])
            P.add("dve", lambda e, i=i, ptb=ptb: e.tensor_scalar(kw[:, i * 128:(i + 1) * 128], ptb[:, 0:128], sv[:, 2:3], None, ALU.mult), reads=[rpt, rsv], writes=[rkw])
        pcus = [ps.next(), ps.next()]
        for i in range(2):
            P.add("pe", lambda e, i=i: e.matmul(pcus[i][0][:, 0:DH + 1], kw[:, i * 128:(i + 1) * 128], VA[:, c, :], start=True, stop=True),
                  reads=[rkw, rva[c]], writes=[pcus[i][1]])
        for i in range(2):
            P.add("dve", lambda e, i=i: e.scalar_tensor_tensor(C32[i][:], C32[i][:], sv[:, 3:4], pcus[i][0][:, 0:DH + 1], ALU.mult, ALU.add),
                  reads=[rc32[i], rsv, pcus[i][1]], writes=[rc32[i]])
            P.add("act", lambda e, i=i: e.copy(CB[i][:], C32[i][:]), reads=[rc32[i]], writes=[rcb[i]])

    def zero_state():
        for i in range(2):
            P.add("dve", lambda e, i=i: e.memset(C32[i][:], 0.0), writes=[rc32[i]])
            P.add("dve", lambda e, i=i: e.memset(CB[i][:], 0.0), writes=[rcb[i]])

    def do_batch(b):
        base = b * TPB
        ngrp = TPB // MG
        for g in range(ngrp):
            first = g in (0, 1)
            last = g in (0, ngrp - 1)
            do_group(base, g, g * MG, first, last)
        for col in (1, 3):
            P.add("act", lambda e, col=col: e.activation(GT[:, :, col], GT[:, :, col], AF.Exp, scale=-1.0), reads=[rgt], writes=[rgt])
            P.add("act", lambda e, col=col: e.activation(GT[:, :, col], GT[:, :, col], AF.Ln, bias=1.0), reads=[rgt], writes=[rgt])
            P.add("dve", lambda e, col=col: e.tensor_scalar_mul(GT[:, :, col], GT[:, :, col], -1.0), reads=[rgt], writes=[rgt])
        zero_state()
        for c in range(NCH):
            do_chunk(base, c, 0, False)
        zero_state()
        for c in [1, 0] + list(range(NCH - 1, 1, -1)):
            do_chunk(base, c, 1, True)

    for b in range(nbatch):
        do_batch(b)
    P.emit()
    return nc


def build_Am2(nbatch, nc=None):
    NT = nbatch * TPB
    nc = nc or new_nc()
    uT = din(nc, "uTm", [D, NT], BF16)
    wq = din(nc, "wq", [D, 256], F32)
    wk = din(nc, "wk", [D, 256], F32)
    wvo = din(nc, "wvo", [D, 512], F32)
    wgt = din(nc, "wgt", [D, 4], F32)
    gb = din(nc, "gb", [128, 4], F32)
    cwd = din(nc, "cw", [128, 4, 4], F32)
    cbd = din(nc, "cb", [128, 4], F32)
    gn = din(nc, "gn", [128, 256], F32)
    ident = din(nc, "ident", [128, 128], BF16)
    tri = din(nc, "tri", [128, 2, 128], F32)
    negm = din(nc, "negm", [128, 2, 128], F32)
    ym = dout(nc, "ym", [NT, 256], BF16)
    P = Prog(nc)
    banks = [P.psum([128, 512], F32) for _ in range(7)]
    rbk = [Res(excl=True) for _ in range(7)]
    tbank = P.psum([128, 1024], BF16)
    rtb = Res(excl=True)
    ps = Rot(P, 0, None, None)
    ps.bufs, ps.res, ps.n = banks[0:6], rbk[0:6], 6
    tmp = Rot(P, 6, [128, MG + 3], F32)
    wqs = P.sbuf([128, KD, 256], BF16)
    wks = P.sbuf([128, KD, 256], BF16)
    wvos = P.sbuf([128, KD, 512], BF16)
    wgts = P.sbuf([128, KD, 4], BF16)
    r_w = Res()
    for dst, src in ((wqs, wq), (wks, wk), (wvos, wvo), (wgts, wgt)):
        P.dma("pool", dst[:], src.rearrange("(k p) c -> p k c", p=128), writes=[r_w])
    cst = {}
    r_c = Res()
    for name, src, shape, dt in (("gb", gb, [128, 4], F32), ("cw", cwd, [128, 4, 4], F32), ("cb", cbd, [128, 4], F32), ("gn", gn, [128, 256], F32),
                                 ("ident", ident, [128, 128], BF16), ("tri", tri, [128, 2, 128], F32), ("negm", negm, [128, 2, 128], F32)):
        cst[name] = P.sbuf(shape, dt)
        P.dma("sp", cst[name][:], src, writes=[r_c])
    ones = P.sbuf([128, 128], F32)
    P.add("dve", lambda e: e.memset(ones[:], 1.0), writes=[r_c])

    ub = Rot(P, 2, [128, KD, MG + 3], BF16)
    QK = [P.sbuf([128, TPB], BF16) for _ in range(4)]
    rqk = [[Res() for _ in range(NCH // 2)] for _ in range(4)]
    VA = P.sbuf([128, NCH, DH + 1], BF16)
    rva = [Res() for _ in range(NCH)]
    SO = P.sbuf([128, NCH, DH], BF16)
    rso = [Res() for _ in range(NCH)]
    GT = P.sbuf([128, NCH, 4], F32)
    rgt = Res()
    HF = P.sbuf([128, NCH, DH], F32)
    rhf = [Res() for _ in range(NCH)]
    C32 = P.sbuf([128, 2, DH + 1], F32)
    rc32 = Res()
    NS = 8
    LUa = P.sbuf([128, NS, 128], F32)
    PRa = P.sbuf([128, NS, 128], F32)
    rPR = [Res() for _ in range(NS)]
    DTa = P.sbuf([128, NS, 128], F32)
    EBa = P.sbuf([128, NS, 128], F32)
    QSa = P.sbuf([128, NS, 2, 128], BF16)
    STa = P.sbuf([128, NS, 128], BF16)
    KWa = P.sbuf([128, NS, DH], BF16)
    SVa = P.sbuf([128, NS, 8], F32)
    rLU = [Res() for _ in range(NS)]
    rDT = [Res() for _ in range(NS)]
    rEB = [Res() for _ in range(NS)]
    rQS = [Res() for _ in range(NS)]
    rST = [Res() for _ in range(NS)]
    rKW = [Res() for _ in range(NS)]
    rSV = [Res() for _ in range(NS)]
    CUs = Rot(P, 4, [128, 2, DH + 1], F32)
    CBr = [P.sbuf([128, 2, DH + 16], BF16) for _ in range(4)]
    rCBr = [Res() for _ in range(4)]
    vsm = Rot(P, 16, [128, 8], F32)
    hsr = Rot(P, 3, [128, DH], F32)
    obr = Rot(P, 3, [128, DH], BF16)
    P.add("dve", lambda e: e.memset(VA[:, :, DH:DH + 1], 1.0), writes=rva)
    QSCALE = DH ** -0.5

    def do_group(base, g, t0, first, last):
        ubuf, rub = ub.next()
        hl = 0 if first else 2
        hr = 0 if last else 1
        if first:
            P.add("dve", lambda e: e.memset(ubuf[:, :, 0:2], 0.0), writes=[rub])
        if last:
            P.add("dve", lambda e: e.memset(ubuf[:, :, MG + 2:MG + 3], 0.0), writes=[rub])
        P.dma("sp", ubuf[:, :, 2 - hl:2 + MG + hr],
              uT[:, base + t0 - hl:base + t0 + MG + hr].rearrange("(k p) t -> p k t", p=128), writes=[rub])
        for ci in range(4):
            wsb = wqs if ci < 2 else wks
            c0 = (ci % 2) * 128
            pb, rp = ps.next()
            for k in range(KD):
                P.add("pe", lambda e, k=k, pb=pb, wsb=wsb, c0=c0: e.matmul(pb[:, 0:MG + 3], wsb[:, k, c0:c0 + 128], ubuf[:, k, :], start=(k == 0), stop=(k == KD - 1)),
                      reads=[r_w, rub], writes=[rp])
            cv, rcv = tmp.next()
            sg, rsg = tmp.next()
            P.add("dve", lambda e, pb=pb, cv=cv, ci=ci: e.tensor_scalar(cv[:, 0:MG], pb[:, 0:MG], cst["cw"][:, ci, 0:1], cst["cb"][:, ci:ci + 1], ALU.mult, ALU.add),
                  reads=[rp, r_c], writes=[rcv])
            for j in range(1, 4):
                P.add("dve", lambda e, pb=pb, cv=cv, ci=ci, j=j: e.scalar_tensor_tensor(cv[:, 0:MG], pb[:, j:j + MG], cst["cw"][:, ci, j:j + 1], cv[:, 0:MG], ALU.mult, ALU.add),
                      reads=[rp, r_c, rcv], writes=[rcv])
            P.add("act", lambda e, cv=cv, sg=sg: e.activation(sg[:, 0:MG], cv[:, 0:MG], AF.Sigmoid), reads=[rcv], writes=[rsg])
            sc = QSCALE if ci < 2 else 1.0
            P.add("dve", lambda e, cv=cv, sg=sg, ci=ci, sc=sc: e.scalar_tensor_tensor(QK[ci][:, t0:t0 + MG], cv[:, 0:MG], sc, sg[:, 0:MG], ALU.mult, ALU.mult),
                  reads=[rcv, rsg], writes=[rqk[ci][g]])
        for i in range(2):
            c = 2 * g + i
            pv, rpv = ps.next()
            pg, rpg = ps.next()
            for k in range(KD):
                P.add("pe", lambda e, k=k, pv=pv, i=i: e.matmul(pv[:, 0:512], ubuf[:, k, 2 + i * 128:2 + (i + 1) * 128], wvos[:, k, :], start=(k == 0), stop=(k == KD - 1)),
                      reads=[r_w, rub], writes=[rpv])
            for k in range(KD):
                P.add("pe", lambda e, k=k, pg=pg, i=i: e.matmul(pg[:, 0:4], ubuf[:, k, 2 + i * 128:2 + (i + 1) * 128], wgts[:, k, :], start=(k == 0), stop=(k == KD - 1)),
                      reads=[r_w, rub], writes=[rpg])
            P.add("act", lambda e, pv=pv, c=c: e.copy(VA[:, c, 0:DH], pv[:, 0:DH]), reads=[rpv], writes=[rva[c]])
            P.add("act", lambda e, pv=pv, c=c: e.activation(SO[:, c, :], pv[:, DH:2 * DH], AF.Sigmoid), reads=[rpv], writes=[rso[c]])
            P.add("dve", lambda e, pg=pg, c=c: e.tensor_tensor(GT[:, c, :], pg[:, 0:4], cst["gb"][:], ALU.add), reads=[rpg, r_c], writes=[rgt])

    def block_X(base, chunks, d, par):
        lastcol = 127 if d == 0 else 0
        sl = [par * 4 + i for i in range(len(chunks))]

        def part0():
            for i, c in enumerate(chunks):
                P.add("dve", lambda e, i=i, c=c: e.tensor_scalar(LUa[:, sl[i], :], cst["tri"][:, d, :], GT[:, c, 2 * d + 1:2 * d + 2], None, ALU.mult),
                      reads=[r_c, rgt], writes=[rLU[sl[i]]])

        def part1():
            for i, c in enumerate(chunks):
                P.add("pe", lambda e, i=i: e.matmul(banks[0][:, i * 128:(i + 1) * 128], ones[:], LUa[:, sl[i], :], start=True, stop=True),
                      reads=[r_c, rLU[sl[i]]], writes=[rbk[0]])
                P.add("pe", lambda e, i=i, c=c: e.matmul(banks[2][:, 16 * i:16 * i + 1], cst["tri"][:, d, :], GT[:, c, 2 * d + 1:2 * d + 2], start=True, stop=True),
                      reads=[r_c, rgt], writes=[rbk[2]])

        def part2():
            for i, c in enumerate(chunks):
                P.add("act", lambda e, i=i: e.copy(PRa[:, sl[i], :], banks[0][:, i * 128:(i + 1) * 128]), reads=[rbk[0]], writes=[rPR[sl[i]]])
            for i, c in enumerate(chunks):
                sv = SVa[:, sl[i], :]
                P.add("dve", lambda e, i=i, c=c, sv=sv: e.tensor_tensor(sv[:, 0:1], GT[:, c, 2 * d:2 * d + 1], banks[2][:, 16 * i:16 * i + 1], ALU.subtract),
                      reads=[rgt, rbk[2]], writes=[rSV[sl[i]]])
                P.add("dve", lambda e, i=i: e.tensor_tensor(DTa[:, sl[i], :], PRa[:, sl[i], :], cst["negm"][:, d, :], ALU.add),
                      reads=[rPR[sl[i]], r_c], writes=[rDT[sl[i]]])
            for i, c in enumerate(chunks):
                sv = SVa[:, sl[i], :]
                P.add("act", lambda e, i=i, sv=sv: e.activation(DTa[:, sl[i], :], DTa[:, sl[i], :], AF.Exp, bias=sv[:, 0:1]), reads=[rDT[sl[i]], rSV[sl[i]]], writes=[rDT[sl[i]]])
                P.add("act", lambda e, i=i: e.activation(EBa[:, sl[i], :], PRa[:, sl[i], :], AF.Exp), reads=[rPR[sl[i]]], writes=[rEB[sl[i]]])
                P.add("act", lambda e, i=i, sv=sv: e.activation(sv[:, 2:3], sv[:, 0:1], AF.Exp, bias=PRa[:, sl[i], lastcol:lastcol + 1]), reads=[rSV[sl[i]], rPR[sl[i]]], writes=[rSV[sl[i]]])
                P.add("act", lambda e, i=i, sv=sv: e.activation(sv[:, 3:4], PRa[:, sl[i], lastcol:lastcol + 1], AF.Exp), reads=[rPR[sl[i]]], writes=[rSV[sl[i]]])

        def part3():
            for i, c in enumerate(chunks):
                cs = slice(c * 128, (c + 1) * 128)
                for h in range(2):
                    P.add(AM2_POOL, lambda e, i=i, h=h, cs=cs: e.tensor_tensor(QSa[:, sl[i], h, :], QK[h][:, cs], EBa[:, sl[i], :], ALU.mult),
                          reads=[rqk[h][c // 2], rEB[sl[i]]], writes=[rQS[sl[i]]])
            for i, c in enumerate(chunks):
                cs = slice(c * 128, (c + 1) * 128)
                for h in range(2):
                    P.add("pe", lambda e, i=i, h=h, cs=cs: e.matmul(banks[1][:, i * 128:(i + 1) * 128], QK[2 + h][:, cs], QK[h][:, cs], start=(h == 0), stop=(h == 1)),
                          reads=[rqk[2 + h][c // 2], rqk[h][c // 2]], writes=[rbk[1]])
                for h in range(2):
                    P.add("pe", lambda e, i=i, h=h, cs=cs: e.transpose(tbank[:, (2 * i + h) * 128:(2 * i + h + 1) * 128], QK[2 + h][:, cs], cst["ident"][:]),
                          reads=[rqk[2 + h][c // 2], r_c], writes=[rtb])
            for i, c in enumerate(chunks):
                sv = SVa[:, sl[i], :]
                P.add("dve", lambda e, i=i: e.tensor_tensor(STa[:, sl[i], :], banks[1][:, i * 128:(i + 1) * 128], DTa[:, sl[i], :], ALU.mult),
                      reads=[rbk[1], rDT[sl[i]]], writes=[rST[sl[i]]])
                P.add("dve", lambda e, i=i, sv=sv: e.tensor_scalar(KWa[:, sl[i], :], tbank[:, 2 * i * 128:(2 * i + 2) * 128], sv[:, 2:3], None, ALU.mult),
                      reads=[rtb, rSV[sl[i]]], writes=[rKW[sl[i]]])

        return [part0, part1, part2, part3]

    def block_YA(base, chunks, d, finalize, par, n0, i):
        sl = [par * 4 + t for t in range(len(chunks))]
        c = chunks[i]
        n = n0 + i
        sv = SVa[:, sl[i], :]
        cu, rcu = CUs.next()
        for h in range(2):
            P.add("pe", lambda e, i=i, h=h, c=c: e.matmul(banks[5 + h][:, 0:DH + 1], KWa[:, sl[i], h * 128:(h + 1) * 128], VA[:, c, :], start=True, stop=True),
                  reads=[rKW[sl[i]], rva[c]], writes=[rbk[5 + h]])
        for h in range(2):
            P.add("act", lambda e, h=h, cu=cu: e.copy(cu[:, h, :], banks[5 + h][:, 0:DH + 1]), reads=[rbk[5 + h]], writes=[rcu])
        P.add("dve", lambda e, cu=cu, sv=sv: e.scalar_tensor_tensor(C32[:], C32[:], sv[:, 3:4], cu[:], ALU.mult, ALU.add),
              reads=[rc32, rSV[sl[i]], rcu], writes=[rc32])
        P.add("act", lambda e, n=n: e.copy(CBr[(n + 1) % 4][:, :, 0:DH + 1], C32[:]), reads=[rc32], writes=[rCBr[(n + 1) % 4]])

    def block_YB(base, chunks, d, finalize, par, n0, i):
        sl = [par * 4 + t for t in range(len(chunks))]
        c = chunks[i]
        n = n0 + i
        sv = SVa[:, sl[i], :]
        pn = banks[3 + (n % 2)]
        rpn = rbk[3 + (n % 2)]
        P.add("pe", lambda e, i=i, c=c, pn=pn: e.matmul(pn[:, 0:DH + 1], STa[:, sl[i], :], VA[:, c, :], start=True, stop=False), reads=[rST[sl[i]], rva[c]], writes=[rpn])
        for h in range(2):
            P.add("pe", lambda e, i=i, h=h, pn=pn, n=n: e.matmul(pn[:, 0:DH + 1], QSa[:, sl[i], h, :], CBr[n % 4][:, h, 0:DH + 1], start=False, stop=(h == 1)),
                  reads=[rQS[sl[i]], rCBr[n % 4]], writes=[rpn])
        dn, rdn = vsm.next()
        P.add("act", lambda e, pn=pn, dn=dn: e.activation(dn[:, 0:1], pn[:, DH:DH + 1], AF.Abs), reads=[rpn], writes=[rdn])
        P.add("dve", lambda e, dn=dn: e.tensor_scalar_max(dn[:, 0:1], dn[:, 0:1], 1.0), reads=[rdn], writes=[rdn])
        P.add("dve", lambda e, dn=dn: e.reciprocal(dn[:, 1:2], dn[:, 0:1]), reads=[rdn], writes=[rdn])
        if not finalize:
            P.add("act", lambda e, pn=pn, dn=dn, c=c: e.activation(HF[:, c, :], pn[:, 0:DH], AF.Copy, scale=dn[:, 1:2]), reads=[rpn, rdn], writes=[rhf[c]])
        else:
            hs, rhs = hsr.next()
            P.add("dve", lambda e, pn=pn, dn=dn, c=c, hs=hs: e.scalar_tensor_tensor(hs[:], pn[:, 0:DH], dn[:, 1:2], HF[:, c, :], ALU.mult, ALU.add),
                  reads=[rpn, rdn, rhf[c]], writes=[rhs])
            P.add("dve", lambda e, dn=dn, hs=hs: e.bn_stats(dn[:, 2:8], hs[:]), reads=[rhs], writes=[rdn])
            mv, rmv = vsm.next()
            P.add("dve", lambda e, dn=dn, mv=mv: e.bn_aggr(mv[:, 0:2], dn[:, 2:8]), reads=[rdn], writes=[rmv])
            P.add("act", lambda e, mv=mv: e.activation(mv[:, 2:3], mv[:, 1:2], AF.Sqrt, bias=LN_EPS, scale=1.0), reads=[rmv], writes=[rmv])
            P.add("dve", lambda e, mv=mv: e.reciprocal(mv[:, 2:3], mv[:, 2:3]), reads=[rmv], writes=[rmv])
            P.add("dve", lambda e, mv=mv, hs=hs: e.tensor_scalar(hs[:], hs[:], mv[:, 0:1], mv[:, 2:3], ALU.subtract, ALU.mult), reads=[rhs, rmv], writes=[rhs])
            P.add(AM2_POOL, lambda e, hs=hs: e.tensor_tensor(hs[:], hs[:], cst["gn"][:], ALU.mult), reads=[rhs, r_c], writes=[rhs])
            ob, rob = obr.next()
            P.add(AM2_POOL, lambda e, hs=hs, ob=ob, c=c: e.tensor_tensor(ob[:], hs[:], SO[:, c, :], ALU.mult), reads=[rhs, rso[c]], writes=[rob])
            P.dma("sp", ym[base + c * 128:base + (c + 1) * 128, :], ob[:], reads=[rob])


    def zero_state():
        P.add("dve", lambda e: e.memset(C32[:], 0.0), writes=[rc32])
        P.add("dve", lambda e: e.memset(CBr[0][:], 0.0), writes=[rCBr[0]])

    blk_counter = [0]

    def do_dir(base, order, d, finalize):
        zero_state()
        blocks = [order[s0:s0 + AM2_BLK] for s0 in range(0, len(order), AM2_BLK)]
        par0 = blk_counter[0]
        for p in block_X(base, blocks[0], d, par0 % 2):
            p()
        pending = []
        for k, chunks in enumerate(blocks):
            nxt = block_X(base, blocks[k + 1], d, (par0 + k + 1) % 2) if k + 1 < len(blocks) else []
            for t in range(max(4, len(chunks))):
                if t < len(nxt):
                    nxt[t]()
                if t < len(chunks):
                    args = (base, chunks, d, finalize, (par0 + k) % 2, k * AM2_BLK, t)
                    block_YA(*args)
                    pending.append(args)
                    if len(pending) > AM2_LAG:
                        block_YB(*pending.pop(0))
        while pending:
            block_YB(*pending.pop(0))
        blk_counter[0] += len(blocks)

    def do_batch(b):
        base = b * TPB
        ngrp = TPB // MG
        for g in range(ngrp):
            first = g in (0, 1)
            last = g in (0, ngrp - 1)
            do_group(base, g, g * MG, first, last)
        for col in (1, 3):
            P.add("act", lambda e, col=col: e.activation(GT[:, :, col], GT[:, :, col], AF.Exp, scale=-1.0), reads=[rgt], writes=[rgt])
            P.add("act", lambda e, col=col: e.activation(GT[:, :, col], GT[:, :, col], AF.Ln, bias=1.0), reads=[rgt], writes=[rgt])
            P.add("dve", lambda e, col=col: e.tensor_scalar_mul(GT[:, :, col], GT[:, :, col], -1.0), reads=[rgt], writes=[rgt])
        do_dir(base, list(range(NCH)), 0, False)
        do_dir(base, [1, 0] + list(range(NCH - 1, 1, -1)), 1, True)

    for b in range(nbatch):
        do_batch(b)
    P.emit()
    return nc


def build_A(nbatch):
    nc = new_nc()
    build_Arg(nbatch, nc)
    nc.all_engine_barrier()
    build_Am2(nbatch, nc)
    return nc


_PROGS = {}


def _prog(key, builder):
    if key not in _PROGS:
        _PROGS[key] = builder()
    return _PROGS[key]


def _run(nc, in_maps, tag=""):
    r = run_bass_kernel_spmd(nc, in_maps, core_ids=list(range(NCORE)))
    if getattr(r, "exec_time_ns", None):
        print(f"[kernel] launch {tag} exec_time_ns={r.exec_time_ns}", flush=True)
    return r.results


def _vec_pack(vecs):
    return np.ascontiguousarray(np.asarray(vecs, np.float32).reshape(16, KD, 128).transpose(2, 0, 1))


def _cm_perm():
    t = np.arange(SEQ)
    return (t % GRID) * GRID + (t // GRID)


def _core_tokens(b, half, with_ctx):
    lat = CTX + np.arange(half * (SEQ // 2), (half + 1) * (SEQ // 2))
    if with_ctx:
        return np.concatenate([np.arange(half * (CTX // 2), (half + 1) * (CTX // 2)), lat])
    return lat


def kernel(x, c, ctx, c_ctx, w_mod, b_mod, w_in, conv_rg_w, conv_rg_b, conv_m_w, conv_m_b, rg_wa, rg_ba,
           rg_wx, rg_bx, rg_lam, m_gate_b, m_gn_g, p_rg, p_m, w_out, ln_g, ln_b, ff_w1, ff_w3, ff_w2,
           router_w, router_b, ex_w1, ex_w3, ex_w2):
    f32 = lambda a: np.asarray(a, np.float32)
    x, c, ctx, c_ctx = f32(x), f32(c), f32(ctx), f32(c_ctx)
    w_mod, b_mod, w_in = f32(w_mod), f32(b_mod), f32(w_in)
    ncores = NCORE
    cores = [(j // 2, j % 2) for j in range(ncores)]

    cT = np.ascontiguousarray(np.concatenate([c, c_ctx[None]], 0).T)
    maps = []
    for j in range(ncores):
        l, q = divmod(j, 4)
        cs = slice(q * MOD_COLS, (q + 1) * MOD_COLS)
        maps.append({"cT": cT, "wm": np.ascontiguousarray(w_mod[l][:, cs]),
                     "bm": np.ascontiguousarray(np.broadcast_to(b_mod[l][cs], (5, MOD_COLS)))})
    res = _run(_prog("Kmod", build_Kmod), maps, "Kmod")
    mod = [np.concatenate([res[l * 4 + q]["mod"] for q in range(4)], 1).reshape(5, 6, D) for l in range(2)]
    SH1, SC1, G1, SH2, SC2, G2 = range(6)

    def vecs_for(l, b, nxt):
        v = np.zeros((16, D), np.float32)
        for kind, row in ((0, b), (1, 4)):
            o = kind * 6
            v[o + V_G1] = mod[l][row, G1]
            v[o + V_SC2] = mod[l][row, SC2]
            v[o + V_SH2] = mod[l][row, SH2]
            v[o + V_G2] = mod[l][row, G2]
            if nxt is not None:
                v[o + V_NSC1] = mod[nxt][row, SC1]
                v[o + V_NSH1] = mod[nxt][row, SH1]
        v[V_LN + 0], v[V_LN + 1] = ln_g[l, 0], ln_b[l, 0]
        v[V_LN + 2], v[V_LN + 3] = ln_g[l, 1], ln_b[l, 1]
        return _vec_pack(v)

    hfull = [np.concatenate([ctx[b], x[b]], 0) for b in range(NB)]

    groups0 = [(0, 128, 1)] + [(128 + 512 * i, 512, 0) for i in range(4)]
    T0 = 128 + 2048
    hT_core = []
    maps = []
    for (b, half) in cores:
        hT = np.ascontiguousarray(hfull[b][_core_tokens(b, half, True)].T)
        hT_core.append(hT)
        maps.append({"hT": hT, "vec": vecs_for(0, b, 0)})
    res = _run(_prog("Kln", lambda: build_Kln(groups0, T0)), maps, "Kln")
    u_core = [r["uT_out"] for r in res]

    perm = _cm_perm()
    tri_s = np.arange(128)
    U = (tri_s[:, None] <= tri_s[None, :]).astype(np.float32)
    Lo = (tri_s[:, None] >= tri_s[None, :]).astype(np.float32)
    tri = np.ascontiguousarray(np.stack([U, Lo], 1))
    negm = np.ascontiguousarray(((tri - 1.0) * 30000.0).astype(np.float32))
    ident = np.eye(128).astype(NPBF)

    def assemble_u(u_core):
        uT = np.empty((D, NB * TPB), NPBF)
        for j, (b, half) in enumerate(cores):
            uT[:, b * TPB + _core_tokens(b, half, True)] = u_core[j]
        uTm = uT.copy()
        for b in range(NB):
            lat = uT[:, b * TPB + CTX:(b + 1) * TPB]
            uTm[:, b * TPB + CTX:(b + 1) * TPB] = lat[:, perm]
        return uT, uTm

    def mixer(l, uT, uTm):
        wl = w_in[l]
        maps = []
        for j in range(ncores):
            cs = slice(j * 256, (j + 1) * 256)
            rgw = np.zeros((128, 8, 128), np.float32)
            rgb = np.zeros((128, 8), np.float32)
            lm = np.zeros((128, 4), np.float32)
            for d in range(2):
                for ch in range(2):
                    ia = (d * 2 + ch) * 2
                    blk = 2 * j + ch
                    rgw[:, ia, :] = rg_wa[l, d, blk]
                    rgw[:, ia + 1, :] = rg_wx[l, d, blk]
                    rgb[:, ia] = rg_ba[l, d, blk * 128:(blk + 1) * 128]
                    rgb[:, ia + 1] = rg_bx[l, d, blk * 128:(blk + 1) * 128]
                    lm[:, d * 2 + ch] = rg_lam[l, d, blk * 128:(blk + 1) * 128]
            maps.append({"uT": uT, "wx": np.ascontiguousarray(wl[:, cs]), "wg": np.ascontiguousarray(wl[:, C_RGG + j * 256:C_RGG + (j + 1) * 256]),
                         "rcw": np.ascontiguousarray(f32(conv_rg_w[l])[:, cs].reshape(4, 2, 128).transpose(2, 1, 0)),
                         "rcb": np.ascontiguousarray(f32(conv_rg_b[l])[cs].reshape(2, 128).T),
                         "rgw": rgw, "rgb": rgb, "lam": lm})
        for j in range(ncores):
            qs = slice(C_M + j * 256, C_M + (j + 1) * 256)
            ks = slice(C_M + W_M + j * 256, C_M + W_M + (j + 1) * 256)
            vs = slice(C_M + 2 * W_M + j * 256, C_M + 2 * W_M + (j + 1) * 256)
            os_ = slice(C_M + 3 * W_M + j * 256, C_M + 3 * W_M + (j + 1) * 256)
            gcols = [C_M + 4 * W_M + d * 16 + g * 8 + j for d in range(2) for g in range(2)]
            gbv = np.array([m_gate_b[l][d, g, j] for d in range(2) for g in range(2)], np.float32)
            cwm = f32(conv_m_w[l])
            cbm = f32(conv_m_b[l])
            cwj = np.concatenate([cwm[:, j * 256:(j + 1) * 256], cwm[:, W_M + j * 256:W_M + (j + 1) * 256]], 1)
            cbj = np.concatenate([cbm[j * 256:(j + 1) * 256], cbm[W_M + j * 256:W_M + (j + 1) * 256]])
            maps[j].update({"uTm": uTm, "wq": np.ascontiguousarray(wl[:, qs]), "wk": np.ascontiguousarray(wl[:, ks]),
                         "wvo": np.ascontiguousarray(np.concatenate([wl[:, vs], wl[:, os_]], 1)),
                         "wgt": np.ascontiguousarray(wl[:, gcols]),
                         "gb": np.ascontiguousarray(np.broadcast_to(gbv, (128, 4))),
                         "cw": np.ascontiguousarray(cwj.reshape(4, 4, 128).transpose(2, 1, 0)),
                         "cb": np.ascontiguousarray(cbj.reshape(4, 128).T),
                         "gn": np.ascontiguousarray(np.broadcast_to(f32(m_gn_g[l])[j * 256:(j + 1) * 256], (128, 256))),
                         "ident": ident, "tri": tri, "negm": negm})
        res = _run(_prog("A", lambda: build_A(NB)), maps, "A")
        yrT = np.concatenate([r["yrT"] for r in res], 0)
        ymT = np.empty((D, NB * TPB), NPBF)
        for j in range(ncores):
            ymj = res[j]["ym"]
            for b in range(NB):
                blk = ymj[b * TPB:(b + 1) * TPB]
                ymT[j * 256:(j + 1) * 256, b * TPB:b * TPB + CTX] = blk[:CTX].T
                lat = np.empty((SEQ, 256), NPBF)
                lat[perm] = blk[CTX:]
                ymT[j * 256:(j + 1) * 256, b * TPB + CTX:(b + 1) * TPB] = lat.T
        return yrT, ymT

    uT, uTm = assemble_u(u_core)
    yrT, ymT = mixer(0, uT, uTm)
    w_g0 = np.ascontiguousarray(w_in[0][:, C_G:])
    maps = []
    for j, (b, half) in enumerate(cores):
        tk = b * TPB + _core_tokens(b, half, True)
        maps.append({"yrT": np.ascontiguousarray(yrT[:, tk]), "ymT": np.ascontiguousarray(ymT[:, tk]), "uT": u_core[j], "hT": hT_core[j],
                     "w_g": w_g0, "p_rg": f32(p_rg[0]), "p_m": f32(p_m[0]), "w_out": f32(w_out[0]), "vec": vecs_for(0, b, 1),
                     "w1": f32(ff_w1[0]), "w3": f32(ff_w3[0]), "w2": f32(ff_w2[0])})
    res = _run(_prog("B0", lambda: build_B(groups0, T0, True, False)), maps, "B0")
    h1_core = [r["hT_out"] for r in res]
    u1_core = [r["uT_out"] for r in res]

    uT, uTm = assemble_u(u1_core)
    yrT, ymT = mixer(1, uT, uTm)
    groups1 = [(512 * i, 512, 0) for i in range(4)]
    T1 = 2048
    w_g1 = np.ascontiguousarray(w_in[1][:, C_G:])
    rb = np.ascontiguousarray(np.broadcast_to(f32(router_b[0]), (128, 8)))
    maps = []
    for j, (b, half) in enumerate(cores):
        tk = b * TPB + _core_tokens(b, half, False)
        maps.append({"yrT": np.ascontiguousarray(yrT[:, tk]), "ymT": np.ascontiguousarray(ymT[:, tk]),
                     "uT": np.ascontiguousarray(u1_core[j][:, 128:]), "hT": np.ascontiguousarray(h1_core[j][:, 128:]),
                     "w_g": w_g1, "p_rg": f32(p_rg[1]), "p_m": f32(p_m[1]), "w_out": f32(w_out[1]), "vec": vecs_for(1, b, None),
                     "rw": f32(router_w[0]), "rb": rb})
    res = _run(_prog("B1", lambda: build_B(groups1, T1, False, True)), maps, "B1")
    hmid_core = [r["h1T_out"] for r in res]
    vT_all = np.concatenate([r["vT_out"] for r in res], 1)
    gw_all = np.concatenate([r["gw"] for r in res], 0)

    NTOK = gw_all.shape[0]
    top2 = np.argsort(-gw_all, axis=1, kind="stable")[:, :2]
    top2.sort(axis=1)
    lists = [np.nonzero((top2 == e).any(1))[0] for e in range(8)]
    nblk = [-(-len(l) // 512) for l in lists]
    plan = None
    for NS in range(max(1, -(-sum(nblk) // 8)), max(nblk) + 1):
        for K1 in range(NS, -1, -1):
            over = [max(0, n - K1) for n in nblk]
            if K1 == NS:
                ok = sum(over) == 0
                need = 0
            else:
                need = sum(-(-o // (NS - K1)) for o in over)
                ok = need <= 8
            if ok:
                plan = (NS, K1)
                break
        if plan:
            break
    NS, K1 = plan
    print(f"[kernel] moe counts={[len(l) for l in lists]} NS={NS} K1={K1}", flush=True)
    sec = []
    for e in range(8):
        o = max(0, nblk[e] - K1)
        b0 = K1
        while o > 0:
            nb_ = min(o, NS - K1)
            sec.append((e, b0, nb_))
            b0 += nb_
            o -= nb_
    while len(sec) < 8:
        sec.append((len(sec), 0, 0))
    T_C = NS * 512
    pos = np.zeros((NTOK, 8), np.int64)
    for e in range(8):
        pos[lists[e], e] = np.arange(len(lists[e]))
    maps = []
    slot_src = []
    for ci in range(8):
        vT = np.zeros((D, T_C), NPBF)
        wr = np.zeros((T_C,), np.float32)
        srcs = []
        blocks = [(ci, bb) for bb in range(min(nblk[ci], K1))]
        blocks += [None] * (K1 - len(blocks))
        se, sb0, snb = sec[ci]
        blocks += [(se, sb0 + i) for i in range(snb)] + [None] * (NS - K1 - snb)
        for sl, blk in enumerate(blocks):
            if blk is None:
                continue
            e, bb = blk
            idx = lists[e][bb * 512:(bb + 1) * 512]
            vT[:, sl * 512:sl * 512 + len(idx)] = vT_all[:, idx]
            wr[sl * 512:sl * 512 + len(idx)] = gw_all[idx, e]
        slot_src.append(blocks)
        m = {"vT": vT, "wrow": np.ascontiguousarray(np.broadcast_to(wr, (128, T_C))),
             "w1": f32(ex_w1[0][ci]), "w3": f32(ex_w3[0][ci]), "w2": f32(ex_w2[0][ci])}
        if K1 < NS:
            m.update({"w1b": f32(ex_w1[0][se]), "w3b": f32(ex_w3[0][se]), "w2b": f32(ex_w2[0][se])})
        maps.append(m)
    res = _run(_prog(("C", NS, K1), lambda: build_C(NS, K1)), maps, f"C{NS}_{K1}")
    Y = [np.zeros((D, nblk[e] * 512), np.float32) for e in range(8)]
    for ci in range(8):
        for sl, blk in enumerate(slot_src[ci]):
            if blk is None:
                continue
            e, bb = blk
            Y[e][:, bb * 512:(bb + 1) * 512] = res[ci]["yT"][:, sl * 512:(sl + 1) * 512]

    maps = []
    for j, (b, half) in enumerate(cores):
        tl = np.arange(j * T1, (j + 1) * T1)
        ya = np.empty((D, T1), np.float32)
        yb = np.empty((D, T1), np.float32)
        for e in range(8):
            ma = top2[tl, 0] == e
            mb = top2[tl, 1] == e
            ya[:, ma] = Y[e][:, pos[tl[ma], e]]
            yb[:, mb] = Y[e][:, pos[tl[mb], e]]
        maps.append({"yaT": ya, "ybT": yb, "h1T": hmid_core[j], "vec": vecs_for(1, b, None)})
    res = _run(_prog("D", lambda: build_D(T1)), maps, "D")
    out = np.empty((NB, SEQ, D), np.float32)
    for j, (b, half) in enumerate(cores):
        out[b, half * (SEQ // 2):(half + 1) * (SEQ // 2)] = res[j]["oT"].T
    return out
```

```python
import numpy as np
import ml_dtypes
from contextlib import ExitStack
import concourse.bass as bass
import concourse.mybir as mybir
from concourse.bass_utils import run_bass_kernel_spmd

F32 = mybir.dt.float32
BF16 = mybir.dt.bfloat16
AF = mybir.ActivationFunctionType
ALU = mybir.AluOpType
AX = mybir.AxisListType
NPBF = ml_dtypes.bfloat16

D = 2048
KD = 16
DFF = 7168
KF = 56
NCORE = 8
ALPHA = 4.0 ** 0.25
LN_EPS = 1e-6
GRID = 64
CTX = 256
SEQ = 4096
NB = 4
W_RG = 2048
W_M = 2048
C_RGG = W_RG
C_M = 2 * W_RG
C_G = C_M + 4 * W_M + 32
N_IN = C_G + 2 * D


class Res:
    __slots__ = ("last_w", "readers", "excl")

    def __init__(self, excl=False):
        self.last_w = None
        self.readers = []
        self.excl = excl


class Op:
    __slots__ = ("eng", "fn", "deps", "dma", "idx", "signal", "count", "slot", "slot_count")

    def __init__(self, eng, fn, dma):
        self.eng = eng
        self.fn = fn
        self.deps = []
        self.dma = dma
        self.signal = False
        self.count = 0
        self.slot = None
        self.slot_count = 0


class Prog:
    ENGS = ("pe", "act", "dve", "pool", "sp")
    NSLOT = 8

    def __init__(self, nc):
        self.nc = nc
        self.ops = []
        self.es = ExitStack()
        self._n = 0

    def sbuf(self, shape, dtype):
        self._n += 1
        return self.es.enter_context(self.nc.sbuf_tensor(f"sb{self._n}_{id(self) % 9973}", list(shape), dtype))

    def psum(self, shape, dtype):
        self._n += 1
        return self.es.enter_context(self.nc.psum_tensor(f"ps{self._n}_{id(self) % 9973}", list(shape), dtype))

    def add(self, eng, fn, reads=(), writes=(), dma=False):
        op = Op(eng, fn, dma)
        op.idx = len(self.ops)
        deps = {}
        for r in reads:
            if r.last_w is not None:
                deps[r.last_w.idx] = r.last_w
            if r.excl:
                for rd in r.readers:
                    if rd.eng != eng:
                        deps[rd.idx] = rd
        for w in writes:
            if w.last_w is not None:
                deps[w.last_w.idx] = w.last_w
            for rd in w.readers:
                deps[rd.idx] = rd
        last = {}
        for d in deps.values():
            if d.dma:
                op.deps.append(d)
                continue
            if d.eng == "pe" and eng == "pe" and not dma:
                continue
            if d.eng not in last or last[d.eng].idx < d.idx:
                last[d.eng] = d
        op.deps.extend(last.values())
        for r in reads:
            r.readers.append(op)
        for w in writes:
            w.last_w = op
            w.readers = []
        self.ops.append(op)
        return op

    def dma(self, q, out, in_, reads=(), writes=()):
        return self.add(q, lambda e: e.dma_start(out=out, in_=in_), reads, writes, dma=True)

    def emit(self):
        nc = self.nc
        ops = self.ops
        for op in ops:
            for d in op.deps:
                d.signal = True
        counts = {e: 0 for e in self.ENGS}
        dcount = {e: 0 for e in self.ENGS}
        slot_counts = {e: [0] * self.NSLOT for e in self.ENGS}
        for op in ops:
            if op.dma:
                k = dcount[op.eng]
                dcount[op.eng] += 1
                op.slot = k % self.NSLOT
                slot_counts[op.eng][op.slot] += 1
                op.slot_count = slot_counts[op.eng][op.slot]
            elif op.signal:
                counts[op.eng] += 1
                op.count = counts[op.eng]
        tag = id(self) % 9973
        sems = {e: self.es.enter_context(nc.semaphore(f"s_{e}_{tag}")) for e in self.ENGS}
        dsems = {}
        for e in self.ENGS:
            if dcount[e]:
                dsems[e] = [self.es.enter_context(nc.semaphore(f"d_{e}{i}_{tag}"))
                            for i in range(min(self.NSLOT, dcount[e]))]
        by_eng = {e: [op for op in ops if op.eng == e] for e in self.ENGS}
        block = self.es.enter_context(nc.Block())

        def run(e, h):
            seen = {}
            for op in by_eng[e]:
                waits = {}
                for d in op.deps:
                    if d.dma:
                        key = ("d", d.eng, d.slot)
                        val = 16 * d.slot_count
                        sem = dsems[d.eng][d.slot]
                    else:
                        key = ("c", d.eng)
                        val = d.count
                        sem = sems[d.eng]
                    if seen.get(key, 0) >= val:
                        continue
                    if key not in waits or waits[key][1] < val:
                        waits[key] = (sem, val)
                if op.dma and op.slot_count > 1:
                    key = ("d", e, op.slot)
                    val = 16 * (op.slot_count - 1)
                    if seen.get(key, 0) < val and (key not in waits or waits[key][1] < val):
                        waits[key] = (dsems[e][op.slot], val)
                for key, (sem, val) in waits.items():
                    h.wait_ge(sem, val)
                    seen[key] = val
                ins = op.fn(h)
                if op.dma:
                    ins.then_inc(dsems[e][op.slot], 16)
                elif op.signal:
                    ins.then_inc(sems[e], 1)
            if e in dsems:
                for s in range(len(dsems[e])):
                    val = 16 * slot_counts[e][s]
                    if seen.get(("d", e, s), 0) < val:
                        h.wait_ge(dsems[e][s], val)

        @block.tensor
        def _(h):
            run("pe", h)

        @block.scalar
        def _(h):
            run("act", h)

        @block.vector
        def _(h):
            run("dve", h)

        @block.gpsimd
        def _(h):
            run("pool", h)

        @block.sync
        def _(h):
            run("sp", h)

        self.es.close()


class Rot:
    def __init__(self, P, n, shape, dtype, psum=False):
        self.bufs = [(P.psum(shape, dtype) if psum else P.sbuf(shape, dtype)) for _ in range(n)]
        self.res = [Res(excl=psum) for _ in range(n)]
        self.i = 0
        self.n = n

    def next(self):
        j = self.i % self.n
        self.i += 1
        return self.bufs[j], self.res[j]


def new_nc():
    return bass.Bass("TRN2", target_bir_lowering=False)


def din(nc, name, shape, dt):
    return nc.dram_tensor(name, list(shape), dt, kind="ExternalInput").ap()


def dout(nc, name, shape, dt):
    return nc.dram_tensor(name, list(shape), dt, kind="ExternalOutput").ap()


class Ctx:
    def __init__(self, P, wslots=6, wcols=4096, ntmp=6, nmm=6, ln=True):
        self.P = P
        self.mm = Rot(P, nmm, [128, 512], F32, psum=True)
        self.st = Rot(P, 2, [128, 512], F32, psum=True)
        self.tmp = Rot(P, ntmp, [128, 512], F32)
        self.wp = Rot(P, wslots, [128, wcols], BF16) if wslots else None
        self.ones = P.sbuf([128, 128], F32)
        self.r_ones = Res()
        P.add("dve", lambda e: e.memset(self.ones[:], 1.0), writes=[self.r_ones])
        self.stat = Rot(P, 5, [128, 512], F32) if ln else None

    def wtile(self, src, nk, ncols):
        buf, res = self.wp.next()
        view = buf[:, 0:nk * ncols].rearrange("p (k c) -> p k c", k=nk)
        self.P.dma("pool", view, src.rearrange("(k p) c -> p k c", p=128), writes=[res])
        return view, res


def fm_mm(cx, wview, wres, c0, xs, xres, G, nk=None, koff=0, ps=None, first=True, last=True):
    P = cx.P
    if ps is None:
        ps = cx.mm.next()
    pbuf, pres = ps
    nk = nk if nk is not None else KD
    for k in range(nk):
        P.add("pe", lambda e, k=k: e.matmul(pbuf[:, 0:G], wview[:, k, c0:c0 + 128], xs(koff + k),
                                            start=(first and k == 0), stop=(last and k == nk - 1)),
              reads=[wres, xres(koff + k)], writes=[pres])
    return ps


def layer_norm_fm(cx, src, src_res, G, outs):
    P = cx.P
    s1, r1 = cx.st.next()
    s2, r2 = cx.st.next()
    accq, racq = cx.stat.next()
    for c in range(KD):
        P.add("pe", lambda e, c=c: e.matmul(s1[:, 0:G], cx.ones[:], src(c), start=(c == 0), stop=(c == KD - 1)),
              reads=[cx.r_ones, src_res(c)], writes=[r1])
        if c == 0:
            P.add("act", lambda e, c=c: e.activation(accq[:, 0:G], src(c), AF.Square), reads=[src_res(c)], writes=[racq])
        else:
            sq, rsq = cx.tmp.next()
            P.add("act", lambda e, c=c, sq=sq: e.activation(sq[:, 0:G], src(c), AF.Square), reads=[src_res(c)], writes=[rsq])
            P.add("dve", lambda e, sq=sq: e.tensor_tensor(accq[:, 0:G], accq[:, 0:G], sq[:, 0:G], ALU.add), reads=[rsq, racq], writes=[racq])
    P.add("pe", lambda e: e.matmul(s2[:, 0:G], cx.ones[:], accq[:, 0:G], start=True, stop=True), reads=[cx.r_ones, racq], writes=[r2])
    mean, rm = cx.stat.next()
    msq, rq = cx.stat.next()
    var, rv = cx.stat.next()
    rstd, rr = cx.stat.next()
    P.add("act", lambda e: e.mul(mean[:, 0:G], s1[:, 0:G], 1.0 / D), reads=[r1], writes=[rm])
    P.add("dve", lambda e: e.tensor_tensor(msq[:, 0:G], mean[:, 0:G], mean[:, 0:G], ALU.mult), reads=[rm], writes=[rq])
    P.add("dve", lambda e: e.scalar_tensor_tensor(var[:, 0:G], s2[:, 0:G], 1.0 / D, msq[:, 0:G], ALU.mult, ALU.subtract),
          reads=[r2, rq], writes=[rv])
    P.add("act", lambda e: e.activation(var[:, 0:G], var[:, 0:G], AF.Sqrt, bias=LN_EPS, scale=1.0), reads=[rv], writes=[rv])
    P.add("dve", lambda e: e.reciprocal(rstd[:, 0:G], var[:, 0:G]), reads=[rv], writes=[rr])
    for c in range(KD):
        t1, rt1 = cx.tmp.next()
        P.add("dve", lambda e, c=c, t1=t1: e.tensor_tensor(t1[:, 0:G], src(c), mean[:, 0:G], ALU.subtract),
              reads=[src_res(c), rm], writes=[rt1])
        P.add("dve", lambda e, t1=t1: e.tensor_tensor(t1[:, 0:G], t1[:, 0:G], rstd[:, 0:G], ALU.mult),
              reads=[rt1, rr], writes=[rt1])
        for (dst, dres, sc, bi, extra) in outs:
            P.add("act", lambda e, c=c, t1=t1, dst=dst, sc=sc, bi=bi: e.activation(dst(c), t1[:, 0:G], AF.Identity, bias=bi(c), scale=sc(c)),
                  reads=[rt1] + list(extra), writes=[dres(c)])


def ffn_fm(cx, w1, w3, w2, vview, vres, G, act_view, act_res, consume):
    P = cx.P
    for f2 in range(KF // 2):
        t1, rw1 = cx.wtile(w1[:, f2 * 256:(f2 + 1) * 256], KD, 256)
        t3, rw3 = cx.wtile(w3[:, f2 * 256:(f2 + 1) * 256], KD, 256)
        for cc in range(2):
            f = 2 * f2 + cc
            p1, rp1 = fm_mm(cx, t1, rw1, cc * 128, vview, vres, G)
            p3, rp3 = fm_mm(cx, t3, rw3, cc * 128, vview, vres, G)
            s, rs = cx.tmp.next()
            P.add("act", lambda e, p1=p1, s=s: e.activation(s[:, 0:G], p1[:, 0:G], AF.Silu), reads=[rp1], writes=[rs])
            P.add("dve", lambda e, f=f, p3=p3, s=s: e.tensor_tensor(act_view(f), s[:, 0:G], p3[:, 0:G], ALU.mult),
                  reads=[rs, rp3], writes=[act_res(f)])
    for c in range(KD):
        ta, ra = cx.wtile(w2[0:(KF // 2) * 128, c * 128:(c + 1) * 128], KF // 2, 128)
        tb, rb = cx.wtile(w2[(KF // 2) * 128:KF * 128, c * 128:(c + 1) * 128], KF // 2, 128)
        ps = cx.mm.next()
        fm_mm(cx, ta, ra, 0, act_view, act_res, G, nk=KF // 2, koff=0, ps=ps, first=True, last=False)
        fm_mm(cx, tb, rb, 0, act_view, act_res, G, nk=KF // 2, koff=KF // 2, ps=ps, first=False, last=True)
        consume(c, ps[0], ps[1])


V_G1, V_SC2, V_SH2, V_G2, V_NSC1, V_NSH1 = range(6)
V_LN = 12


def build_B(groups, T, ffn, router):
    nc = new_nc()
    yrT = din(nc, "yrT", [D, T], BF16)
    ymT = din(nc, "ymT", [D, T], BF16)
    uT = din(nc, "uT", [D, T], BF16)
    hT = din(nc, "hT", [D, T], F32)
    w_g = din(nc, "w_g", [D, 2 * D], F32)
    p_rg = din(nc, "p_rg", [W_RG, D], F32)
    p_m = din(nc, "p_m", [W_M, D], F32)
    w_out = din(nc, "w_out", [D, D], F32)
    vec = din(nc, "vec", [128, 16, KD], F32)
    if ffn:
        w1 = din(nc, "w1", [D, DFF], F32)
        w3 = din(nc, "w3", [D, DFF], F32)
        w2 = din(nc, "w2", [DFF, D], F32)
        hT_out = dout(nc, "hT_out", [D, T], F32)
        uT_out = dout(nc, "uT_out", [D, T], BF16)
    if router:
        rw = din(nc, "rw", [D, 8], F32)
        rb = din(nc, "rb", [128, 8], F32)
        h1T_out = dout(nc, "h1T_out", [D, T], F32)
        vT_out = dout(nc, "vT_out", [D, T], BF16)
        gw_out = dout(nc, "gw", [T, 8], F32)

    P = Prog(nc)
    cx = Ctx(P, wslots=5, ntmp=5) if router else Ctx(P, wslots=8, ntmp=8)
    vs = P.sbuf([128, 16, KD], F32)
    r_vs = Res()
    P.dma("sp", vs[:], vec, writes=[r_vs])
    for kind in range(2):
        for slot in (V_SC2, V_NSC1):
            j = kind * 6 + slot
            P.add("dve", lambda e, j=j: e.tensor_scalar_add(vs[:, j, :], vs[:, j, :], 1.0), reads=[r_vs], writes=[r_vs])

    def vcol(kind, slot):
        j = (kind * 6 + slot) if slot < 6 else slot
        return lambda c: vs[:, j, c:c + 1]

    scr = P.sbuf([128, 64 * 512], BF16)
    rj = [Res() for _ in range(64)]
    hbuf = P.sbuf([128, KD, 512], F32)
    rh = [Res() for _ in range(KD)]
    vbuf = P.sbuf([128, KD, 512], BF16)
    rv = [Res() for _ in range(KD)]
    if router:
        v32 = P.sbuf([128, KD, 512], F32)
        rv32 = [Res() for _ in range(KD)]
        rws = P.sbuf([128, KD, 8], F32)
        r_rws = Res()
        P.dma("sp", rws[:], rw.rearrange("(k p) e -> p k e", p=128), writes=[r_rws])
        rbs = P.sbuf([128, 8], F32)
        r_rbs = Res()
        P.dma("sp", rbs[:], rb, writes=[r_rbs])
        small = Rot(P, 12, [128, 8], F32)

    def do_group(tok0, G, kind):
        def q(qi, k, G=G):
            return scr[:, (qi * 16 + k) * 512:(qi * 16 + k) * 512 + G]

        def qall(qi, G=G):
            return scr[:, qi * 16 * 512:(qi + 1) * 16 * 512].rearrange("p (k t) -> p k t", k=16)[:, :, 0:G]

        for qi, src in enumerate((yrT, ymT, uT)):
            P.dma("sp", qall(qi), src[:, tok0:tok0 + G].rearrange("(k p) t -> p k t", p=128),
                  writes=rj[qi * 16:(qi + 1) * 16])
        P.dma("sp", hbuf[:, :, 0:G], hT[:, tok0:tok0 + G].rearrange("(k p) t -> p k t", p=128), writes=rh)
        for c in range(KD):
            P.add("act", lambda e, c=c, G=G: e.mul(hbuf[:, c, 0:G], hbuf[:, c, 0:G], ALPHA), reads=[rh[c]], writes=[rh[c]])
        for c2 in range(KD // 2):
            cs = slice(c2 * 256, (c2 + 1) * 256)
            sa = [cx.tmp.next() for _ in range(2)]
            sb_ = [cx.tmp.next() for _ in range(2)]
            tg1, rg1 = cx.wtile(w_g[:, cs], KD, 256)
            for cc in range(2):
                pg, rpg = fm_mm(cx, tg1, rg1, cc * 128, lambda k: q(2, k), lambda k: rj[32 + k], G)
                P.add("act", lambda e, pg=pg, t=sa[cc][0]: e.activation(t[:, 0:G], pg[:, 0:G], AF.Sigmoid), reads=[rpg], writes=[sa[cc][1]])
            tg2, rg2 = cx.wtile(w_g[:, D + c2 * 256:D + (c2 + 1) * 256], KD, 256)
            for cc in range(2):
                pg, rpg = fm_mm(cx, tg2, rg2, cc * 128, lambda k: q(2, k), lambda k: rj[32 + k], G)
                P.add("act", lambda e, pg=pg, t=sb_[cc][0]: e.activation(t[:, 0:G], pg[:, 0:G], AF.Sigmoid), reads=[rpg], writes=[sb_[cc][1]])
            tp1, rp1 = cx.wtile(p_rg[:, cs], KD, 256)
            for cc in range(2):
                pa, rpa = fm_mm(cx, tp1, rp1, cc * 128, lambda k: q(0, k), lambda k: rj[k], G)
                P.add("dve", lambda e, pa=pa, t=sa[cc][0]: e.tensor_tensor(t[:, 0:G], t[:, 0:G], pa[:, 0:G], ALU.mult), reads=[sa[cc][1], rpa], writes=[sa[cc][1]])
            tp2, rp2 = cx.wtile(p_m[:, cs], KD, 256)
            for cc in range(2):
                c = 2 * c2 + cc
                pa, rpa = fm_mm(cx, tp2, rp2, cc * 128, lambda k: q(1, k), lambda k: rj[16 + k], G)
                P.add("dve", lambda e, pa=pa, t=sb_[cc][0]: e.tensor_tensor(t[:, 0:G], t[:, 0:G], pa[:, 0:G], ALU.mult), reads=[sb_[cc][1], rpa], writes=[sb_[cc][1]])
                P.add("dve", lambda e, c=c, t1=sa[cc][0], t2=sb_[cc][0]: e.tensor_tensor(q(3, c, G), t1[:, 0:G], t2[:, 0:G], ALU.add),
                      reads=[sa[cc][1], sb_[cc][1]], writes=[rj[48 + c]])
        g1 = vcol(kind, V_G1)
        for c2 in range(KD // 2):
            to, ro = cx.wtile(w_out[:, c2 * 256:(c2 + 1) * 256], KD, 256)
            for cc in range(2):
                c = 2 * c2 + cc
                po, rpo = fm_mm(cx, to, ro, cc * 128, lambda k: q(3, k), lambda k: rj[48 + k], G)
                P.add("dve", lambda e, c=c, po=po, G=G, g1=g1: e.scalar_tensor_tensor(hbuf[:, c, 0:G], po[:, 0:G], g1(c), hbuf[:, c, 0:G], ALU.mult, ALU.add),
                      reads=[rpo, rh[c], r_vs], writes=[rh[c]])
        hsrc = lambda c, G=G: hbuf[:, c, 0:G]
        layer_norm_fm(cx, hsrc, lambda c: rh[c], G,
                      [(hsrc, lambda c: rh[c], vcol(0, V_LN + 0), vcol(0, V_LN + 1), [r_vs])])
        outs = [(lambda c, G=G: vbuf[:, c, 0:G], lambda c: rv[c], vcol(kind, V_SC2), vcol(kind, V_SH2), [r_vs])]
        if router:
            outs.append((lambda c, G=G: v32[:, c, 0:G], lambda c: rv32[c], vcol(kind, V_SC2), vcol(kind, V_SH2), [r_vs]))
        layer_norm_fm(cx, hsrc, lambda c: rh[c], G, outs)
        if ffn:
            g2 = vcol(kind, V_G2)
            for c in range(KD):
                P.add("act", lambda e, c=c, G=G: e.mul(hbuf[:, c, 0:G], hbuf[:, c, 0:G], ALPHA), reads=[rh[c]], writes=[rh[c]])

            def consume(c, pbuf, pres, G=G, g2=g2):
                P.add("dve", lambda e: e.scalar_tensor_tensor(hbuf[:, c, 0:G], pbuf[:, 0:G], g2(c), hbuf[:, c, 0:G], ALU.mult, ALU.add),
                      reads=[pres, rh[c], r_vs], writes=[rh[c]])

            ffn_fm(cx, w1, w3, w2, lambda k, G=G: vbuf[:, k, 0:G], lambda k: rv[k], G,
                   lambda f, G=G: scr[:, f * 512:f * 512 + G], lambda f: rj[f], consume)
            layer_norm_fm(cx, hsrc, lambda c: rh[c], G,
                          [(hsrc, lambda c: rh[c], vcol(0, V_LN + 2), vcol(0, V_LN + 3), [r_vs])])
            P.dma("sp", hT_out[:, tok0:tok0 + G].rearrange("(k p) t -> p k t", p=128), hbuf[:, :, 0:G], reads=rh)
            layer_norm_fm(cx, hsrc, lambda c: rh[c], G,
                          [(lambda c, G=G: vbuf[:, c, 0:G], lambda c: rv[c], vcol(kind, V_NSC1), vcol(kind, V_NSH1), [r_vs])])
            P.dma("sp", uT_out[:, tok0:tok0 + G].rearrange("(k p) t -> p k t", p=128), vbuf[:, :, 0:G], reads=rv)
        if router:
            P.dma("sp", h1T_out[:, tok0:tok0 + G].rearrange("(k p) t -> p k t", p=128), hbuf[:, :, 0:G], reads=rh)
            P.dma("sp", vT_out[:, tok0:tok0 + G].rearrange("(k p) t -> p k t", p=128), vbuf[:, :, 0:G], reads=rv)
            for t0 in range(0, G, 128):
                pl, rpl = cx.mm.next()
                for k in range(KD):
                    P.add("pe", lambda e, k=k, t0=t0, pl=pl: e.matmul(pl[:, 0:8], v32[:, k, t0:t0 + 128], rws[:, k, :], start=(k == 0), stop=(k == KD - 1)),
                          reads=[rv32[k], r_rws], writes=[rpl])
                lg, rlg = small.next()
                m1, rm1 = small.next()
                e1, re1 = small.next()
                l2, rl2 = small.next()
                m2, rm2 = small.next()
                e2, re2 = small.next()
                dd, rdd = small.next()
                gwt, rgw = small.next()
                P.add("dve", lambda e, pl=pl, lg=lg: e.tensor_tensor(lg[:], pl[:, 0:8], rbs[:], ALU.add), reads=[rpl, r_rbs], writes=[rlg])
                P.add("dve", lambda e, lg=lg, m1=m1: e.reduce_max(m1[:, 0:1], lg[:], AX.X), reads=[rlg], writes=[rm1])
                P.add("dve", lambda e, lg=lg, m1=m1, e1=e1: e.tensor_scalar(e1[:], lg[:], m1[:, 0:1], None, ALU.is_equal), reads=[rlg, rm1], writes=[re1])
                P.add("dve", lambda e, lg=lg, e1=e1, l2=l2: e.scalar_tensor_tensor(l2[:], e1[:], -1e30, lg[:], ALU.mult, ALU.add), reads=[rlg, re1], writes=[rl2])
                P.add("dve", lambda e, l2=l2, m2=m2: e.reduce_max(m2[:, 0:1], l2[:], AX.X), reads=[rl2], writes=[rm2])
                P.add("dve", lambda e, l2=l2, m2=m2, e2=e2: e.tensor_scalar(e2[:], l2[:], m2[:, 0:1], None, ALU.is_equal), reads=[rl2, rm2], writes=[re2])
                P.add("dve", lambda e, m1=m1, m2=m2, dd=dd: e.tensor_tensor(dd[:, 0:1], m2[:, 0:1], m1[:, 0:1], ALU.subtract), reads=[rm1, rm2], writes=[rdd])
                P.add("act", lambda e, dd=dd: e.activation(dd[:, 1:2], dd[:, 0:1], AF.Exp), reads=[rdd], writes=[rdd])
                P.add("dve", lambda e, dd=dd: e.tensor_scalar_add(dd[:, 2:3], dd[:, 1:2], 1.0), reads=[rdd], writes=[rdd])
                P.add("dve", lambda e, dd=dd: e.reciprocal(dd[:, 2:3], dd[:, 2:3]), reads=[rdd], writes=[rdd])
                P.add("dve", lambda e, dd=dd: e.tensor_tensor(dd[:, 3:4], dd[:, 1:2], dd[:, 2:3], ALU.mult), reads=[rdd], writes=[rdd])
                P.add("dve", lambda e, e1=e1, dd=dd, gwt=gwt: e.tensor_scalar(gwt[:], e1[:], dd[:, 2:3], None, ALU.mult), reads=[re1, rdd], writes=[rgw])
                P.add("dve", lambda e, e2=e2, dd=dd, gwt=gwt: e.scalar_tensor_tensor(gwt[:], e2[:], dd[:, 3:4], gwt[:], ALU.mult, ALU.add), reads=[re2, rdd, rgw], writes=[rgw])
                P.dma("sp", gw_out[tok0 + t0:tok0 + t0 + 128, :], gwt[:], reads=[rgw])
    for (tok0, G, kind) in groups:
        do_group(tok0, G, kind)
    P.emit()
    return nc


def build_C(NS, K1):
    T = NS * 512
    nc = new_nc()
    vT = din(nc, "vT", [D, T], BF16)
    wrow = din(nc, "wrow", [128, T], F32)
    wts = [(din(nc, "w1", [D, DFF], F32), din(nc, "w3", [D, DFF], F32), din(nc, "w2", [DFF, D], F32))]
    if K1 < NS:
        wts.append((din(nc, "w1b", [D, DFF], F32), din(nc, "w3b", [D, DFF], F32), din(nc, "w2b", [DFF, D], F32)))
    yT = dout(nc, "yT", [D, T], F32)
    P = Prog(nc)
    cx = Ctx(P, wslots=8)
    vb = Rot(P, 2, [128, KD, 512], BF16)
    wr = Rot(P, 2, [128, 512], F32)
    scr = P.sbuf([128, KF * 512], BF16)
    rj = [Res() for _ in range(KF)]

    def do_group(tok0, G, w1, w3, w2):
        vbuf, rvb = vb.next()
        wrt, rwr = wr.next()
        P.dma("sp", vbuf[:, :, 0:G], vT[:, tok0:tok0 + G].rearrange("(k p) t -> p k t", p=128), writes=[rvb])
        P.dma("sp", wrt[:, 0:G], wrow[:, tok0:tok0 + G], writes=[rwr])

        def consume(c, pbuf, pres):
            o, ro = cx.tmp.next()
            P.add("dve", lambda e: e.tensor_tensor(o[:, 0:G], pbuf[:, 0:G], wrt[:, 0:G], ALU.mult), reads=[pres, rwr], writes=[ro])
            P.dma("sp", yT[c * 128:(c + 1) * 128, tok0:tok0 + G], o[:, 0:G], reads=[ro])

        ffn_fm(cx, w1, w3, w2, lambda k: vbuf[:, k, 0:G], lambda k: rvb, G,
               lambda f: scr[:, f * 512:f * 512 + G], lambda f: rj[f], consume)

    for sl in range(NS):
        w1, w3, w2 = wts[0] if sl < K1 else wts[1]
        do_group(sl * 512, 512, w1, w3, w2)
    P.emit()
    return nc


def build_D(T):
    nc = new_nc()
    yaT = din(nc, "yaT", [D, T], F32)
    ybT = din(nc, "ybT", [D, T], F32)
    h1T = din(nc, "h1T", [D, T], F32)
    vec = din(nc, "vec", [128, 16, KD], F32)
    oT = dout(nc, "oT", [D, T], F32)
    P = Prog(nc)
    cx = Ctx(P, wslots=0)
    vs = P.sbuf([128, 16, KD], F32)
    r_vs = Res()
    P.dma("sp", vs[:], vec, writes=[r_vs])
    ya = P.sbuf([128, KD, 512], F32)
    yb = P.sbuf([128, KD, 512], F32)
    hb = P.sbuf([128, KD, 512], F32)
    rya = [Res() for _ in range(KD)]
    ryb = [Res() for _ in range(KD)]
    rh = [Res() for _ in range(KD)]

    def do_group(tok0, G):
        for buf, res, src in ((ya, rya, yaT), (yb, ryb, ybT), (hb, rh, h1T)):
            P.dma("sp", buf[:, :, 0:G], src[:, tok0:tok0 + G].rearrange("(k p) t -> p k t", p=128), writes=res)
        for c in range(KD):
            P.add("act", lambda e, c=c: e.mul(hb[:, c, 0:G], hb[:, c, 0:G], ALPHA), reads=[rh[c]], writes=[rh[c]])
            P.add("dve", lambda e, c=c: e.tensor_tensor(ya[:, c, 0:G], ya[:, c, 0:G], yb[:, c, 0:G], ALU.add), reads=[rya[c], ryb[c]], writes=[rya[c]])
            P.add("dve", lambda e, c=c: e.scalar_tensor_tensor(hb[:, c, 0:G], ya[:, c, 0:G], vs[:, V_G2, c:c + 1], hb[:, c, 0:G], ALU.mult, ALU.add),
                  reads=[rya[c], rh[c], r_vs], writes=[rh[c]])
        hsrc = lambda c: hb[:, c, 0:G]
        layer_norm_fm(cx, hsrc, lambda c: rh[c], G,
                      [(hsrc, lambda c: rh[c], lambda c: vs[:, V_LN + 2, c:c + 1], lambda c: vs[:, V_LN + 3, c:c + 1], [r_vs])])
        P.dma("sp", oT[:, tok0:tok0 + G].rearrange("(k p) t -> p k t", p=128), hb[:, :, 0:G], reads=rh)

    for tok0 in range(0, T, 512):
        do_group(tok0, min(512, T - tok0))
    P.emit()
    return nc


def build_Kln(groups, T):
    nc = new_nc()
    hT = din(nc, "hT", [D, T], F32)
    vec = din(nc, "vec", [128, 16, KD], F32)
    uT_out = dout(nc, "uT_out", [D, T], BF16)
    P = Prog(nc)
    cx = Ctx(P, wslots=0)
    vs = P.sbuf([128, 16, KD], F32)
    r_vs = Res()
    P.dma("sp", vs[:], vec, writes=[r_vs])
    for kind in range(2):
        j = kind * 6 + V_NSC1
        P.add("dve", lambda e, j=j: e.tensor_scalar_add(vs[:, j, :], vs[:, j, :], 1.0), reads=[r_vs], writes=[r_vs])
    hb = Rot(P, 2, [128, KD, 512], F32)
    ub = Rot(P, 2, [128, KD, 512], BF16)

    def do_group(tok0, G, kind):
        hbuf, rhb = hb.next()
        ubuf, rub = ub.next()
        P.dma("sp", hbuf[:, :, 0:G], hT[:, tok0:tok0 + G].rearrange("(k p) t -> p k t", p=128), writes=[rhb])
        layer_norm_fm(cx, lambda c: hbuf[:, c, 0:G], lambda c: rhb, G,
                      [(lambda c: ubuf[:, c, 0:G], lambda c: rub, lambda c: vs[:, kind * 6 + V_NSC1, c:c + 1],
                        lambda c: vs[:, kind * 6 + V_NSH1, c:c + 1], [r_vs])])
        P.dma("sp", uT_out[:, tok0:tok0 + G].rearrange("(k p) t -> p k t", p=128), ubuf[:, :, 0:G], reads=[rub])

    for (tok0, G, kind) in groups:
        do_group(tok0, G, kind)
    P.emit()
    return nc


MOD_COLS = 2 * 6 * D // NCORE


def build_Kmod():
    nc = new_nc()
    cT = din(nc, "cT", [D, 5], F32)
    wm = din(nc, "wm", [D, MOD_COLS], F32)
    bm = din(nc, "bm", [5, MOD_COLS], F32)
    out = dout(nc, "mod", [5, MOD_COLS], F32)
    P = Prog(nc)
    ps = Rot(P, 2, [128, 512], F32, psum=True)
    wt = Rot(P, 2, [128, KD, 512], F32)
    cs = P.sbuf([128, KD, 5], F32)
    r_cs = Res()
    P.dma("sp", cs[:], cT.rearrange("(k p) r -> p k r", p=128), writes=[r_cs])
    P.add("act", lambda e: e.activation(cs[:], cs[:], AF.Silu), reads=[r_cs], writes=[r_cs])
    bs = P.sbuf([5, MOD_COLS], F32)
    r_bs = Res()
    P.dma("sp", bs[:], bm, writes=[r_bs])
    ob = P.sbuf([5, MOD_COLS], F32)
    r_ob = Res()
    for n in range(MOD_COLS // 512):
        w, rw_ = wt.next()
        P.dma("sp" if n % 2 == 0 else "act", w[:], wm[:, n * 512:(n + 1) * 512].rearrange("(k p) c -> p k c", p=128), writes=[rw_])
        p, rp = ps.next()
        for k in range(KD):
            P.add("pe", lambda e, k=k, p=p, w=w: e.matmul(p[0:5, :], cs[:, k, :], w[:, k, :], start=(k == 0), stop=(k == KD - 1)),
                  reads=[r_cs, rw_], writes=[rp])
        P.add("dve", lambda e, n=n, p=p: e.tensor_tensor(ob[:, n * 512:(n + 1) * 512], p[0:5, :], bs[:, n * 512:(n + 1) * 512], ALU.add),
              reads=[rp, r_bs], writes=[r_ob])
    P.dma("sp", out, ob[:], reads=[r_ob])
    P.emit()
    return nc


TPB = CTX + SEQ
GELU_C = 0.044715
GELU_S = 1.5957691216057308


def batch_groups():
    return [(0, CTX)] + [(CTX + 512 * i, 512) for i in range(SEQ // 512)]


def build_Arg(nbatch, nc=None):
    NT = nbatch * TPB
    nc = nc or new_nc()
    uT = din(nc, "uT", [D, NT], BF16)
    wx = din(nc, "wx", [D, 256], F32)
    wg = din(nc, "wg", [D, 256], F32)
    cw = din(nc, "rcw", [128, 2, 4], F32)
    cb = din(nc, "rcb", [128, 2], F32)
    rgw = din(nc, "rgw", [128, 8, 128], F32)
    rgb = din(nc, "rgb", [128, 8], F32)
    lam = din(nc, "lam", [128, 4], F32)
    yrT = dout(nc, "yrT", [256, NT], BF16)
    P = Prog(nc)
    cx = Ctx(P, wslots=2, wcols=KD * 256, ntmp=10, ln=False)
    twx, rwx = cx.wtile(wx, KD, 256)
    twg, rwg = cx.wtile(wg, KD, 256)
    small = {}
    r_small = Res()
    for name, src, shape in (("cw", cw, [128, 2, 4]), ("cb", cb, [128, 2]), ("rgw", rgw, [128, 8, 128]), ("rgb", rgb, [128, 8]), ("lam", lam, [128, 4])):
        small[name] = P.sbuf(shape, F32)
        P.dma("sp", small[name][:], src, writes=[r_small])
    clam = P.sbuf([128, 4], F32)
    clam2 = P.sbuf([128, 4], F32)
    r_cl = Res()
    P.add("act", lambda e: e.activation(clam[:], small["lam"][:], AF.Exp, scale=-1.0), reads=[r_small], writes=[r_cl])
    P.add("act", lambda e: e.activation(clam[:], clam[:], AF.Ln, bias=1.0), reads=[r_cl], writes=[r_cl])
    P.add("dve", lambda e: e.tensor_scalar_mul(clam2[:], clam[:], -16.0), reads=[r_cl], writes=[r_cl])
    P.add("dve", lambda e: e.tensor_scalar_mul(clam[:], clam[:], -8.0), reads=[r_cl], writes=[r_cl])

    ub = Rot(P, 2, [128, KD, 512], BF16)
    XR = [P.sbuf([128, TPB + 6], F32) for _ in range(2)]
    rxr = [Res(), Res()]
    XC = [P.sbuf([128, TPB], F32) for _ in range(2)]
    rxc = [Res(), Res()]
    GB = [P.sbuf([128, TPB], BF16) for _ in range(2)]
    rgbuf = [Res(), Res()]
    H = [P.sbuf([128, TPB + 1], F32) for _ in range(2)]
    rH = [Res(), Res()]
    for ch in range(2):
        P.add("dve", lambda e, ch=ch: e.memset(XR[ch][:], 0.0), writes=[rxr[ch]])
    A, B = XR[0], XR[1]
    segs = ((0, 0, CTX), (CTX + 3, CTX, SEQ))

    def posraw(tok):
        return tok + 2 if tok < CTX else tok + 5

    def do_batch(b):
        base = b * TPB
        if b > 0:
            for ch in range(2):
                for col in (0, CTX + 2, TPB + 5):
                    w = 2 if col != TPB + 5 else 1
                    if col == CTX + 2:
                        w = 3
                    P.add("dve", lambda e, ch=ch, col=col, w=w: e.memset(XR[ch][:, col:col + w], 0.0), writes=[rxr[ch]])
        for (tok0, G) in batch_groups():
            ubuf, rub = ub.next()
            P.dma("sp", ubuf[:, :, 0:G], uT[:, base + tok0:base + tok0 + G].rearrange("(k p) t -> p k t", p=128), writes=[rub])
            for ch in range(2):
                px, rpx = fm_mm(cx, twx, rwx, ch * 128, lambda k, ubuf=ubuf, G=G: ubuf[:, k, 0:G], lambda k, rub=rub: rub, G)
                P.add("act", lambda e, ch=ch, px=px, tok0=tok0, G=G: e.copy(XR[ch][:, posraw(tok0):posraw(tok0) + G], px[:, 0:G]),
                      reads=[rpx], writes=[rxr[ch]])
                pg, rpg = fm_mm(cx, twg, rwg, ch * 128, lambda k, ubuf=ubuf, G=G: ubuf[:, k, 0:G], lambda k, rub=rub: rub, G)
                t, rt = cx.tmp.next()
                P.add("act", lambda e, pg=pg, t=t, G=G: e.activation(t[:, 0:G], pg[:, 0:G], AF.Square), reads=[rpg], writes=[rt])
                P.add("dve", lambda e, t=t, G=G: e.tensor_scalar(t[:, 0:G], t[:, 0:G], GELU_C, 1.0, ALU.mult, ALU.add), reads=[rt], writes=[rt])
                P.add("dve", lambda e, pg=pg, t=t, G=G: e.tensor_tensor(t[:, 0:G], t[:, 0:G], pg[:, 0:G], ALU.mult), reads=[rt, rpg], writes=[rt])
                P.add("act", lambda e, t=t, G=G: e.activation(t[:, 0:G], t[:, 0:G], AF.Sigmoid, scale=GELU_S), reads=[rt], writes=[rt])
                P.add("dve", lambda e, ch=ch, pg=pg, t=t, tok0=tok0, G=G: e.tensor_tensor(GB[ch][:, tok0:tok0 + G], t[:, 0:G], pg[:, 0:G], ALU.mult),
                      reads=[rt, rpg], writes=[rgbuf[ch]])
        for ch in range(2):
            for (praw, ptok, L) in segs:
                for h0 in range(0, L, 2048):
                    hl = min(2048, L - h0)
                    o = XC[ch][:, ptok + h0:ptok + h0 + hl]
                    P.add("dve", lambda e, ch=ch, o=o, praw=praw, h0=h0, hl=hl: e.tensor_scalar(
                        o, XR[ch][:, praw + h0:praw + h0 + hl], small["cw"][:, ch, 0:1], small["cb"][:, ch:ch + 1], ALU.mult, ALU.add),
                        reads=[rxr[ch], r_small], writes=[rxc[ch]])
                    for j in range(1, 4):
                        P.add("dve", lambda e, ch=ch, o=o, praw=praw, h0=h0, hl=hl, j=j: e.scalar_tensor_tensor(
                            o, XR[ch][:, praw + h0 + j:praw + h0 + j + hl], small["cw"][:, ch, j:j + 1], o, ALU.mult, ALU.add),
                            reads=[rxr[ch], r_small, rxc[ch]], writes=[rxc[ch]])
        for ch in range(2):
            for d in range(2):
                ia = (d * 2 + ch) * 2
                grps = batch_groups()
                for g0 in range(0, len(grps), 3):
                    items = []
                    for (tok0, G) in grps[g0:g0 + 3]:
                        sb_, sl = (0, CTX) if tok0 < CTX else (CTX, SEQ)
                        if d == 0:
                            oa = A[:, 1 + tok0:1 + tok0 + G]
                            obb = B[:, 1 + tok0:1 + tok0 + G]
                        else:
                            phi = sb_ + sl - 1 - (tok0 - sb_)
                            oa = A[:, 1 + phi:1 + phi - G:-1]
                            obb = B[:, 1 + phi:1 + phi - G:-1]
                        xcs = XC[ch][:, tok0:tok0 + G]
                        pr, rpr = cx.mm.next()
                        P.add("pe", lambda e, pr=pr, xcs=xcs, G=G, ia=ia: e.matmul(pr[:, 0:G], small["rgw"][:, ia, :], xcs, start=True, stop=True),
                              reads=[r_small, rxc[ch]], writes=[rpr])
                        pi, rpi = cx.mm.next()
                        P.add("pe", lambda e, pi=pi, xcs=xcs, G=G, ia=ia: e.matmul(pi[:, 0:G], small["rgw"][:, ia + 1, :], xcs, start=True, stop=True),
                              reads=[r_small, rxc[ch]], writes=[rpi])
                        r_, rr = cx.tmp.next()
                        s_, rs = cx.tmp.next()
                        i_, ri = cx.tmp.next()
                        items.append((G, oa, obb, xcs, pr, rpr, pi, rpi, r_, rr, s_, rs, i_, ri))
                    for (G, oa, obb, xcs, pr, rpr, pi, rpi, r_, rr, s_, rs, i_, ri) in items:
                        P.add("act", lambda e, pr=pr, r_=r_, G=G, ia=ia: e.activation(r_[:, 0:G], pr[:, 0:G], AF.Sigmoid, bias=small["rgb"][:, ia:ia + 1]),
                              reads=[rpr, r_small], writes=[rr])
                        P.add("act", lambda e, pi=pi, i_=i_, G=G, ia=ia: e.activation(i_[:, 0:G], pi[:, 0:G], AF.Sigmoid, bias=small["rgb"][:, ia + 1:ia + 2]),
                              reads=[rpi, r_small], writes=[ri])
                    for (G, oa, obb, xcs, pr, rpr, pi, rpi, r_, rr, s_, rs, i_, ri) in items:
                        P.add("act", lambda e, r_=r_, oa=oa, G=G, d=d, ch=ch: e.activation(oa, r_[:, 0:G], AF.Exp, scale=clam[:, d * 2 + ch:d * 2 + ch + 1]),
                              reads=[rr, r_cl], writes=[rxr[0]])
                        P.add("act", lambda e, r_=r_, s_=s_, G=G, d=d, ch=ch: e.activation(s_[:, 0:G], r_[:, 0:G], AF.Exp, scale=clam2[:, d * 2 + ch:d * 2 + ch + 1]),
                              reads=[rr, r_cl], writes=[rs])
                    for (G, oa, obb, xcs, pr, rpr, pi, rpi, r_, rr, s_, rs, i_, ri) in items:
                        P.add("act", lambda e, s_=s_, G=G: e.activation(s_[:, 0:G], s_[:, 0:G], AF.Ln, bias=1.0, scale=-1.0), reads=[rs], writes=[rs])
                    for (G, oa, obb, xcs, pr, rpr, pi, rpi, r_, rr, s_, rs, i_, ri) in items:
                        P.add("act", lambda e, s_=s_, G=G: e.activation(s_[:, 0:G], s_[:, 0:G], AF.Exp, scale=0.5), reads=[rs], writes=[rs])
                    for (G, oa, obb, xcs, pr, rpr, pi, rpi, r_, rr, s_, rs, i_, ri) in items:
                        P.add("dve", lambda e, s_=s_, i_=i_, G=G: e.tensor_tensor(s_[:, 0:G], s_[:, 0:G], i_[:, 0:G], ALU.mult), reads=[rs, ri], writes=[rs])
                        P.add("dve", lambda e, s_=s_, obb=obb, xcs=xcs, G=G: e.tensor_tensor(obb, s_[:, 0:G], xcs, ALU.mult), reads=[rs, rxc[ch]], writes=[rxr[1]])
                half = TPB // 2
                P.add("dve", lambda e, d=d: e.tensor_tensor_scan(H[d][:, 1:1 + half], A[:, 1:1 + half], B[:, 1:1 + half], 0.0, ALU.mult, ALU.add),
                      reads=[rxr[0], rxr[1]], writes=[rH[d]])
                P.add("dve", lambda e, d=d: e.tensor_tensor_scan(H[d][:, 1 + half:1 + TPB], A[:, 1 + half:1 + TPB], B[:, 1 + half:1 + TPB],
                                                                 H[d][:, half:half + 1], ALU.mult, ALU.add),
                      reads=[rxr[0], rxr[1], rH[d]], writes=[rH[d]])
            for (praw, ptok, L) in segs:
                P.add("dve", lambda e, ptok=ptok, L=L: e.tensor_tensor(H[0][:, 1 + ptok:1 + ptok + L], H[1][:, ptok + L:ptok:-1],
                                                                       H[0][:, 1 + ptok:1 + ptok + L], ALU.add),
                      reads=[rH[0], rH[1]], writes=[rH[0]])
            P.add("dve", lambda e, ch=ch: e.tensor_tensor(GB[ch][:], H[0][:, 1:1 + TPB], GB[ch][:], ALU.mult), reads=[rH[0], rgbuf[ch]], writes=[rgbuf[ch]])
            P.dma("sp", yrT[ch * 128:(ch + 1) * 128, base:base + TPB], GB[ch][:], reads=[rgbuf[ch]])

    for b in range(nbatch):
        do_batch(b)
    P.emit()
    return nc


AM2_POOL = "pool"
AM2_BLK = 4
AM2_LAG = 2
MG = 256
NCH = TPB // 128
DH = 256


def build_Am(nbatch, nc=None):
    NT = nbatch * TPB
    nc = nc or new_nc()
    uT = din(nc, "uTm", [D, NT], BF16)
    wq = din(nc, "wq", [D, 256], F32)
    wk = din(nc, "wk", [D, 256], F32)
    wvo = din(nc, "wvo", [D, 512], F32)
    wgt = din(nc, "wgt", [D, 4], F32)
    gb = din(nc, "gb", [128, 4], F32)
    cwd = din(nc, "cw", [128, 4, 4], F32)
    cbd = din(nc, "cb", [128, 4], F32)
    gn = din(nc, "gn", [128, 256], F32)
    ident = din(nc, "ident", [128, 128], BF16)
    tri = din(nc, "tri", [128, 2, 128], F32)
    negm = din(nc, "negm", [128, 2, 128], F32)
    ym = dout(nc, "ym", [NT, 256], BF16)
    P = Prog(nc)
    ps = Rot(P, 6, [128, 512], F32, psum=True)
    pt = Rot(P, 2, [128, 512], BF16, psum=True)
    tmp = Rot(P, 6, [128, MG + 3], F32)
    wqs = P.sbuf([128, KD, 256], BF16)
    wks = P.sbuf([128, KD, 256], BF16)
    wvos = P.sbuf([128, KD, 512], BF16)
    wgts = P.sbuf([128, KD, 4], BF16)
    r_w = Res()
    for dst, src in ((wqs, wq), (wks, wk), (wvos, wvo), (wgts, wgt)):
        P.dma("pool", dst[:], src.rearrange("(k p) c -> p k c", p=128), writes=[r_w])
    cst = {}
    r_c = Res()
    for name, src, shape, dt in (("gb", gb, [128, 4], F32), ("cw", cwd, [128, 4, 4], F32), ("cb", cbd, [128, 4], F32), ("gn", gn, [128, 256], F32),
                                 ("ident", ident, [128, 128], BF16), ("tri", tri, [128, 2, 128], F32), ("negm", negm, [128, 2, 128], F32)):
        cst[name] = P.sbuf(shape, dt)
        P.dma("sp", cst[name][:], src, writes=[r_c])
    ones = P.sbuf([128, 128], F32)
    P.add("dve", lambda e: e.memset(ones[:], 1.0), writes=[r_c])

    ub = Rot(P, 2, [128, KD, MG + 3], BF16)
    QK = [P.sbuf([128, TPB], BF16) for _ in range(4)]
    rqk = [[Res() for _ in range(NCH // 2)] for _ in range(4)]
    VA = P.sbuf([128, NCH, DH + 1], BF16)
    rva = [Res() for _ in range(NCH)]
    SO = P.sbuf([128, NCH, DH], BF16)
    rso = [Res() for _ in range(NCH)]
    GT = P.sbuf([128, NCH, 4], F32)
    rgt = Res()
    HF = P.sbuf([128, NCH, DH], F32)
    rhf = [Res() for _ in range(NCH)]
    C32 = [P.sbuf([128, DH + 1], F32) for _ in range(2)]
    CB = [P.sbuf([128, DH + 1], BF16) for _ in range(2)]
    rc32 = [Res(), Res()]
    rcb = [Res(), Res()]
    t128 = Rot(P, 8, [128, 128], F32)
    b128 = Rot(P, 8, [128, 128], BF16)
    kwr = Rot(P, 2, [128, DH], BF16)
    vsm = Rot(P, 16, [128, 8], F32)
    hsr = Rot(P, 3, [128, DH], F32)
    obr = Rot(P, 3, [128, DH], BF16)
    P.add("dve", lambda e: e.memset(VA[:, :, DH:DH + 1], 1.0), writes=rva)
    QSCALE = DH ** -0.5

    def do_group(base, g, t0, first, last):
        ubuf, rub = ub.next()
        hl = 0 if first else 2
        hr = 0 if last else 1
        if first:
            P.add("dve", lambda e: e.memset(ubuf[:, :, 0:2], 0.0), writes=[rub])
        if last:
            P.add("dve", lambda e: e.memset(ubuf[:, :, MG + 2:MG + 3], 0.0), writes=[rub])
        P.dma("sp", ubuf[:, :, 2 - hl:2 + MG + hr],
              uT[:, base + t0 - hl:base + t0 + MG + hr].rearrange("(k p) t -> p k t", p=128), writes=[rub])
        for ci in range(4):
            wsb = wqs if ci < 2 else wks
            c0 = (ci % 2) * 128
            pb, rp = ps.next()
            for k in range(KD):
                P.add("pe", lambda e, k=k, pb=pb, wsb=wsb, c0=c0: e.matmul(pb[:, 0:MG + 3], wsb[:, k, c0:c0 + 128], ubuf[:, k, :], start=(k == 0), stop=(k == KD - 1)),
                      reads=[r_w, rub], writes=[rp])
            cv, rcv = tmp.next()
            sg, rsg = tmp.next()
            P.add("dve", lambda e, pb=pb, cv=cv, ci=ci: e.tensor_scalar(cv[:, 0:MG], pb[:, 0:MG], cst["cw"][:, ci, 0:1], cst["cb"][:, ci:ci + 1], ALU.mult, ALU.add),
                  reads=[rp, r_c], writes=[rcv])
            for j in range(1, 4):
                P.add("dve", lambda e, pb=pb, cv=cv, ci=ci, j=j: e.scalar_tensor_tensor(cv[:, 0:MG], pb[:, j:j + MG], cst["cw"][:, ci, j:j + 1], cv[:, 0:MG], ALU.mult, ALU.add),
                      reads=[rp, r_c, rcv], writes=[rcv])
            P.add("act", lambda e, cv=cv, sg=sg: e.activation(sg[:, 0:MG], cv[:, 0:MG], AF.Sigmoid), reads=[rcv], writes=[rsg])
            sc = QSCALE if ci < 2 else 1.0
            P.add("dve", lambda e, cv=cv, sg=sg, ci=ci, sc=sc: e.scalar_tensor_tensor(QK[ci][:, t0:t0 + MG], cv[:, 0:MG], sc, sg[:, 0:MG], ALU.mult, ALU.mult),
                  reads=[rcv, rsg], writes=[rqk[ci][g]])
        for i in range(2):
            c = 2 * g + i
            pv, rpv = ps.next()
            pg, rpg = ps.next()
            for k in range(KD):
                P.add("pe", lambda e, k=k, pv=pv, i=i: e.matmul(pv[:, 0:512], ubuf[:, k, 2 + i * 128:2 + (i + 1) * 128], wvos[:, k, :], start=(k == 0), stop=(k == KD - 1)),
                      reads=[r_w, rub], writes=[rpv])
            for k in range(KD):
                P.add("pe", lambda e, k=k, pg=pg, i=i: e.matmul(pg[:, 0:4], ubuf[:, k, 2 + i * 128:2 + (i + 1) * 128], wgts[:, k, :], start=(k == 0), stop=(k == KD - 1)),
                      reads=[r_w, rub], writes=[rpg])
            P.add("act", lambda e, pv=pv, c=c: e.copy(VA[:, c, 0:DH], pv[:, 0:DH]), reads=[rpv], writes=[rva[c]])
            P.add("act", lambda e, pv=pv, c=c: e.activation(SO[:, c, :], pv[:, DH:2 * DH], AF.Sigmoid), reads=[rpv], writes=[rso[c]])
            P.add("dve", lambda e, pg=pg, c=c: e.tensor_tensor(GT[:, c, :], pg[:, 0:4], cst["gb"][:], ALU.add), reads=[rpg, r_c], writes=[rgt])

    def do_chunk(base, c, d, finalize):
        cs = slice(c * 128, (c + 1) * 128)
        g = c // 2
        ig = GT[:, c, 2 * d:2 * d + 1]
        lf = GT[:, c, 2 * d + 1:2 * d + 2]
        lastcol = 127 if d == 0 else 0
        LU, rLU = t128.next()
        P.add("dve", lambda e: e.tensor_scalar(LU[:], cst["tri"][:, d, :], lf, None, ALU.mult), reads=[r_c, rgt], writes=[rLU])
        prow, rprow = ps.next()
        P.add("pe", lambda e: e.matmul(prow[:, 0:128], ones[:], LU[:], start=True, stop=True), reads=[r_c, rLU], writes=[rprow])
        pcol, rpcol = ps.next()
        P.add("pe", lambda e: e.matmul(pcol[:, 0:1], cst["tri"][:, d, :], lf, start=True, stop=True), reads=[r_c, rgt], writes=[rpcol])
        sv, rsv = vsm.next()
        P.add("dve", lambda e: e.tensor_tensor(sv[:, 0:1], ig, pcol[:, 0:1], ALU.subtract), reads=[rgt, rpcol], writes=[rsv])
        P.add("dve", lambda e: e.tensor_copy(sv[:, 1:2], prow[:, lastcol:lastcol + 1]), reads=[rprow], writes=[rsv])
        DT, rDT = t128.next()
        P.add("dve", lambda e: e.tensor_tensor(DT[:], prow[:, 0:128], cst["negm"][:, d, :], ALU.add), reads=[rprow, r_c], writes=[rDT])
        P.add("act", lambda e: e.activation(DT[:], DT[:], AF.Exp, bias=sv[:, 0:1]), reads=[rDT, rsv], writes=[rDT])
        EB, rEB = t128.next()
        P.add("act", lambda e: e.activation(EB[:], prow[:, 0:128], AF.Exp), reads=[rprow], writes=[rEB])
        QS = []
        for i in range(2):
            qs, rqs = b128.next()
            P.add("dve", lambda e, i=i, qs=qs: e.tensor_tensor(qs[:], QK[i][:, cs], EB[:], ALU.mult), reads=[rqk[i][g], rEB], writes=[rqs])
            QS.append((qs, rqs))
        pst, rpst = ps.next()
        for i in range(2):
            P.add("pe", lambda e, i=i: e.matmul(pst[:, 0:128], QK[2 + i][:, cs], QK[i][:, cs], start=(i == 0), stop=(i == 1)),
                  reads=[rqk[2 + i][g], rqk[i][g]], writes=[rpst])
        ST, rST = b128.next()
        P.add("dve", lambda e: e.tensor_tensor(ST[:], pst[:, 0:128], DT[:], ALU.mult), reads=[rpst, rDT], writes=[rST])
        pnum, rpnum = ps.next()
        P.add("pe", lambda e: e.matmul(pnum[:, 0:DH + 1], ST[:], VA[:, c, :], start=True, stop=False), reads=[rST, rva[c]], writes=[rpnum])
        for i in range(2):
            P.add("pe", lambda e, i=i: e.matmul(pnum[:, 0:DH + 1], QS[i][0][:], CB[i][:], start=False, stop=(i == 1)),
                  reads=[QS[i][1], rcb[i]], writes=[rpnum])
        P.add("act", lambda e: e.activation(sv[:, 4:5], pnum[:, DH:DH + 1], AF.Abs), reads=[rpnum], writes=[rsv])
        P.add("dve", lambda e: e.tensor_scalar_max(sv[:, 4:5], sv[:, 4:5], 1.0), reads=[rsv], writes=[rsv])
        P.add("dve", lambda e: e.reciprocal(sv[:, 5:6], sv[:, 4:5]), reads=[rsv], writes=[rsv])
        if not finalize:
            P.add("act", lambda e: e.activation(HF[:, c, :], pnum[:, 0:DH], AF.Copy, scale=sv[:, 5:6]), reads=[rpnum, rsv], writes=[rhf[c]])
        else:
            hs, rhs = hsr.next()
            P.add("dve", lambda e: e.scalar_tensor_tensor(hs[:], pnum[:, 0:DH], sv[:, 5:6], HF[:, c, :], ALU.mult, ALU.add), reads=[rpnum, rsv, rhf[c]], writes=[rhs])
            st6, rst6 = vsm.next()
            P.add("dve", lambda e: e.bn_stats(st6[:, 0:6], hs[:]), reads=[rhs], writes=[rst6])
            mv, rmv = vsm.next()
            P.add("dve", lambda e: e.bn_aggr(mv[:, 0:2], st6[:, 0:6]), reads=[rst6], writes=[rmv])
            P.add("act", lambda e: e.activation(mv[:, 2:3], mv[:, 1:2], AF.Sqrt, bias=LN_EPS, scale=1.0), reads=[rmv], writes=[rmv])
            P.add("dve", lambda e: e.reciprocal(mv[:, 2:3], mv[:, 2:3]), reads=[rmv], writes=[rmv])
            P.add("dve", lambda e: e.tensor_scalar(hs[:], hs[:], mv[:, 0:1], mv[:, 2:3], ALU.subtract, ALU.mult), reads=[rhs, rmv], writes=[rhs])
            P.add("dve", lambda e: e.tensor_tensor(hs[:], hs[:], cst["gn"][:], ALU.mult), reads=[rhs, r_c], writes=[rhs])
            ob, rob = obr.next()
            P.add("dve", lambda e: e.tensor_tensor(ob[:], hs[:], SO[:, c, :], ALU.mult), reads=[rhs, rso[c]], writes=[rob])
            P.dma("sp", ym[base + c * 128:base + (c + 1) * 128, :], ob[:], reads=[rob])
        P.add("act", lambda e: e.activation(sv[:, 2:3], sv[:, 0:1], AF.Exp, bias=sv[:, 1:2]), reads=[rsv], writes=[rsv])
        P.add("act", lambda e: e.activation(sv[:, 3:4], sv[:, 1:2], AF.Exp), reads=[rsv], writes=[rsv])
        kw, rkw = kwr.next()
        for i in range(2):
            ptb, rpt = pt.next()
            P.add("pe", lambda e, i=i, ptb=ptb: e.transpose(ptb[:, 0:128], QK[2 + i][:, cs], cst["ident"][:]), reads=[rqk[2 + i][g], r_c], writes=[rpt])
            P.add("dve", lambda e, i=i, ptb=ptb: e.tensor_scalar(kw[:, i * 128:(i + 1) * 128], ptb[:, 0:128], sv[:, 2:3], None, ALU.mult), reads=[rpt, rsv], writes=[rkw])
        pcus = [ps.next(), ps.next()]
        for i in range(2):
            P.add("pe", lambda e, i=i: e.matmul(pcus[i][0][:, 0:DH + 1], kw[:, i * 128:(i + 1) * 128], VA[:, c, :], start=True, stop=True),
                  reads=[rkw, rva[c]], writes=[pcus[i][1]])
        for i in range(2):
            P.add("dve", lambda e, i=i: e.scalar_tensor_tensor(C32[i][:], C32[i][:], sv[:, 3:4], pcus[i][0][:, 0:DH + 1], ALU.mult, ALU.add),
                  reads=[rc32[i], rsv, pcus[i][1]], writes=[rc32[i]])
            P.add("act", lambda e, i=i: e.copy(CB[i][:], C32[i][:]), reads=[rc32[i]], writes=[rcb[i]])

    def zero_state():
        for i in range(2):
            P.add("dve", lambda e, i=i: e.memset(C32[i][:], 0.0), writes=[rc32[i]])
            P.add("dve", lambda e, i=i: e.memset(CB[i][:], 0.0), writes=[rcb[i]])

    def do_batch(b):
        base = b * TPB
        ngrp = TPB // MG
        for g in range(ngrp):
            first = g in (0, 1)
            last = g in (0, ngrp - 1)
            do_group(base, g, g * MG, first, last)
        for col in (1, 3):
            P.add("act", lambda e, col=col: e.activation(GT[:, :, col], GT[:, :, col], AF.Exp, scale=-1.0), reads=[rgt], writes=[rgt])
            P.add("act", lambda e, col=col: e.activation(GT[:, :, col], GT[:, :, col], AF.Ln, bias=1.0), reads=[rgt], writes=[rgt])
            P.add("dve", lambda e, col=col: e.tensor_scalar_mul(GT[:, :, col], GT[:, :, col], -1.0), reads=[rgt], writes=[rgt])
        zero_state()
        for c in range(NCH):
            do_chunk(base, c, 0, False)
        zero_state()
        for c in [1, 0] + list(range(NCH - 1, 1, -1)):
            do_chunk(base, c, 1, True)

    for b in range(nbatch):
        do_batch(b)
    P.emit()
    return nc


def build_Am2(nbatch, nc=None):
    NT = nbatch * TPB
    nc = nc or new_nc()
    uT = din(nc, "uTm", [D, NT], BF16)
    wq = din(nc, "wq", [D, 256], F32)
    wk = din(nc, "wk", [D, 256], F32)
    wvo = din(nc, "wvo", [D, 512], F32)
    wgt = din(nc, "wgt", [D, 4], F32)
    gb = din(nc, "gb", [128, 4], F32)
    cwd = din(nc, "cw", [128, 4, 4], F32)
    cbd = din(nc, "cb", [128, 4], F32)
    gn = din(nc, "gn", [128, 256], F32)
    ident = din(nc, "ident", [128, 128], BF16)
    tri = din(nc, "tri", [128, 2, 128], F32)
    negm = din(nc, "negm", [128, 2, 128], F32)
    ym = dout(nc, "ym", [NT, 256], BF16)
    P = Prog(nc)
    banks = [P.psum([128, 512], F32) for _ in range(7)]
    rbk = [Res(excl=True) for _ in range(7)]
    tbank = P.psum([128, 1024], BF16)
    rtb = Res(excl=True)
    ps = Rot(P, 0, None, None)
    ps.bufs, ps.res, ps.n = banks[0:6], rbk[0:6], 6
    tmp = Rot(P, 6, [128, MG + 3], F32)
    wqs = P.sbuf([128, KD, 256], BF16)
    wks = P.sbuf([128, KD, 256], BF16)
    wvos = P.sbuf([128, KD, 512], BF16)
    wgts = P.sbuf([128, KD, 4], BF16)
    r_w = Res()
    for dst, src in ((wqs, wq), (wks, wk), (wvos, wvo), (wgts, wgt)):
        P.dma("pool", dst[:], src.rearrange("(k p) c -> p k c", p=128), writes=[r_w])
    cst = {}
    r_c = Res()
    for name, src, shape, dt in (("gb", gb, [128, 4], F32), ("cw", cwd, [128, 4, 4], F32), ("cb", cbd, [128, 4], F32), ("gn", gn, [128, 256], F32),
                                 ("ident", ident, [128, 128], BF16), ("tri", tri, [128, 2, 128], F32), ("negm", negm, [128, 2, 128], F32)):
        cst[name] = P.sbuf(shape, dt)
        P.dma("sp", cst[name][:], src, writes=[r_c])
    ones = P.sbuf([128, 128], F32)
    P.add("dve", lambda e: e.memset(ones[:], 1.0), writes=[r_c])

    ub = Rot(P, 2, [128, KD, MG + 3], BF16)
    QK = [P.sbuf([128, TPB], BF16) for _ in range(4)]
    rqk = [[Res() for _ in range(NCH // 2)] for _ in range(4)]
    VA = P.sbuf([128, NCH, DH + 1], BF16)
    rva = [Res() for _ in range(NCH)]
    SO = P.sbuf([128, NCH, DH], BF16)
    rso = [Res() for _ in range(NCH)]
    GT = P.sbuf([128, NCH, 4], F32)
    rgt = Res()
    HF = P.sbuf([128, NCH, DH], F32)
    rhf = [Res() for _ in range(NCH)]
    C32 = P.sbuf([128, 2, DH + 1], F32)
    rc32 = Res()
    NS = 8
    LUa = P.sbuf([128, NS, 128], F32)
    PRa = P.sbuf([128, NS, 128], F32)
    rPR = [Res() for _ in range(NS)]
    DTa = P.sbuf([128, NS, 128], F32)
    EBa = P.sbuf([128, NS, 128], F32)
    QSa = P.sbuf([128, NS, 2, 128], BF16)
    STa = P.sbuf([128, NS, 128], BF16)
    KWa = P.sbuf([128, NS, DH], BF16)
    SVa = P.sbuf([128, NS, 8], F32)
    rLU = [Res() for _ in range(NS)]
    rDT = [Res() for _ in range(NS)]
    rEB = [Res() for _ in range(NS)]
    rQS = [Res() for _ in range(NS)]
    rST = [Res() for _ in range(NS)]
    rKW = [Res() for _ in range(NS)]
    rSV = [Res() for _ in range(NS)]
    CUs = Rot(P, 4, [128, 2, DH + 1], F32)
    CBr = [P.sbuf([128, 2, DH + 16], BF16) for _ in range(4)]
    rCBr = [Res() for _ in range(4)]
    vsm = Rot(P, 16, [128, 8], F32)
    hsr = Rot(P, 3, [128, DH], F32)
    obr = Rot(P, 3, [128, DH], BF16)
    P.add("dve", lambda e: e.memset(VA[:, :, DH:DH + 1], 1.0), writes=rva)
    QSCALE = DH ** -0.5

    def do_group(base, g, t0, first, last):
        ubuf, rub = ub.next()
        hl = 0 if first else 2
        hr = 0 if last else 1
        if first:
            P.add("dve", lambda e: e.memset(ubuf[:, :, 0:2], 0.0), writes=[rub])
        if last:
            P.add("dve", lambda e: e.memset(ubuf[:, :, MG + 2:MG + 3], 0.0), writes=[rub])
        P.dma("sp", ubuf[:, :, 2 - hl:2 + MG + hr],
              uT[:, base + t0 - hl:base + t0 + MG + hr].rearrange("(k p) t -> p k t", p=128), writes=[rub])
        for ci in range(4):
            wsb = wqs if ci < 2 else wks
            c0 = (ci % 2) * 128
            pb, rp = ps.next()
            for k in range(KD):
                P.add("pe", lambda e, k=k, pb=pb, wsb=wsb, c0=c0: e.matmul(pb[:, 0:MG + 3], wsb[:, k, c0:c0 + 128], ubuf[:, k, :], start=(k == 0), stop=(k == KD - 1)),
                      reads=[r_w, rub], writes=[rp])
            cv, rcv = tmp.next()
            sg, rsg = tmp.next()
            P.add("dve", lambda e, pb=pb, cv=cv, ci=ci: e.tensor_scalar(cv[:, 0:MG], pb[:, 0:MG], cst["cw"][:, ci, 0:1], cst["cb"][:, ci:ci + 1], ALU.mult, ALU.add),
                  reads=[rp, r_c], writes=[rcv])
            for j in range(1, 4):
                P.add("dve", lambda e, pb=pb, cv=cv, ci=ci, j=j: e.scalar_tensor_tensor(cv[:, 0:MG], pb[:, j:j + MG], cst["cw"][:, ci, j:j + 1], cv[:, 0:MG], ALU.mult, ALU.add),
                      reads=[rp, r_c, rcv], writes=[rcv])
            P.add("act", lambda e, cv=cv, sg=sg: e.activation(sg[:, 0:MG], cv[:, 0:MG], AF.Sigmoid), reads=[rcv], writes=[rsg])
            sc = QSCALE if ci < 2 else 1.0
            P.add("dve", lambda e, cv=cv, sg=sg, ci=ci, sc=sc: e.scalar_tensor_tensor(QK[ci][:, t0:t0 + MG], cv[:, 0:MG], sc, sg[:, 0:MG], ALU.mult, ALU.mult),
                  reads=[rcv, rsg], writes=[rqk[ci][g]])
        for i in range(2):
            c = 2 * g + i
            pv, rpv = ps.next()
            pg, rpg = ps.next()
            for k in range(KD):
                P.add("pe", lambda e, k=k, pv=pv, i=i: e.matmul(pv[:, 0:512], ubuf[:, k, 2 + i * 128:2 + (i + 1) * 128], wvos[:, k, :], start=(k == 0), stop=(k == KD - 1)),
                      reads=[r_w, rub], writes=[rpv])
            for k in range(KD):
                P.add("pe", lambda e, k=k, pg=pg, i=i: e.matmul(pg[:, 0:4], ubuf[:, k, 2 + i * 128:2 + (i + 1) * 128], wgts[:, k, :], start=(k == 0), stop=(k == KD - 1)),
                      reads=[r_w, rub], writes=[rpg])
            P.add("act", lambda e, pv=pv, c=c: e.copy(VA[:, c, 0:DH], pv[:, 0:DH]), reads=[rpv], writes=[rva[c]])
            P.add("act", lambda e, pv=pv, c=c: e.activation(SO[:, c, :], pv[:, DH:2 * DH], AF.Sigmoid), reads=[rpv], writes=[rso[c]])
            P.add("dve", lambda e, pg=pg, c=c: e.tensor_tensor(GT[:, c, :], pg[:, 0:4], cst["gb"][:], ALU.add), reads=[rpg, r_c], writes=[rgt])

    def block_X(base, chunks, d, par):
        lastcol = 127 if d == 0 else 0
        sl = [par * 4 + i for i in range(len(chunks))]

        def part0():
            for i, c in enumerate(chunks):
                P.add("dve", lambda e, i=i, c=c: e.tensor_scalar(LUa[:, sl[i], :], cst["tri"][:, d, :], GT[:, c, 2 * d + 1:2 * d + 2], None, ALU.mult),
                      reads=[r_c, rgt], writes=[rLU[sl[i]]])

        def part1():
            for i, c in enumerate(chunks):
                P.add("pe", lambda e, i=i: e.matmul(banks[0][:, i * 128:(i + 1) * 128], ones[:], LUa[:, sl[i], :], start=True, stop=True),
                      reads=[r_c, rLU[sl[i]]], writes=[rbk[0]])
                P.add("pe", lambda e, i=i, c=c: e.matmul(banks[2][:, 16 * i:16 * i + 1], cst["tri"][:, d, :], GT[:, c, 2 * d + 1:2 * d + 2], start=True, stop=True),
                      reads=[r_c, rgt], writes=[rbk[2]])

        def part2():
            for i, c in enumerate(chunks):
                P.add("act", lambda e, i=i: e.copy(PRa[:, sl[i], :], banks[0][:, i * 128:(i + 1) * 128]), reads=[rbk[0]], writes=[rPR[sl[i]]])
            for i, c in enumerate(chunks):
                sv = SVa[:, sl[i], :]
                P.add("dve", lambda e, i=i, c=c, sv=sv: e.tensor_tensor(sv[:, 0:1], GT[:, c, 2 * d:2 * d + 1], banks[2][:, 16 * i:16 * i + 1], ALU.subtract),
                      reads=[rgt, rbk[2]], writes=[rSV[sl[i]]])
                P.add("pool", lambda e, i=i: e.tensor_tensor(DTa[:, sl[i], :], PRa[:, sl[i], :], cst["negm"][:, d, :], ALU.add),
                      reads=[rPR[sl[i]], r_c], writes=[rDT[sl[i]]])
            for i, c in enumerate(chunks):
                sv = SVa[:, sl[i], :]
                P.add("act", lambda e, i=i, sv=sv: e.activation(DTa[:, sl[i], :], DTa[:, sl[i], :], AF.Exp, bias=sv[:, 0:1]), reads=[rDT[sl[i]], rSV[sl[i]]], writes=[rDT[sl[i]]])
                P.add("act", lambda e, i=i: e.activation(EBa[:, sl[i], :], PRa[:, sl[i], :], AF.Exp), reads=[rPR[sl[i]]], writes=[rEB[sl[i]]])
                P.add("act", lambda e, i=i, sv=sv: e.activation(sv[:, 2:3], sv[:, 0:1], AF.Exp, bias=PRa[:, sl[i], lastcol:lastcol + 1]), reads=[rSV[sl[i]], rPR[sl[i]]], writes=[rSV[sl[i]]])
                P.add("act", lambda e, i=i, sv=sv: e.activation(sv[:, 3:4], PRa[:, sl[i], lastcol:lastcol + 1], AF.Exp), reads=[rPR[sl[i]]], writes=[rSV[sl[i]]])

        def part3():
            for i, c in enumerate(chunks):
                cs = slice(c * 128, (c + 1) * 128)
                for h in range(2):
                    P.add(AM2_POOL, lambda e, i=i, h=h, cs=cs: e.tensor_tensor(QSa[:, sl[i], h, :], QK[h][:, cs], EBa[:, sl[i], :], ALU.mult),
                          reads=[rqk[h][c // 2], rEB[sl[i]]], writes=[rQS[sl[i]]])
            for i, c in enumerate(chunks):
                cs = slice(c * 128, (c + 1) * 128)
                for h in range(2):
                    P.add("pe", lambda e, i=i, h=h, cs=cs: e.matmul(banks[1][:, i * 128:(i + 1) * 128], QK[2 + h][:, cs], QK[h][:, cs], start=(h == 0), stop=(h == 1)),
                          reads=[rqk[2 + h][c // 2], rqk[h][c // 2]], writes=[rbk[1]])
                for h in range(2):
                    P.add("pe", lambda e, i=i, h=h, cs=cs: e.transpose(tbank[:, (2 * i + h) * 128:(2 * i + h + 1) * 128], QK[2 + h][:, cs], cst["ident"][:]),
                          reads=[rqk[2 + h][c // 2], r_c], writes=[rtb])
            for i, c in enumerate(chunks):
                sv = SVa[:, sl[i], :]
                P.add("dve", lambda e, i=i: e.tensor_tensor(STa[:, sl[i], :], banks[1][:, i * 128:(i + 1) * 128], DTa[:, sl[i], :], ALU.mult),
                      reads=[rbk[1], rDT[sl[i]]], writes=[rST[sl[i]]])
                P.add("act", lambda e, i=i, sv=sv: e.activation(KWa[:, sl[i], :], tbank[:, 2 * i * 128:(2 * i + 2) * 128], AF.Copy, scale=sv[:, 2:3]),
                      reads=[rtb, rSV[sl[i]]], writes=[rKW[sl[i]]])

        return [part0, part1, part2, part3]

    def block_YA(base, chunks, d, finalize, par, n0, i):
        sl = [par * 4 + t for t in range(len(chunks))]
        c = chunks[i]
        n = n0 + i
        sv = SVa[:, sl[i], :]
        cu, rcu = CUs.next()
        for h in range(2):
            P.add("pe", lambda e, i=i, h=h, c=c: e.matmul(banks[5 + h][:, 0:DH + 1], KWa[:, sl[i], h * 128:(h + 1) * 128], VA[:, c, :], start=True, stop=True),
                  reads=[rKW[sl[i]], rva[c]], writes=[rbk[5 + h]])
        for h in range(2):
            P.add("dve", lambda e, h=h, sv=sv: e.scalar_tensor_tensor(C32[:, h, :], C32[:, h, :], sv[:, 3:4], banks[5 + h][:, 0:DH + 1], ALU.mult, ALU.add),
                  reads=[rc32, rSV[sl[i]], rbk[5 + h]], writes=[rc32])
        P.add("pool", lambda e, n=n: e.tensor_copy(CBr[(n + 1) % 4][:, :, 0:DH + 1], C32[:]), reads=[rc32], writes=[rCBr[(n + 1) % 4]])

    def block_YB(base, chunks, d, finalize, par, n0, i):
        sl = [par * 4 + t for t in range(len(chunks))]
        c = chunks[i]
        n = n0 + i
        sv = SVa[:, sl[i], :]
        pn = banks[3 + (n % 2)]
        rpn = rbk[3 + (n % 2)]
        P.add("pe", lambda e, i=i, c=c, pn=pn: e.matmul(pn[:, 0:DH + 1], STa[:, sl[i], :], VA[:, c, :], start=True, stop=False), reads=[rST[sl[i]], rva[c]], writes=[rpn])
        for h in range(2):
            P.add("pe", lambda e, i=i, h=h, pn=pn, n=n: e.matmul(pn[:, 0:DH + 1], QSa[:, sl[i], h, :], CBr[n % 4][:, h, 0:DH + 1], start=False, stop=(h == 1)),
                  reads=[rQS[sl[i]], rCBr[n % 4]], writes=[rpn])
        dn, rdn = vsm.next()
        P.add("act", lambda e, pn=pn, dn=dn: e.activation(dn[:, 0:1], pn[:, DH:DH + 1], AF.Abs), reads=[rpn], writes=[rdn])
        P.add("dve", lambda e, dn=dn: e.tensor_scalar_max(dn[:, 0:1], dn[:, 0:1], 1.0), reads=[rdn], writes=[rdn])
        P.add("dve", lambda e, dn=dn: e.reciprocal(dn[:, 1:2], dn[:, 0:1]), reads=[rdn], writes=[rdn])
        if not finalize:
            P.add("act", lambda e, pn=pn, dn=dn, c=c: e.activation(HF[:, c, :], pn[:, 0:DH], AF.Copy, scale=dn[:, 1:2]), reads=[rpn, rdn], writes=[rhf[c]])
        else:
            hs, rhs = hsr.next()
            P.add("dve", lambda e, pn=pn, dn=dn, c=c, hs=hs: e.scalar_tensor_tensor(hs[:], pn[:, 0:DH], dn[:, 1:2], HF[:, c, :], ALU.mult, ALU.add),
                  reads=[rpn, rdn, rhf[c]], writes=[rhs])
            P.add("dve", lambda e, dn=dn, hs=hs: e.bn_stats(dn[:, 2:8], hs[:]), reads=[rhs], writes=[rdn])
            mv, rmv = vsm.next()
            P.add("dve", lambda e, dn=dn, mv=mv: e.bn_aggr(mv[:, 0:2], dn[:, 2:8]), reads=[rdn], writes=[rmv])
            P.add("act", lambda e, mv=mv: e.activation(mv[:, 2:3], mv[:, 1:2], AF.Sqrt, bias=LN_EPS, scale=1.0), reads=[rmv], writes=[rmv])
            P.add("dve", lambda e, mv=mv: e.reciprocal(mv[:, 2:3], mv[:, 2:3]), reads=[rmv], writes=[rmv])
            P.add("dve", lambda e, mv=mv, hs=hs: e.tensor_scalar(hs[:], hs[:], mv[:, 0:1], mv[:, 2:3], ALU.subtract, ALU.mult), reads=[rhs, rmv], writes=[rhs])
            P.add(AM2_POOL, lambda e, hs=hs: e.tensor_tensor(hs[:], hs[:], cst["gn"][:], ALU.mult), reads=[rhs, r_c], writes=[rhs])
            ob, rob = obr.next()
            P.add(AM2_POOL, lambda e, hs=hs, ob=ob, c=c: e.tensor_tensor(ob[:], hs[:], SO[:, c, :], ALU.mult), reads=[rhs, rso[c]], writes=[rob])
            P.dma("sp", ym[base + c * 128:base + (c + 1) * 128, :], ob[:], reads=[rob])


    def zero_state():
        P.add("dve", lambda e: e.memset(C32[:], 0.0), writes=[rc32])
        P.add("dve", lambda e: e.memset(CBr[0][:], 0.0), writes=[rCBr[0]])

    blk_counter = [0]

    def do_dir(base, order, d, finalize):
        zero_state()
        blocks = [order[s0:s0 + AM2_BLK] for s0 in range(0, len(order), AM2_BLK)]
        par0 = blk_counter[0]
        for p in block_X(base, blocks[0], d, par0 % 2):
            p()
        pending = []
        for k, chunks in enumerate(blocks):
            nxt = block_X(base, blocks[k + 1], d, (par0 + k + 1) % 2) if k + 1 < len(blocks) else []
            for t in range(max(4, len(chunks))):
                if t < len(nxt):
                    nxt[t]()
                if t < len(chunks):
                    args = (base, chunks, d, finalize, (par0 + k) % 2, k * AM2_BLK, t)
                    block_YA(*args)
                    pending.append(args)
                    if len(pending) > AM2_LAG:
                        block_YB(*pending.pop(0))
        while pending:
            block_YB(*pending.pop(0))
        blk_counter[0] += len(blocks)

    def do_batch(b):
        base = b * TPB
        ngrp = TPB // MG
        for g in range(ngrp):
            first = g in (0, 1)
            last = g in (0, ngrp - 1)
            do_group(base, g, g * MG, first, last)
        for col in (1, 3):
            P.add("act", lambda e, col=col: e.activation(GT[:, :, col], GT[:, :, col], AF.Exp, scale=-1.0), reads=[rgt], writes=[rgt])
            P.add("act", lambda e, col=col: e.activation(GT[:, :, col], GT[:, :, col], AF.Ln, bias=1.0), reads=[rgt], writes=[rgt])
            P.add("dve", lambda e, col=col: e.tensor_scalar_mul(GT[:, :, col], GT[:, :, col], -1.0), reads=[rgt], writes=[rgt])
        do_dir(base, list(range(NCH)), 0, False)
        do_dir(base, [1, 0] + list(range(NCH - 1, 1, -1)), 1, True)

    for b in range(nbatch):
        do_batch(b)
    P.emit()
    return nc


def build_A(nbatch):
    nc = new_nc()
    build_Arg(nbatch, nc)
    nc.all_engine_barrier()
    build_Am2(nbatch, nc)
    return nc


_PROGS = {}


def _prog(key, builder):
    if key not in _PROGS:
        _PROGS[key] = builder()
    return _PROGS[key]


def _run(nc, in_maps, tag=""):
    r = run_bass_kernel_spmd(nc, in_maps, core_ids=list(range(NCORE)))
    if getattr(r, "exec_time_ns", None):
        print(f"[kernel] launch {tag} exec_time_ns={r.exec_time_ns}", flush=True)
    return r.results


def _vec_pack(vecs):
    return np.ascontiguousarray(np.asarray(vecs, np.float32).reshape(16, KD, 128).transpose(2, 0, 1))


def _cm_perm():
    t = np.arange(SEQ)
    return (t % GRID) * GRID + (t // GRID)


def _core_tokens(b, half, with_ctx):
    lat = CTX + np.arange(half * (SEQ // 2), (half + 1) * (SEQ // 2))
    if with_ctx:
        return np.concatenate([np.arange(half * (CTX // 2), (half + 1) * (CTX // 2)), lat])
    return lat


def kernel(x, c, ctx, c_ctx, w_mod, b_mod, w_in, conv_rg_w, conv_rg_b, conv_m_w, conv_m_b, rg_wa, rg_ba,
           rg_wx, rg_bx, rg_lam, m_gate_b, m_gn_g, p_rg, p_m, w_out, ln_g, ln_b, ff_w1, ff_w3, ff_w2,
           router_w, router_b, ex_w1, ex_w3, ex_w2):
    f32 = lambda a: np.asarray(a, np.float32)
    x, c, ctx, c_ctx = f32(x), f32(c), f32(ctx), f32(c_ctx)
    w_mod, b_mod, w_in = f32(w_mod), f32(b_mod), f32(w_in)
    ncores = NCORE
    cores = [(j // 2, j % 2) for j in range(ncores)]

    cT = np.ascontiguousarray(np.concatenate([c, c_ctx[None]], 0).T)
    maps = []
    for j in range(ncores):
        l, q = divmod(j, 4)
        cs = slice(q * MOD_COLS, (q + 1) * MOD_COLS)
        maps.append({"cT": cT, "wm": np.ascontiguousarray(w_mod[l][:, cs]),
                     "bm": np.ascontiguousarray(np.broadcast_to(b_mod[l][cs], (5, MOD_COLS)))})
    res = _run(_prog("Kmod", build_Kmod), maps, "Kmod")
    mod = [np.concatenate([res[l * 4 + q]["mod"] for q in range(4)], 1).reshape(5, 6, D) for l in range(2)]
    SH1, SC1, G1, SH2, SC2, G2 = range(6)

    def vecs_for(l, b, nxt):
        v = np.zeros((16, D), np.float32)
        for kind, row in ((0, b), (1, 4)):
            o = kind * 6
            v[o + V_G1] = mod[l][row, G1]
            v[o + V_SC2] = mod[l][row, SC2]
            v[o + V_SH2] = mod[l][row, SH2]
            v[o + V_G2] = mod[l][row, G2]
            if nxt is not None:
                v[o + V_NSC1] = mod[nxt][row, SC1]
                v[o + V_NSH1] = mod[nxt][row, SH1]
        v[V_LN + 0], v[V_LN + 1] = ln_g[l, 0], ln_b[l, 0]
        v[V_LN + 2], v[V_LN + 3] = ln_g[l, 1], ln_b[l, 1]
        return _vec_pack(v)

    hfull = [np.concatenate([ctx[b], x[b]], 0) for b in range(NB)]

    groups0 = [(0, 128, 1)] + [(128 + 512 * i, 512, 0) for i in range(4)]
    T0 = 128 + 2048
    hT_core = []
    maps = []
    for (b, half) in cores:
        hT = np.ascontiguousarray(hfull[b][_core_tokens(b, half, True)].T)
        hT_core.append(hT)
        maps.append({"hT": hT, "vec": vecs_for(0, b, 0)})
    res = _run(_prog("Kln", lambda: build_Kln(groups0, T0)), maps, "Kln")
    u_core = [r["uT_out"] for r in res]

    perm = _cm_perm()
    tri_s = np.arange(128)
    U = (tri_s[:, None] <= tri_s[None, :]).astype(np.float32)
    Lo = (tri_s[:, None] >= tri_s[None, :]).astype(np.float32)
    tri = np.ascontiguousarray(np.stack([U, Lo], 1))
    negm = np.ascontiguousarray(((tri - 1.0) * 30000.0).astype(np.float32))
    ident = np.eye(128).astype(NPBF)

    def assemble_u(u_core):
        uT = np.empty((D, NB * TPB), NPBF)
        for j, (b, half) in enumerate(cores):
            uT[:, b * TPB + _core_tokens(b, half, True)] = u_core[j]
        uTm = uT.copy()
        for b in range(NB):
            lat = uT[:, b * TPB + CTX:(b + 1) * TPB]
            uTm[:, b * TPB + CTX:(b + 1) * TPB] = lat[:, perm]
        return uT, uTm

    def mixer(l, uT, uTm):
        wl = w_in[l]
        maps = []
        for j in range(ncores):
            cs = slice(j * 256, (j + 1) * 256)
            rgw = np.zeros((128, 8, 128), np.float32)
            rgb = np.zeros((128, 8), np.float32)
            lm = np.zeros((128, 4), np.float32)
            for d in range(2):
                for ch in range(2):
                    ia = (d * 2 + ch) * 2
                    blk = 2 * j + ch
                    rgw[:, ia, :] = rg_wa[l, d, blk]
                    rgw[:, ia + 1, :] = rg_wx[l, d, blk]
                    rgb[:, ia] = rg_ba[l, d, blk * 128:(blk + 1) * 128]
                    rgb[:, ia + 1] = rg_bx[l, d, blk * 128:(blk + 1) * 128]
                    lm[:, d * 2 + ch] = rg_lam[l, d, blk * 128:(blk + 1) * 128]
            maps.append({"uT": uT, "wx": np.ascontiguousarray(wl[:, cs]), "wg": np.ascontiguousarray(wl[:, C_RGG + j * 256:C_RGG + (j + 1) * 256]),
                         "rcw": np.ascontiguousarray(f32(conv_rg_w[l])[:, cs].reshape(4, 2, 128).transpose(2, 1, 0)),
                         "rcb": np.ascontiguousarray(f32(conv_rg_b[l])[cs].reshape(2, 128).T),
                         "rgw": rgw, "rgb": rgb, "lam": lm})
        for j in range(ncores):
            qs = slice(C_M + j * 256, C_M + (j + 1) * 256)
            ks = slice(C_M + W_M + j * 256, C_M + W_M + (j + 1) * 256)
            vs = slice(C_M + 2 * W_M + j * 256, C_M + 2 * W_M + (j + 1) * 256)
            os_ = slice(C_M + 3 * W_M + j * 256, C_M + 3 * W_M + (j + 1) * 256)
            gcols = [C_M + 4 * W_M + d * 16 + g * 8 + j for d in range(2) for g in range(2)]
            gbv = np.array([m_gate_b[l][d, g, j] for d in range(2) for g in range(2)], np.float32)
            cwm = f32(conv_m_w[l])
            cbm = f32(conv_m_b[l])
            cwj = np.concatenate([cwm[:, j * 256:(j + 1) * 256], cwm[:, W_M + j * 256:W_M + (j + 1) * 256]], 1)
            cbj = np.concatenate([cbm[j * 256:(j + 1) * 256], cbm[W_M + j * 256:W_M + (j + 1) * 256]])
            maps[j].update({"uTm": uTm, "wq": np.ascontiguousarray(wl[:, qs]), "wk": np.ascontiguousarray(wl[:, ks]),
                         "wvo": np.ascontiguousarray(np.concatenate([wl[:, vs], wl[:, os_]], 1)),
                         "wgt": np.ascontiguousarray(wl[:, gcols]),
                         "gb": np.ascontiguousarray(np.broadcast_to(gbv, (128, 4))),
                         "cw": np.ascontiguousarray(cwj.reshape(4, 4, 128).transpose(2, 1, 0)),
                         "cb": np.ascontiguousarray(cbj.reshape(4, 128).T),
                         "gn": np.ascontiguousarray(np.broadcast_to(f32(m_gn_g[l])[j * 256:(j + 1) * 256], (128, 256))),
                         "ident": ident, "tri": tri, "negm": negm})
        res = _run(_prog("A", lambda: build_A(NB)), maps, "A")
        yrT = np.concatenate([r["yrT"] for r in res], 0)
        ymT = np.empty((D, NB * TPB), NPBF)
        for j in range(ncores):
            ymj = res[j]["ym"]
            for b in range(NB):
                blk = ymj[b * TPB:(b + 1) * TPB]
                ymT[j * 256:(j + 1) * 256, b * TPB:b * TPB + CTX] = blk[:CTX].T
                lat = np.empty((SEQ, 256), NPBF)
                lat[perm] = blk[CTX:]
                ymT[j * 256:(j + 1) * 256, b * TPB + CTX:(b + 1) * TPB] = lat.T
        return yrT, ymT

    uT, uTm = assemble_u(u_core)
    yrT, ymT = mixer(0, uT, uTm)
    w_g0 = np.ascontiguousarray(w_in[0][:, C_G:])
    maps = []
    for j, (b, half) in enumerate(cores):
        tk = b * TPB + _core_tokens(b, half, True)
        maps.append({"yrT": np.ascontiguousarray(yrT[:, tk]), "ymT": np.ascontiguousarray(ymT[:, tk]), "uT": u_core[j], "hT": hT_core[j],
                     "w_g": w_g0, "p_rg": f32(p_rg[0]), "p_m": f32(p_m[0]), "w_out": f32(w_out[0]), "vec": vecs_for(0, b, 1),
                     "w1": f32(ff_w1[0]), "w3": f32(ff_w3[0]), "w2": f32(ff_w2[0])})
    res = _run(_prog("B0", lambda: build_B(groups0, T0, True, False)), maps, "B0")
    h1_core = [r["hT_out"] for r in res]
    u1_core = [r["uT_out"] for r in res]

    uT, uTm = assemble_u(u1_core)
    yrT, ymT = mixer(1, uT, uTm)
    groups1 = [(512 * i, 512, 0) for i in range(4)]
    T1 = 2048
    w_g1 = np.ascontiguousarray(w_in[1][:, C_G:])
    rb = np.ascontiguousarray(np.broadcast_to(f32(router_b[0]), (128, 8)))
    maps = []
    for j, (b, half) in enumerate(cores):
        tk = b * TPB + _core_tokens(b, half, False)
        maps.append({"yrT": np.ascontiguousarray(yrT[:, tk]), "ymT": np.ascontiguousarray(ymT[:, tk]),
                     "uT": np.ascontiguousarray(u1_core[j][:, 128:]), "hT": np.ascontiguousarray(h1_core[j][:, 128:]),
                     "w_g": w_g1, "p_rg": f32(p_rg[1]), "p_m": f32(p_m[1]), "w_out": f32(w_out[1]), "vec": vecs_for(1, b, None),
                     "rw": f32(router_w[0]), "rb": rb})
    res = _run(_prog("B1", lambda: build_B(groups1, T1, False, True)), maps, "B1")
    hmid_core = [r["h1T_out"] for r in res]
    vT_all = np.concatenate([r["vT_out"] for r in res], 1)
    gw_all = np.concatenate([r["gw"] for r in res], 0)

    NTOK = gw_all.shape[0]
    top2 = np.argsort(-gw_all, axis=1, kind="stable")[:, :2]
    top2.sort(axis=1)
    lists = [np.nonzero((top2 == e).any(1))[0] for e in range(8)]
    nblk = [-(-len(l) // 512) for l in lists]
    plan = None
    for NS in range(max(1, -(-sum(nblk) // 8)), max(nblk) + 1):
        for K1 in range(NS, -1, -1):
            over = [max(0, n - K1) for n in nblk]
            if K1 == NS:
                ok = sum(over) == 0
                need = 0
            else:
                need = sum(-(-o // (NS - K1)) for o in over)
                ok = need <= 8
            if ok:
                plan = (NS, K1)
                break
        if plan:
            break
    NS, K1 = plan
    print(f"[kernel] moe counts={[len(l) for l in lists]} NS={NS} K1={K1}", flush=True)
    sec = []
    for e in range(8):
        o = max(0, nblk[e] - K1)
        b0 = K1
        while o > 0:
            nb_ = min(o, NS - K1)
            sec.append((e, b0, nb_))
            b0 += nb_
            o -= nb_
    while len(sec) < 8:
        sec.append((len(sec), 0, 0))
    T_C = NS * 512
    pos = np.zeros((NTOK, 8), np.int64)
    for e in range(8):
        pos[lists[e], e] = np.arange(len(lists[e]))
    maps = []
    slot_src = []
    for ci in range(8):
        vT = np.zeros((D, T_C), NPBF)
        wr = np.zeros((T_C,), np.float32)
        srcs = []
        blocks = [(ci, bb) for bb in range(min(nblk[ci], K1))]
        blocks += [None] * (K1 - len(blocks))
        se, sb0, snb = sec[ci]
        blocks += [(se, sb0 + i) for i in range(snb)] + [None] * (NS - K1 - snb)
        for sl, blk in enumerate(blocks):
            if blk is None:
                continue
            e, bb = blk
            idx = lists[e][bb * 512:(bb + 1) * 512]
            vT[:, sl * 512:sl * 512 + len(idx)] = vT_all[:, idx]
            wr[sl * 512:sl * 512 + len(idx)] = gw_all[idx, e]
        slot_src.append(blocks)
        m = {"vT": vT, "wrow": np.ascontiguousarray(np.broadcast_to(wr, (128, T_C))),
             "w1": f32(ex_w1[0][ci]), "w3": f32(ex_w3[0][ci]), "w2": f32(ex_w2[0][ci])}
        if K1 < NS:
            m.update({"w1b": f32(ex_w1[0][se]), "w3b": f32(ex_w3[0][se]), "w2b": f32(ex_w2[0][se])})
        maps.append(m)
    res = _run(_prog(("C", NS, K1), lambda: build_C(NS, K1)), maps, f"C{NS}_{K1}")
    Y = [np.zeros((D, nblk[e] * 512), np.float32) for e in range(8)]
    for ci in range(8):
        for sl, blk in enumerate(slot_src[ci]):
            if blk is None:
                continue
            e, bb = blk
            Y[e][:, bb * 512:(bb + 1) * 512] = res[ci]["yT"][:, sl * 512:(sl + 1) * 512]

    maps = []
    for j, (b, half) in enumerate(cores):
        tl = np.arange(j * T1, (j + 1) * T1)
        ya = np.empty((D, T1), np.float32)
        yb = np.empty((D, T1), np.float32)
        for e in range(8):
            ma = top2[tl, 0] == e
            mb = top2[tl, 1] == e
            ya[:, ma] = Y[e][:, pos[tl[ma], e]]
            yb[:, mb] = Y[e][:, pos[tl[mb], e]]
        maps.append({"yaT": ya, "ybT": yb, "h1T": hmid_core[j], "vec": vecs_for(1, b, None)})
    res = _run(_prog("D", lambda: build_D(T1)), maps, "D")
    out = np.empty((NB, SEQ, D), np.float32)
    for j, (b, half) in enumerate(cores):
        out[b, half * (SEQ // 2):(half + 1) * (SEQ // 2)] = res[j]["oT"].T
    return out
```
